# Optimizing a Trainium2 kernel written in Bass

```python
import math
import jax, jax.numpy as jnp
from jax import lax
import numpy as np

D_MODEL = 2048
BATCH = 4
SEQ = 2048
DEPTH = 1

HEAD_DIM = 64
N_ATTN_HEADS = 16
D_ATTN = N_ATTN_HEADS * HEAD_DIM
SSM_GROUP = 16
N_SSM_GROUPS = 64
D_SSM = N_SSM_GROUPS * SSM_GROUP
SSM_STATE = 64
D_MIX = D_ATTN + D_SSM
D_IN_PROJ = 3 * D_ATTN + D_SSM
DILATED_BRANCHES = ((128, 1), (512, 4), (2048, 16))
BLK = 128
N_BUCKETS = 32
MAX_DISTANCE = 2048
PEER_HEADS = 8
PEER_KEYS = 128
PEER_EXPERTS = PEER_KEYS * PEER_KEYS
PEER_QDIM = 256
PEER_TOPK = 16
PEER_TOKEN_BLOCK = 128
EPS = 1e-6
NEG = -1e30

kernel_name = "hymba_s5_dilated_attn_peer_layer"


def rmsnorm(x, g):
    x32 = x.astype(jnp.float32)
    y = x32 * lax.rsqrt(jnp.mean(x32 * x32, axis=-1, keepdims=True) + EPS)
    return (y * g.astype(jnp.float32)).astype(x.dtype)


def t5_bucket(dist):
    max_exact = N_BUCKETS // 2
    n = jnp.maximum(dist, 0)
    nf = jnp.maximum(n, 1).astype(jnp.float32)
    large = max_exact + (jnp.log(nf / max_exact) / math.log(MAX_DISTANCE / max_exact)
                         * (N_BUCKETS - max_exact)).astype(jnp.int32)
    large = jnp.minimum(large, N_BUCKETS - 1)
    return jnp.where(n < max_exact, n, large)


def dilated_branch(q, k, v, rel_bias, window, dil):
    B, S, H, Dh = q.shape
    L = S // dil
    W = window // dil
    Lp = -(-L // BLK) * BLK
    nb = Lp // BLK

    def sub(t, front):
        t = t.reshape(B, L, dil, H, Dh)
        return jnp.pad(t, ((0, 0), (front, Lp - L), (0, 0), (0, 0), (0, 0)))

    qb = sub(q, 0).reshape(B, nb, BLK, dil, H, Dh)
    kp = sub(k, BLK).reshape(B, nb + 1, BLK, dil, H, Dh)
    vp = sub(v, BLK).reshape(B, nb + 1, BLK, dil, H, Dh)
    kb = jnp.concatenate([kp[:, :-1], kp[:, 1:]], axis=2)
    vb = jnp.concatenate([vp[:, :-1], vp[:, 1:]], axis=2)

    qi = jnp.arange(BLK)[:, None]
    kj = jnp.arange(2 * BLK)[None, :]
    rel = qi - kj + BLK
    blk_idx = jnp.arange(nb)[:, None, None]
    valid = (rel >= 0) & (rel <= W) & (blk_idx * BLK - BLK + kj >= 0)
    bias = jnp.transpose(rel_bias.astype(jnp.float32)[t5_bucket(rel * dil)], (2, 0, 1))

    s = jnp.einsum('bnqrhd,bnkrhd->bnrhqk', qb, kb).astype(jnp.float32) / math.sqrt(Dh)
    s = jnp.where(valid[None, :, None, None], s + bias, NEG)
    m = jnp.max(s, axis=-1)
    p = jnp.exp(s - m[..., None])
    l = jnp.sum(p, axis=-1)
    o = jnp.einsum('bnrhqk,bnkrhd->bnrhqd', p, vb.astype(jnp.float32)) / l[..., None]

    o = jnp.transpose(o, (0, 1, 4, 2, 3, 5)).reshape(B, Lp, dil, H, Dh)[:, :L].reshape(B, S, H, Dh)
    m = jnp.transpose(m, (0, 1, 4, 2, 3)).reshape(B, Lp, dil, H)[:, :L].reshape(B, S, H)
    l = jnp.transpose(l, (0, 1, 4, 2, 3)).reshape(B, Lp, dil, H)[:, :L].reshape(B, S, H)
    return o, m, l


def dilated_attention(q, k, v, rel_bias):
    outs, ms, ls = [], [], []
    for window, dil in DILATED_BRANCHES:
        o, m, l = dilated_branch(q, k, v, rel_bias, window, dil)
        outs.append(o)
        ms.append(m)
        ls.append(l)
    m = jnp.stack(ms)
    w = jnp.stack(ls) * jnp.exp(m - jnp.max(m, axis=0, keepdims=True))
    o = jnp.einsum('nbsh,nbshd->bshd', w, jnp.stack(outs)) / jnp.sum(w, axis=0)[..., None]
    return o


def s5_mixer(u, lam_re, lam_im, log_dt, b_re, b_im, c_re, c_im, d_skip, glu_w, glu_b):
    B, S, _ = u.shape
    f32 = jnp.float32
    u32 = u.astype(f32).reshape(B, S, N_SSM_GROUPS, SSM_GROUP)
    lr, li = lam_re.astype(f32), lam_im.astype(f32)
    dt = jnp.exp(log_dt.astype(f32))[:, None]
    mag = jnp.exp(lr * dt)
    a_re, a_im = mag * jnp.cos(li * dt), mag * jnp.sin(li * dt)
    den = lr * lr + li * li
    f_re = ((a_re - 1.0) * lr + a_im * li) / den
    f_im = (a_im * lr - (a_re - 1.0) * li) / den
    br, bi = b_re.astype(f32), b_im.astype(f32)
    bb_re = f_re[..., None] * br - f_im[..., None] * bi
    bb_im = f_re[..., None] * bi + f_im[..., None] * br
    bu_re = jnp.einsum('bsgc,gnc->bsgn', u32, bb_re)
    bu_im = jnp.einsum('bsgc,gnc->bsgn', u32, bb_im)
    A_re = jnp.broadcast_to(a_re, bu_re.shape)
    A_im = jnp.broadcast_to(a_im, bu_im.shape)

    def combine(e1, e2):
        a1r, a1i, b1r, b1i = e1
        a2r, a2i, b2r, b2i = e2
        return (a2r * a1r - a2i * a1i,
                a2r * a1i + a2i * a1r,
                a2r * b1r - a2i * b1i + b2r,
                a2r * b1i + a2i * b1r + b2i)

    _, _, x_re, x_im = lax.associative_scan(combine, (A_re, A_im, bu_re, bu_im), axis=1)
    y = (jnp.einsum('bsgn,gcn->bsgc', x_re, c_re.astype(f32))
         - jnp.einsum('bsgn,gcn->bsgc', x_im, c_im.astype(f32))
         + d_skip.astype(f32) * u32)
    y = jax.nn.gelu(y, approximate=False).reshape(B, S, D_SSM)
    y = y * jax.nn.sigmoid(y @ glu_w.astype(f32) + glu_b.astype(f32))
    return y.astype(u.dtype)


def peer(h, w_q, keys1, keys2, u_tab, v_tab):
    B, S, D = h.shape
    T = B * S
    ht = h.reshape(T, D)
    q = (ht @ w_q).reshape(T, PEER_HEADS, 2, PEER_QDIM // 2)
    s1 = jnp.einsum('thd,hkd->thk', q[:, :, 0], keys1).astype(jnp.float32)
    s2 = jnp.einsum('thd,hkd->thk', q[:, :, 1], keys2).astype(jnp.float32)
    v1, i1 = lax.top_k(s1, PEER_TOPK)
    v2, i2 = lax.top_k(s2, PEER_TOPK)
    cand = (v1[..., :, None] + v2[..., None, :]).reshape(T, PEER_HEADS, PEER_TOPK * PEER_TOPK)
    sc, ci = lax.top_k(cand, PEER_TOPK)
    e1 = jnp.take_along_axis(i1, ci // PEER_TOPK, axis=-1)
    e2 = jnp.take_along_axis(i2, ci % PEER_TOPK, axis=-1)
    experts = e1 * PEER_KEYS + e2
    gates = jax.nn.softmax(sc, axis=-1)

    n_blk = T // PEER_TOKEN_BLOCK
    K = PEER_HEADS * PEER_TOPK

    def block(args):
        hb, eb, gb = args
        a = jnp.einsum('td,tkd->tk', hb, u_tab[eb]).astype(jnp.float32)
        w = gb * jax.nn.gelu(a, approximate=False)
        return jnp.einsum('tk,tkd->td', w, v_tab[eb].astype(jnp.float32))

    out = lax.map(block, (ht.reshape(n_blk, PEER_TOKEN_BLOCK, D),
                          experts.reshape(n_blk, PEER_TOKEN_BLOCK, K),
                          gates.reshape(n_blk, PEER_TOKEN_BLOCK, K)))
    return out.reshape(B, S, D).astype(h.dtype)


def setup_inputs(seed: int = 0) -> dict:
    key = jax.random.key(seed)
    ks = jax.random.split(key, 26)
    f32 = jnp.float32
    nrm = lambda k, shape, scale: jax.random.normal(k, shape, f32) * scale
    gain = lambda k, n: 1.0 + 0.02 * jax.random.normal(k, (n,), f32)
    n_idx = jnp.arange(SSM_STATE, dtype=f32)[None, :]
    return {
        "x": nrm(ks[0], (BATCH, SEQ, D_MODEL), 1.0),
        "norm_mix_g": gain(ks[1], D_MODEL),
        "w_in": nrm(ks[2], (D_MODEL, D_IN_PROJ), D_MODEL ** -0.5),
        "q_norm_g": gain(ks[3], HEAD_DIM),
        "k_norm_g": gain(ks[4], HEAD_DIM),
        "rel_bias": nrm(ks[5], (N_BUCKETS, N_ATTN_HEADS), 0.5),
        "ssm_lambda_re": -0.5 + 0.01 * jax.random.normal(ks[6], (N_SSM_GROUPS, SSM_STATE), f32),
        "ssm_lambda_im": math.pi * n_idx + 0.01 * jax.random.normal(ks[7], (N_SSM_GROUPS, SSM_STATE), f32),
        "ssm_log_dt": jax.random.uniform(ks[8], (N_SSM_GROUPS,), f32, math.log(1e-3), math.log(1e-1)),
        "ssm_b_re": nrm(ks[9], (N_SSM_GROUPS, SSM_STATE, SSM_GROUP), (2 * SSM_GROUP) ** -0.5),
        "ssm_b_im": nrm(ks[10], (N_SSM_GROUPS, SSM_STATE, SSM_GROUP), (2 * SSM_GROUP) ** -0.5),
        "ssm_c_re": nrm(ks[11], (N_SSM_GROUPS, SSM_GROUP, SSM_STATE), (2 * SSM_STATE) ** -0.5),
        "ssm_c_im": nrm(ks[12], (N_SSM_GROUPS, SSM_GROUP, SSM_STATE), (2 * SSM_STATE) ** -0.5),
        "ssm_d": nrm(ks[13], (N_SSM_GROUPS, SSM_GROUP), 1.0),
        "ssm_glu_w": nrm(ks[14], (D_SSM, D_SSM), D_SSM ** -0.5),
        "ssm_glu_b": nrm(ks[15], (D_SSM,), 0.01),
        "attn_out_g": gain(ks[16], D_ATTN),
        "ssm_out_g": gain(ks[17], D_SSM),
        "w_out": nrm(ks[18], (D_MIX, D_MODEL), D_MIX ** -0.5),
        "norm_ffn_g": gain(ks[19], D_MODEL),
        "peer_w_q": nrm(ks[20], (D_MODEL, PEER_HEADS * PEER_QDIM), D_MODEL ** -0.5),
        "peer_keys1": nrm(ks[21], (PEER_HEADS, PEER_KEYS, PEER_QDIM // 2), (PEER_QDIM // 2) ** -0.5),
        "peer_keys2": nrm(ks[22], (PEER_HEADS, PEER_KEYS, PEER_QDIM // 2), (PEER_QDIM // 2) ** -0.5),
        "peer_u": nrm(ks[23], (PEER_EXPERTS, D_MODEL), D_MODEL ** -0.5),
        "peer_v": nrm(ks[24], (PEER_EXPERTS, D_MODEL), 0.5),
    }


def reference(x, norm_mix_g, w_in, q_norm_g, k_norm_g, rel_bias,
              ssm_lambda_re, ssm_lambda_im, ssm_log_dt, ssm_b_re, ssm_b_im,
              ssm_c_re, ssm_c_im, ssm_d, ssm_glu_w, ssm_glu_b,
              attn_out_g, ssm_out_g, w_out, norm_ffn_g,
              peer_w_q, peer_keys1, peer_keys2, peer_u, peer_v):
    B, S, _ = x.shape
    for _layer in range(DEPTH):
        h = rmsnorm(x, norm_mix_g)
        proj = h @ w_in
        q = proj[..., :D_ATTN].reshape(B, S, N_ATTN_HEADS, HEAD_DIM)
        k = proj[..., D_ATTN:2 * D_ATTN].reshape(B, S, N_ATTN_HEADS, HEAD_DIM)
        v = proj[..., 2 * D_ATTN:3 * D_ATTN].reshape(B, S, N_ATTN_HEADS, HEAD_DIM)
        u = proj[..., 3 * D_ATTN:]
        q = rmsnorm(q, q_norm_g)
        k = rmsnorm(k, k_norm_g)
        attn = dilated_attention(q, k, v, rel_bias).reshape(B, S, D_ATTN).astype(x.dtype)
        ssm = s5_mixer(u, ssm_lambda_re, ssm_lambda_im, ssm_log_dt, ssm_b_re, ssm_b_im,
                       ssm_c_re, ssm_c_im, ssm_d, ssm_glu_w, ssm_glu_b)
        mixed = jnp.concatenate([rmsnorm(attn, attn_out_g), rmsnorm(ssm, ssm_out_g)], axis=-1)
        x = x + (mixed @ w_out).astype(x.dtype)
        x = x + peer(rmsnorm(x, norm_ffn_g), peer_w_q, peer_keys1, peer_keys2, peer_u, peer_v)
    return x
```

```python
import math
from contextlib import ExitStack

import numpy as np
import concourse.bass as bass
import concourse.mybir as mybir
from concourse.bass_utils import run_bass_kernel_spmd

F32 = mybir.dt.float32
BF16 = mybir.dt.bfloat16
I32 = mybir.dt.int32
AF = mybir.ActivationFunctionType
ALU = mybir.AluOpType

D = 2048
EPS = 1e-6
GW = 2560


class _Res:
    __slots__ = ("w", "rs")

    def __init__(self):
        self.w = None
        self.rs = {}


class Region:
    def __init__(self, lo, hi):
        self.lo, self.hi, self.top = lo, hi, lo

    def take(self, nbytes, name=""):
        off = self.top
        self.top += nbytes
        assert self.top <= self.hi, f"SBUF region overflow at {name}: {self.top} > {self.hi}"
        return off


KBYTE = 1024
SB0 = 16512


class KB:
    ENG = ("pe", "act", "dve", "pool", "sp")

    def __init__(self, nc):
        self.nc = nc
        self.streams = {e: [] for e in self.ENG}
        self.sems = {}
        self.cnt = {}
        self.waited = {e: {} for e in self.ENG}
        self.res = {}
        self.es = ExitStack()

    def sb(self, name, shape, dtype, region):
        nbytes = int(np.prod(shape[1:])) * (2 if dtype == BF16 else 4)
        nbytes = (nbytes + 63) // 64 * 64
        off = region.take(nbytes, name) + SB0
        self.nalloc = getattr(self, "nalloc", 0) + 1
        return self.nc.alloc_sbuf_tensor_at(f"{name}_{self.nalloc}", list(shape), dtype, offset=off)

    def ps(self, name, shape, dtype, stack=None):
        return (stack or self.es).enter_context(self.nc.psum_tensor(name, list(shape), dtype))

    def _sem(self, name):
        if name not in self.sems:
            self.sems[name] = self.es.enter_context(self.nc.semaphore(name))
            self.cnt[name] = 0
        return name

    def _deps(self, eng, reads, writes):
        need = {}

        def add(s, v):
            if need.get(s, 0) < v:
                need[s] = v

        for r in reads:
            rr = self.res.get(r)
            if rr is not None and rr.w is not None:
                add(*rr.w)
        for w in writes:
            rr = self.res.get(w)
            if rr is not None:
                if rr.w is not None:
                    add(*rr.w)
                for s, v in rr.rs.items():
                    add(s, v)
        out = []
        wd = self.waited[eng]
        for s, v in need.items():
            if eng == "pe" and s == "e_pe":
                continue
            if wd.get(s, 0) < v:
                wd[s] = v
                out.append((s, v))
        return out

    def op(self, eng, fn, reads=(), writes=(), sem=None, inc=1, wr_late=()):
        waits = self._deps(eng, reads, writes)
        s = self._sem(sem or ("e_" + eng))
        self.cnt[s] += inc
        ev = (s, self.cnt[s])
        self.streams[eng].append((waits, fn, s, inc))
        for r in reads:
            rr = self.res.setdefault(r, _Res())
            if rr.rs.get(ev[0], 0) < ev[1]:
                rr.rs[ev[0]] = ev[1]
        for w in tuple(writes) + tuple(wr_late):
            rr = self.res.setdefault(w, _Res())
            rr.w = ev
            rr.rs = {}
        return ev

    def dma(self, out, in_, reads=(), writes=(), chan="d0", eng="sp", **kw):
        return self.op(eng, lambda e: e.dma_start(out=out, in_=in_, **kw), reads, writes,
                       sem="dma_" + chan, inc=16)

    def wait_all(self, eng, keys):
        waits = self._deps(eng, keys, ())
        self.streams[eng].append((waits, None, None, 0))

    def flush(self):
        for eng in self.ENG:
            waits = []
            wd = self.waited[eng]
            for s, v in self.cnt.items():
                if v > 0 and wd.get(s, 0) < v:
                    wd[s] = v
                    waits.append((s, v))
            self.streams[eng].append((waits, None, None, 0))
        with self.nc.Block() as block:
            def mk(stream):
                def body(e):
                    for waits, fn, s, inc in stream:
                        for ws, wv in waits:
                            e.wait_ge(self.sems[ws], wv)
                        if fn is not None:
                            fn(e).then_inc(self.sems[s], inc)
                return body
            block.tensor(mk(self.streams["pe"]))
            block.scalar(mk(self.streams["act"]))
            block.vector(mk(self.streams["dve"]))
            block.gpsimd(mk(self.streams["pool"]))
            block.sync(mk(self.streams["sp"]))
        self.streams = {e: [] for e in self.ENG}


def build(stop=None, dbg=False):
    nc = bass.Bass("TRN2", target_bir_lowering=False)
    k = KB(nc)
    KB_ = KBYTE

    def din(name, shape, dt=F32):
        return nc.dram_tensor(name, list(shape), dt, kind="ExternalInput").ap()

    def dout(name, shape, dt=F32):
        return nc.dram_tensor(name, list(shape), dt, kind="ExternalOutput").ap()

    x_all = din("x_all", [2048, D])
    gmix = din("gmix", [128, 16])
    w_in_t = din("w_in_t", [32, 128, 16, 128])
    gqk = din("gqk", [128, 2])
    relb = din("relb", [32, 16])
    ohm = din("ohm", [32, GW])
    kvalid = din("kvalid", [128, 16])
    cmats = din("cmats", [4, 128, 128])
    gmixed = din("gmixed", [128, 16])
    gsc = nc.dram_tensor("gsc", [16, GW], BF16, kind="Internal").ap()
    y = dout("y", [1024, D])

    def dbg_dump(name, src_ap, shape, key, dt=F32):
        if not dbg:
            return
        o = dout("dbg_" + name, shape, dt)
        k.dma(o, src_ap, reads=[key], writes=["dbg_" + name], chan="dbg")

    banks = [k.ps(f"bank{i}", [128, 512], F32) for i in range(8)]
    PS = lambda i: ("ps", i)

    RC = Region(0, 8 * KB_)
    cm = k.sb("cm", [128, 4, 128], F32, RC)
    k.dma(cm[:], cmats.rearrange("c p j -> p c j"), writes=["cm"], chan="c0")
    identb = k.sb("identb", [128, 128], BF16, RC)
    antib = k.sb("antib", [128, 128], BF16, RC)
    k.op("dve", lambda e: e.tensor_copy(out=identb[:], in_=cm[:, 0, :]), ["cm"], ["identb"])
    k.op("dve", lambda e: e.tensor_copy(out=antib[:], in_=cm[:, 1, :]), ["cm"], ["antib"])
    blk64 = cm[:, 2, :]
    onesf = cm[:, 3, :]
    gqk_sb = k.sb("gqk_sb", [128, 2], F32, RC)
    k.dma(gqk_sb[:], gqk[:, :], writes=["gqk"], chan="c1")
    kv_sb = k.sb("kv_sb", [128, 16], F32, RC)
    k.dma(kv_sb[:], kvalid[:, :], writes=["kv"], chan="c2")
    gmixed_sb = k.sb("gmixed_sb", [128, 16], F32, RC)
    k.dma(gmixed_sb[:], gmixed[:, :], writes=["gmixed"], chan="c3")
    gmix_sb = k.sb("gmix_sb", [128, 16], F32, RC)
    k.dma(gmix_sb[:], gmix[:, :], writes=["gmix"], chan="c4")

    RM = Region(8 * KB_, 120 * KB_)
    qT = k.sb("qT", [128, 8, 1024], BF16, RM)
    kT = k.sb("kT", [128, 8, 2048], BF16, RM)
    Vt = k.sb("Vt", [128, 16, 1024], BF16, RM)
    uT = k.sb("uT", [128, 8, 2048], BF16, RM)

    R = Region(120 * KB_, 207 * KB_)
    hnT2 = [k.sb(f"hnT{i}", [128, 16, 512], BF16, R) for i in range(2)]
    xbuf = [k.sb(f"xbuf{i}", [128, D], F32, R) for i in range(2)]
    hnb = [k.sb(f"hnb{i}", [128, D], BF16, R) for i in range(2)]
    stat = k.sb("stat", [128, 16], F32, R)
    wbf = [k.sb(f"wbf{i}", [128, 16, 128], BF16, R) for i in range(4)]
    sqb = [k.sb(f"sqb{i}", [128, 512], F32, R) for i in range(2)]
    rsb = [k.sb(f"rsb{i}", [128, 512], F32, R) for i in range(2)]
    pc_ = {"w": 0, "p": 0, "x": 0}

    def norm_tile(ps_, tt):
        hnT = hnT2[ps_ % 2]
        hk = ("hnT", ps_ % 2)
        b = pc_["x"] % 2
        pc_["x"] += 1
        xi = pc_["x"] % 16
        r0 = ps_ * 512 + tt * 128
        sk = ("stat", xi)
        sc = stat[:, xi: xi + 1]
        k.dma(xbuf[b][:], x_all[r0:r0 + 128, :], writes=[("xb", b)], chan=f"x{b}")
        k.op("act", lambda e: e.activation(out=hnb[b][:], in_=xbuf[b][:], func=AF.Square, accum_out=sc), [("xb", b)], [("hnb", b), sk])
        k.op("dve", lambda e: e.tensor_scalar(out=sc, in0=sc, scalar1=1.0 / D, scalar2=EPS, op0=ALU.mult, op1=ALU.add), [sk], [sk])
        k.op("act", lambda e: e.activation(out=sc, in_=sc, func=AF.Sqrt), [sk], [sk])
        k.op("dve", lambda e: e.reciprocal(out=sc, in_=sc), [sk], [sk])
        k.op("dve", lambda e: e.tensor_scalar(out=hnb[b][:], in0=xbuf[b][:], scalar1=sc, scalar2=None, op0=ALU.mult), [("xb", b), sk], [("hnb", b)])
        for hf in range(2):
            bk = hf
            pv = banks[bk][:].bitcast(BF16)
            for j in range(8):
                kc = hf * 8 + j
                k.op("pe", lambda e, kc=kc, j=j, pv=pv: e.transpose(out=pv[:, j * 128:(j + 1) * 128], in_=hnb[b][:, kc * 128:(kc + 1) * 128], identity=identb[:]),
                     [("hnb", b), "identb"], [PS(bk)] if j == 0 else [], wr_late=[PS(bk)] if j else [])
            for j in range(8):
                kc = hf * 8 + j
                k.op("act", lambda e, kc=kc, j=j, pv=pv: e.activation(out=hnT[:, kc, tt * 128:(tt + 1) * 128], in_=pv[:, j * 128:(j + 1) * 128],
                                                                 func=AF.Copy, scale=gmix_sb[:, kc:kc + 1]),
                     [PS(bk), "gmix"], [hk])

    def proj_chunk(ps_, fc):
        hnT = hnT2[ps_ % 2]
        hk = ("hnT", ps_ % 2)
        tok0 = ps_ * 512
        wb = pc_["w"] % 4
        pc_["w"] += 1
        k.dma(wbf[wb][:], w_in_t[fc], writes=[("wbf", wb)], chan=f"w{wb}", eng="pool")
        if fc < 16 or fc >= 24:
            bk = 2 + pc_["p"] % 2
            pb = pc_["p"] % 2
            pc_["p"] += 1
            for kc in range(16):
                k.op("pe", lambda e, kc=kc: e.matmul(out=banks[bk][:], lhsT=wbf[wb][:, kc, :], rhs=hnT[:, kc, :], start=(kc == 0), stop=(kc == 15)),
                     [("wbf", wb), hk], [PS(bk)] if kc == 0 else [], wr_late=[PS(bk)] if kc else [])
            if fc >= 24:
                dst = uT[:, fc - 24, tok0: tok0 + 512]
                k.op("act", lambda e: e.activation(out=dst, in_=banks[bk][:], func=AF.Copy), [PS(bk)], ["uT"])
                return
            sb_ = 4 + pb
            k.op("act", lambda e: e.activation(out=sqb[pb][:], in_=banks[bk][:], func=AF.Square), [PS(bk)], [("sqb", pb)])
            k.op("pe", lambda e: e.matmul(out=banks[sb_][:], lhsT=blk64, rhs=sqb[pb][:], start=True, stop=True), [("sqb", pb), "cm"], [PS(sb_)])
            k.op("dve", lambda e: e.tensor_scalar(out=rsb[pb][:], in0=banks[sb_][:], scalar1=1.0 / 64, scalar2=EPS, op0=ALU.mult, op1=ALU.add), [PS(sb_)], [("rsb", pb)])
            k.op("act", lambda e: e.activation(out=rsb[pb][:], in_=rsb[pb][:], func=AF.Sqrt), [("rsb", pb)], [("rsb", pb)])
            k.op("dve", lambda e: e.reciprocal(out=rsb[pb][:], in_=rsb[pb][:]), [("rsb", pb)], [("rsb", pb)])
            if fc < 8:
                dst = qT[:, fc, tok0 - 1024: tok0 - 512]
                g = gqk_sb[:, 0:1]
                dk = "qT"
            else:
                dst = kT[:, fc - 8, tok0: tok0 + 512]
                g = gqk_sb[:, 1:2]
                dk = "kT"
            k.op("dve", lambda e: e.scalar_tensor_tensor(out=dst, in0=banks[bk][:], scalar=g, in1=rsb[pb][:], op0=ALU.mult, op1=ALU.mult),
                 [PS(bk), ("rsb", pb), "gqk"], [dk])
        else:
            for tt in range(4):
                bk = 6 + tt % 2
                for kc in range(16):
                    k.op("pe", lambda e, kc=kc, tt=tt, bk=bk: e.matmul(out=banks[bk][:, 0:128], lhsT=hnT[:, kc, tt * 128:(tt + 1) * 128], rhs=wbf[wb][:, kc, :], start=(kc == 0), stop=(kc == 15)),
                         [("wbf", wb), hk], [PS(bk)] if kc == 0 else [], wr_late=[PS(bk)] if kc else [])
                dst = Vt[:, ps_ * 4 + tt, (fc - 16) * 128:(fc - 15) * 128]
                k.op("act", lambda e, dst=dst, bk=bk: e.activation(out=dst, in_=banks[bk][:, 0:128], func=AF.Copy), [PS(bk)], ["Vt"])

    for tt in range(4):
        norm_tile(0, tt)
    for ps_ in range(4):
        fcs = list(range(32) if ps_ >= 2 else range(8, 32))
        step = len(fcs) // 4
        for idx, fc in enumerate(fcs):
            proj_chunk(ps_, fc)
            if ps_ + 1 < 4 and idx % step == step - 1:
                norm_tile(ps_ + 1, idx // step)
    k.flush()
    if dbg and stop == "proj":
        dbg_dump("qT", qT[:], [128, 8, 1024], "qT", BF16)
        dbg_dump("kT", kT[:], [128, 8, 2048], "kT", BF16)
        dbg_dump("Vt", Vt[:], [128, 16, 1024], "Vt", BF16)
        dbg_dump("uT", uT[:], [128, 8, 2048], "uT", BF16)
    if stop == "proj":
        k.flush()
        return nc

    R = Region(120 * KB_, 207 * KB_)
    attnT = k.sb("attnT", [128, 8, 1024], F32, R)
    relb_sb = k.sb("relb_sb", [32, 16], F32, R)
    ohm_sb = k.sb("ohm_sb", [32, GW], F32, R)
    gs_sb = k.sb("gs_sb", [16, GW], BF16, R)
    Bt = [k.sb(f"Bt{i}", [128, 2048], BF16, R) for i in range(2)]
    Tt = [k.sb(f"Tt{i}", [128, 2048], BF16, R) for i in range(2)]
    pw = [k.sb(f"pw{i}", [128, 512], BF16, R) for i in range(3)]
    pw2 = [k.sb(f"pw2{i}", [128, 512], BF16, R) for i in range(3)]
    kvones = k.sb("kvones", [128, 16, 64], BF16, R)
    rden = k.sb("rden", [128, 1024], F32, R)
    k.dma(relb_sb[:], relb[:, :], writes=["relb"], chan="a0")
    k.dma(ohm_sb[:], ohm[:, :], writes=["ohm"], chan="a1")
    k.op("act", lambda e: e.activation(out=relb_sb[:], in_=relb_sb[:], func=AF.Exp), ["relb"], ["relb"])
    for j in range(GW // 512):
        k.op("pe", lambda e, j=j: e.matmul(out=banks[6][0:16, :], lhsT=relb_sb[:, :], rhs=ohm_sb[:, j * 512:(j + 1) * 512], start=True, stop=True),
             ["relb", "ohm"], [PS(6)])
        k.op("act", lambda e, j=j: e.activation(out=gs_sb[:, j * 512:(j + 1) * 512], in_=banks[6][0:16, :], func=AF.Copy), [PS(6)], ["gs_sb"])
    k.dma(gsc[:, :], gs_sb[:], reads=["gs_sb"], writes=["gsc"], chan="a2")
    k.op("dve", lambda e: e.tensor_copy(out=kvones[:], in_=kv_sb[:].unsqueeze(2).to_broadcast([128, 16, 64])), ["kv"], ["kvones"])
    def head_pieces(h):
        out = []
        for kt in range(16):
            qlo = max(0, (kt - 8) * 128)
            for half in range(2):
                lo = max(qlo, half * 512)
                hi = (half + 1) * 512
                if lo < hi:
                    out.append((h, kt, half, lo, hi))
        return out

    SB3 = (0, 1, 7)

    def build_table(h):
        tb = h % 2
        k.dma(Bt[tb][:], bass.AP(gsc.tensor, h * GW, [[1, 128], [1, 2048]]), reads=["gsc"], writes=[("Bt", tb)], chan=f"bt{tb}")
        for j in range(4):
            k.op("pe", lambda e, tb=tb, j=j: e.matmul(out=banks[6][:], lhsT=antib[:], rhs=Bt[tb][:, j * 512:(j + 1) * 512], start=True, stop=True),
                 [("Bt", tb), "antib"], [PS(6)])
            k.op("act", lambda e, tb=tb, j=j: e.activation(out=Tt[tb][:, j * 512:(j + 1) * 512], in_=banks[6][:], func=AF.Copy), [PS(6)], [("Tt", tb)])

    def s_mm(idx, pc):
        h, kt, half, lo, hi = pc
        hp, hh = h // 2, h % 2
        pl = slice(64 * hh, 64 * hh + 64)
        n = hi - lo
        sbk = SB3[idx % 3]
        k.op("pe", lambda e, pl=pl, hp=hp, kt=kt, lo=lo, hi=hi, n=n, sbk=sbk: e.matmul(out=banks[sbk][:, 0:n], lhsT=kT[pl, hp, kt * 128:(kt + 1) * 128], rhs=qT[pl, hp, lo:hi], start=True, stop=True),
             ["kT", "qT"], [PS(sbk)])

    def rest(idx, pc):
        h, kt, half, lo, hi = pc
        hp, hh = h // 2, h % 2
        tb = h % 2
        pl = slice(64 * hh, 64 * hh + 64)
        n = hi - lo
        c0 = 1024 + lo - kt * 128
        sbk = SB3[idx % 3]
        bi = idx % 3
        k.op("act", lambda e, sbk=sbk, n=n, bi=bi: e.activation(out=pw[bi][:, 0:n], in_=banks[sbk][:, 0:n], func=AF.Exp, scale=0.125), [PS(sbk)], [("pw", bi)])
        k.op("dve", lambda e, bi=bi, n=n, tb=tb, c0=c0: e.tensor_tensor(out=pw2[bi][:, 0:n], in0=pw[bi][:, 0:n], in1=Tt[tb][:, c0:c0 + n], op=ALU.mult),
             [("pw", bi), ("Tt", tb)], [("pw2", bi)])
        first = (kt == 0)
        last = (kt == (11 if half == 0 else 15))
        nb, db = 2 + half, 4 + half
        oc = slice(lo - half * 512, hi - half * 512)
        k.op("pe", lambda e, pl=pl, kt=kt, h=h, bi=bi, n=n, nb=nb, oc=oc, first=first, last=last: e.matmul(out=banks[nb][pl, oc], lhsT=Vt[:, kt, h * 64:(h + 1) * 64], rhs=pw2[bi][:, 0:n], start=first, stop=last),
             [("pw2", bi), "Vt"], [PS(nb)] if (first and hh == 0) else [], wr_late=[] if (first and hh == 0) else [PS(nb)])
        k.op("pe", lambda e, pl=pl, kt=kt, bi=bi, n=n, db=db, oc=oc, first=first, last=last: e.matmul(out=banks[db][pl, oc], lhsT=kvones[:, kt, :], rhs=pw2[bi][:, 0:n], start=first, stop=last),
             [("pw2", bi), "kvones"], [PS(db)] if (first and hh == 0) else [], wr_late=[] if (first and hh == 0) else [PS(db)])

    build_table(0)
    gidx = 0
    for hp in range(8):
        pcs = head_pieces(2 * hp) + head_pieces(2 * hp + 1)
        nh0 = len(head_pieces(2 * hp))
        base = gidx
        for q in range(min(2, len(pcs))):
            s_mm(base + q, pcs[q])
        for q, pc in enumerate(pcs):
            if q == 0:
                build_table(2 * hp + 1)
            if q == nh0 and hp < 7:
                build_table(2 * hp + 2)
            if q + 2 < len(pcs):
                s_mm(base + q + 2, pcs[q + 2])
            rest(base + q, pc)
        gidx += len(pcs)
        for half in range(2):
            hs = slice(half * 512, (half + 1) * 512)
            k.op("dve", lambda e, half=half, hs=hs: e.reciprocal(out=rden[:, hs], in_=banks[4 + half][:]), [PS(4 + half)], [("rden", half)])
            k.op("dve", lambda e, half=half, hs=hs, hp=hp: e.tensor_tensor(out=attnT[:, hp, hs], in0=banks[2 + half][:], in1=rden[:, hs], op=ALU.mult),
                 [PS(2 + half), ("rden", half)], ["attnT"])
    k.flush()
    if dbg and stop == "attn":
        dbg_dump("attnT", attnT[:], [128, 8, 1024], "attnT", F32)
    if stop == "attn":
        k.flush()
        return nc

    RX = Region(8 * KB_, 40 * KB_)
    mixedT = k.sb("mixedT", [128, 16, 1024], BF16, RX)
    RN = Region(152 * KB_, 168 * KB_)
    sqa = [k.sb(f"sqa{i}", [128, 1024], F32, RN) for i in range(2)]
    rstd_a = k.sb("rstd_a", [128, 1024], F32, RN)

    def out_norm(srcT, row0, keyfn):
        for c8 in range(8):
            sq = sqa[c8 % 2]
            k.op("act", lambda e, c8=c8, sq=sq: e.activation(out=sq[:], in_=srcT[:, c8, :], func=AF.Square), [keyfn(c8)], [("sqa", c8 % 2)])
            for half in range(2):
                k.op("pe", lambda e, c8=c8, sq=sq, half=half: e.matmul(out=banks[6 + half][:], lhsT=onesf, rhs=sq[:, half * 512:(half + 1) * 512], start=(c8 == 0), stop=(c8 == 7)),
                     [("sqa", c8 % 2), "cm"], [PS(6 + half)] if c8 == 0 else [], wr_late=[PS(6 + half)] if c8 else [])
        for half in range(2):
            hs = slice(half * 512, (half + 1) * 512)
            k.op("dve", lambda e, half=half, hs=hs: e.tensor_scalar(out=rstd_a[:, hs], in0=banks[6 + half][:], scalar1=1.0 / 1024, scalar2=EPS, op0=ALU.mult, op1=ALU.add),
                 [PS(6 + half)], [("rstd_a", half)])
            k.op("act", lambda e, hs=hs: e.activation(out=rstd_a[:, hs], in_=rstd_a[:, hs], func=AF.Sqrt), [("rstd_a", half)], [("rstd_a", half)])
            k.op("dve", lambda e, hs=hs: e.reciprocal(out=rstd_a[:, hs], in_=rstd_a[:, hs]), [("rstd_a", half)], [("rstd_a", half)])
        for c8 in range(8):
            k.op("dve", lambda e, c8=c8: e.scalar_tensor_tensor(out=mixedT[:, row0 + c8, :], in0=srcT[:, c8, :], scalar=gmixed_sb[:, row0 + c8:row0 + c8 + 1], in1=rstd_a[:],
                                                               op0=ALU.mult, op1=ALU.mult),
                 [keyfn(c8), ("rstd_a", 0), ("rstd_a", 1), "gmixed"], ["mixedT"])

    out_norm(attnT, 0, lambda c8: "attnT")
    k.flush()

    lam_l = din("lam_l", [3, 128, 32])
    bc_l = din("bc_l", [4, 128, 32, 16])
    dg_l = din("dg_l", [128, 16])
    glu_w = din("glu_w", [1024, 1024])
    tidx = din("tidx", [128, 512])
    RA = Region(40 * KB_, 88 * KB_)
    RB = Region(120 * KB_, 207 * KB_)
    BT = k.sb("BT", [128, 8, 2, 128], BF16, RA)
    Cblk = k.sb("Cblk", [128, 32, 2, 64], BF16, RA)
    rho = k.sb("rho", [128, 32], F32, RA)
    uturn = k.sb("uturn", [128, 32], F32, RA)
    c512 = k.sb("c512", [128, 32], F32, RA)
    s512 = k.sb("s512", [128, 32], F32, RA)
    tix = k.sb("tix", [128, 512], F32, RC)
    RA2 = Region(24 * KB_, 40 * KB_)
    BT3 = k.sb("BT3", [128, 8, 2, 128], BF16, RA)
    dg = k.sb("dg", [128, 16], F32, RA)
    k.dma(tix[:], tidx[:, :], writes=["tix"], chan="s0")
    k.dma(dg[:], dg_l[:, :], writes=["dg"], chan="s1")
    RP = Region(120 * KB_, 207 * KB_)
    lam = k.sb("lam", [128, 3, 32], F32, RP)
    bc = k.sb("bc", [128, 4, 32, 16], F32, RP)
    k.dma(lam[:], lam_l.rearrange("c p j -> p c j"), writes=["lam"], chan="s2")
    k.dma(bc[:], bc_l.rearrange("c p j x -> p c j x"), writes=["bc"], chan="s3")
    P_ = {}
    for nm in ["dt", "th", "v", "vf", "sn", "cs", "are", "aim", "den", "fre", "fim", "t1", "t2"]:
        P_[nm] = k.sb("p_" + nm, [128, 32], F32, RP)
    pvi = k.sb("p_vi", [128, 32], I32, RP)
    lr, li, ldt = lam[:, 0, :], lam[:, 1, :], lam[:, 2, :]
    TWO_PI = 6.2831845
    PI_ = 3.1415920

    def dv(fn, reads, writes):
        k.op("dve", fn, reads, writes)

    def ac(fn, reads, writes):
        k.op("act", fn, reads, writes)

    tt_ = lambda o, a, b, op: (lambda e: e.tensor_tensor(out=o, in0=a, in1=b, op=op))
    ac(lambda e: e.activation(out=P_["dt"][:], in_=ldt, func=AF.Exp), ["lam"], ["p_dt"])
    dv(tt_(P_["th"][:], li, P_["dt"][:], ALU.mult), ["lam", "p_dt"], ["p_th"])
    dv(tt_(P_["t1"][:], lr, P_["dt"][:], ALU.mult), ["lam", "p_dt"], ["p_t1"])
    ac(lambda e: e.activation(out=rho[:], in_=P_["t1"][:], func=AF.Exp), ["p_t1"], ["rho"])
    dv(lambda e: e.tensor_scalar(out=uturn[:], in0=P_["th"][:], scalar1=1.0 / (2 * math.pi), scalar2=None, op0=ALU.mult), ["p_th"], ["uturn"])
    for off, dst in ((0.0, "sn"), (0.25, "cs")):
        dv(lambda e, off=off: e.tensor_scalar(out=P_["v"][:], in0=uturn[:], scalar1=off, scalar2=None, op0=ALU.add), ["uturn"], ["p_v"])
        dv(lambda e: e.tensor_copy(out=pvi[:], in_=P_["v"][:]), ["p_v"], ["p_vi"])
        dv(tt_(P_["vf"][:], P_["v"][:], pvi[:], ALU.subtract), ["p_v", "p_vi"], ["p_vf"])
        ac(lambda e, dst=dst: e.activation(out=P_[dst][:], in_=P_["vf"][:], func=AF.Sin, scale=TWO_PI), ["p_vf"], ["p_" + dst])
    dv(lambda e: e.tensor_scalar(out=P_["v"][:], in0=uturn[:], scalar1=512.0, scalar2=None, op0=ALU.mult), ["uturn"], ["p_v"])
    dv(lambda e: e.tensor_copy(out=pvi[:], in_=P_["v"][:]), ["p_v"], ["p_vi"])
    dv(tt_(P_["vf"][:], P_["v"][:], pvi[:], ALU.subtract), ["p_v", "p_vi"], ["p_vf"])
    ac(lambda e: e.activation(out=s512[:], in_=P_["vf"][:], func=AF.Sin, scale=TWO_PI), ["p_vf"], ["s512"])
    ac(lambda e: e.activation(out=P_["t1"][:], in_=P_["vf"][:], func=AF.Sin, scale=PI_), ["p_vf"], ["p_t1"])
    ac(lambda e: e.activation(out=P_["t1"][:], in_=P_["t1"][:], func=AF.Square), ["p_t1"], ["p_t1"])
    dv(lambda e: e.tensor_scalar(out=c512[:], in0=P_["t1"][:], scalar1=-2.0, scalar2=1.0, op0=ALU.mult, op1=ALU.add), ["p_t1"], ["c512"])
    dv(tt_(P_["are"][:], rho[:], P_["cs"][:], ALU.mult), ["rho", "p_cs"], ["p_are"])
    dv(tt_(P_["aim"][:], rho[:], P_["sn"][:], ALU.mult), ["rho", "p_sn"], ["p_aim"])
    dv(tt_(P_["t1"][:], lr, lr, ALU.mult), ["lam"], ["p_t1"])
    dv(tt_(P_["t2"][:], li, li, ALU.mult), ["lam"], ["p_t2"])
    dv(tt_(P_["den"][:], P_["t1"][:], P_["t2"][:], ALU.add), ["p_t1", "p_t2"], ["p_den"])
    dv(lambda e: e.reciprocal(out=P_["den"][:], in_=P_["den"][:]), ["p_den"], ["p_den"])
    dv(lambda e: e.tensor_scalar(out=P_["are"][:], in0=P_["are"][:], scalar1=-1.0, scalar2=None, op0=ALU.add), ["p_are"], ["p_are"])
    dv(tt_(P_["t1"][:], P_["are"][:], lr, ALU.mult), ["p_are", "lam"], ["p_t1"])
    dv(tt_(P_["t2"][:], P_["aim"][:], li, ALU.mult), ["p_aim", "lam"], ["p_t2"])
    dv(tt_(P_["fre"][:], P_["t1"][:], P_["t2"][:], ALU.add), ["p_t1", "p_t2"], ["p_fre"])
    dv(tt_(P_["fre"][:], P_["fre"][:], P_["den"][:], ALU.mult), ["p_fre", "p_den"], ["p_fre"])
    dv(tt_(P_["t1"][:], P_["aim"][:], lr, ALU.mult), ["p_aim", "lam"], ["p_t1"])
    dv(tt_(P_["t2"][:], P_["are"][:], li, ALU.mult), ["p_are", "lam"], ["p_t2"])
    dv(tt_(P_["fim"][:], P_["t1"][:], P_["t2"][:], ALU.subtract), ["p_t1", "p_t2"], ["p_fim"])
    dv(tt_(P_["fim"][:], P_["fim"][:], P_["den"][:], ALU.mult), ["p_fim", "p_den"], ["p_fim"])
    bb = [k.sb(f"bb{i}", [128, 32, 16], F32, RP) for i in range(4)]
    fre_b = P_["fre"][:].unsqueeze(2).to_broadcast([128, 32, 16])
    fim_b = P_["fim"][:].unsqueeze(2).to_broadcast([128, 32, 16])
    dv(tt_(bb[0][:], bc[:, 0], fre_b, ALU.mult), ["bc", "p_fre"], ["bb0"])
    dv(tt_(bb[1][:], bc[:, 1], fim_b, ALU.mult), ["bc", "p_fim"], ["bb1"])
    dv(tt_(bb[0][:], bb[0][:], bb[1][:], ALU.subtract), ["bb0", "bb1"], ["bb0"])
    dv(tt_(bb[2][:], bc[:, 1], fre_b, ALU.mult), ["bc", "p_fre"], ["bb2"])
    dv(tt_(bb[3][:], bc[:, 0], fim_b, ALU.mult), ["bc", "p_fim"], ["bb3"])
    dv(tt_(bb[2][:], bb[2][:], bb[3][:], ALU.add), ["bb2", "bb3"], ["bb2"])
    Bblk = k.sb("Bblk", [128, 8, 2, 4, 32], F32, RP)
    Cf = k.sb("Cf", [128, 32, 2, 64], F32, RP)
    Bblk3 = k.sb("Bblk3", [128, 8, 2, 4, 32], F32, RP)
    dv(lambda e: e.memset(Bblk3[:], 0.0), [], ["Bblk3"])
    dv(lambda e: e.memset(Bblk[:], 0.0), [], ["Bblk"])
    dv(lambda e: e.memset(Cf[:], 0.0), [], ["Cf"])
    dv(lambda e: e.tensor_scalar(out=bc[:, 3], in0=bc[:, 3], scalar1=-1.0, scalar2=None, op0=ALU.mult), ["bc"], ["bc"])
    for gl in range(2):
        ls = slice(64 * gl, 64 * gl + 64)
        cs_ = slice(16 * gl, 16 * gl + 16)
        for ri, src in ((0, bb[0]), (1, bb[2])):
            dv(lambda e, ls=ls, cs_=cs_, ri=ri, src=src: e.tensor_copy(out=Bblk[ls, :, ri, :, cs_], in_=src[ls].rearrange("p (i j) c -> p i j c", j=4)),
               ["bb0", "bb2", "Bblk"], ["Bblk"])
        for ri in range(2):
            cv = Cf[ls, :, ri, :].rearrange("p (i j) c -> p i j c", j=4)
            sv = bc[ls, 2 + ri].rearrange("p (i j) c -> p i j c", j=4)
            dv(lambda e, cv=cv, sv=sv, gl=gl: e.tensor_copy(out=cv[:, :, 0:3, 16 * gl:16 * gl + 16], in_=sv[:, :, 0:3, :]), ["bc", "Cf"], ["Cf"])
            dv(lambda e, cv=cv, sv=sv, gl=gl: e.tensor_copy(out=cv[:, :, 3:4, 32 + 16 * gl:48 + 16 * gl], in_=sv[:, :, 3:4, :]), ["bc", "Cf"], ["Cf"])
    dv(lambda e: e.tensor_copy(out=Cblk[:], in_=Cf[:]), ["Cf"], ["Cblk"])
    for i in range(8):
        for ri in range(2):
            bk = 6 + (2 * i + ri) % 2
            k.op("pe", lambda e, i=i, ri=ri, bk=bk: e.transpose(out=banks[bk][:, 0:128], in_=Bblk[:, i, ri].rearrange("p j c -> p (j c)"), identity=cm[:, 0, :]),
                 ["Bblk", "cm"], [PS(bk)])
            ac(lambda e, i=i, ri=ri, bk=bk: e.activation(out=BT[:, i, ri, :], in_=banks[bk][:, 0:128], func=AF.Copy), [PS(bk)], ["BT"])
    dv(lambda e: e.tensor_copy(out=Bblk3[:, :, :, 3, :], in_=Bblk[:, :, :, 3, :]), ["Bblk", "Bblk3"], ["Bblk3"])
    for i in range(8):
        for ri in range(2):
            bk = 6 + (2 * i + ri) % 2
            k.op("pe", lambda e, i=i, ri=ri, bk=bk: e.transpose(out=banks[bk][:, 0:128], in_=Bblk3[:, i, ri].rearrange("p j c -> p (j c)"), identity=cm[:, 0, :]),
                 ["Bblk3", "cm"], [PS(bk)])
            ac(lambda e, i=i, ri=ri, bk=bk: e.activation(out=BT3[:, i, ri, :], in_=banks[bk][:, 0:128], func=AF.Copy), [PS(bk)], ["BT3"])
    k.flush()
    if dbg and stop == "ssmprep":
        dbg_dump("BT", BT[:], [128, 8, 2, 128], "BT", BF16)
        dbg_dump("Cblk", Cblk[:], [128, 32, 2, 32], "Cblk", BF16)
        dbg_dump("rho", rho[:], [128, 32], "rho")
        dbg_dump("uturn", uturn[:], [128, 32], "uturn")
        k.flush()
        return nc

    ygf = k.sb("ygf", [128, 8, 1024], F32, RB)
    ygb = k.sb("ygb", [128, 8, 1024], BF16, RB)
    gw = k.sb("gw", [128, 8, 1024], BF16, RB)
    sqa = [k.sb(f"sqb_{i}", [128, 1024], F32, RB) for i in range(2)]
    rstd_a = k.sb("rstd_b", [128, 1024], F32, RB)
    ytmp = k.sb("ytmp", [128, 512], F32, RB)
    sg = k.sb("sg", [128, 512], F32, RB)
    for kc in range(8):
        k.dma(gw[:, kc, :], glu_w[kc * 128:(kc + 1) * 128, :], writes=["gw"], chan="gw0", eng="pool")
    vb = k.sb("vb", [128, 512], F32, RA)
    vib = k.sb("vib", [128, 512], I32, RA)
    rb_ = k.sb("rb_", [128, 512], F32, RA)
    snb = [k.sb(f"snb{i}", [128, 512], F32, RA) for i in range(2)]
    csb = [k.sb(f"csb{i}", [128, 512], F32, RA) for i in range(2)]
    tb1 = [k.sb(f"tb_{i}", [128, 512], F32, RA) for i in range(4)]
    tbs = [tb1, tb1]
    negI = k.sb("negI", [128, 128], F32, RA)
    wsb = [[k.sb(f"wsb{p_}_{i}", [128, 512], F32, RA) for i in range(2)] for p_ in range(2)]
    dv(lambda e: e.tensor_scalar(out=negI[:], in0=cm[:, 0, :], scalar1=-1.0, scalar2=None, op0=ALU.mult), ["cm"], ["negI"])
    cin = [k.sb(f"cin{i}", [128, 4], F32, RA) for i in range(2)]
    zre = [k.sb(f"zre{i}", [128, 512], F32, RA2) for i in range(2)]
    zim = [k.sb(f"zim{i}", [128, 512], F32, RA2) for i in range(2)]
    xq = [[k.sb(f"xq{p_}_{i}", [128, 512], BF16, RA2) for i in range(4)] for p_ in range(2)]
    def cpar(c):
        jp, tc = divmod(c, 4)
        i, j = divmod(jp, 4)
        return jp, tc, i, j, jp % 2, c % 2

    def emit_tables(jp):
        pp = jp % 2
        ac(lambda e, jp=jp: e.activation(out=vb[:], in_=tix[:], func=AF.Copy, scale=uturn[:, jp:jp + 1]), ["tix", "uturn"], ["vb"])
        dv(lambda e: e.tensor_copy(out=vib[:], in_=vb[:]), ["vb"], ["vib"])
        dv(tt_(rb_[:], vb[:], vib[:], ALU.subtract), ["vb", "vib"], ["rb_"])
        ac(lambda e, pp=pp: e.activation(out=snb[pp][:], in_=rb_[:], func=AF.Sin, scale=TWO_PI), ["rb_"], [("snb", pp)])
        ac(lambda e: e.activation(out=vb[:], in_=rb_[:], func=AF.Sin, scale=PI_), ["rb_"], ["vb"])
        ac(lambda e: e.activation(out=vb[:], in_=vb[:], func=AF.Square), ["vb"], ["vb"])
        dv(lambda e, pp=pp: e.tensor_scalar(out=csb[pp][:], in0=vb[:], scalar1=-2.0, scalar2=1.0, op0=ALU.mult, op1=ALU.add), ["vb"], [("csb", pp)])

    def emit_bu(c):
        jp, tc, i, j, pp, pb = cpar(c)
        prs = slice(32 * j, 32 * j + 32)
        ts_ = slice(tc * 512, (tc + 1) * 512)
        for ri, bnk in ((0, 0), (1, 1)):
            if j < 3:
                k.op("pe", lambda e, prs=prs, i=i, ts_=ts_, bnk=bnk, ri=ri: e.matmul(out=banks[bnk][:], lhsT=BT[prs, i, ri, :], rhs=uT[prs, i, ts_], start=True, stop=True),
                     ["BT", "uT"], [PS(bnk)])
            else:
                k.op("pe", lambda e, i=i, ts_=ts_, bnk=bnk, ri=ri: e.matmul(out=banks[bnk][:], lhsT=BT3[:, i, ri, :], rhs=uT[:, i, ts_], start=True, stop=True),
                     ["BT3", "uT"], [PS(bnk)])

    WB = ((2, 3), (6, 7))

    def demod_ops(c):
        jp, tc, i, j, pp, pb = cpar(c)
        T = tbs[pb]
        bre, bim = 0, 1
        TK = lambda q: ("tb", q)
        return [
            lambda: dv(tt_(T[0][:], banks[bre][:], csb[pp][:], ALU.mult), [PS(bre), ("csb", pp)], [TK(0)]),
            lambda: dv(tt_(T[1][:], banks[bim][:], snb[pp][:], ALU.mult), [PS(bim), ("snb", pp)], [TK(1)]),
            lambda: dv(tt_(T[2][:], banks[bim][:], csb[pp][:], ALU.mult), [PS(bim), ("csb", pp)], [TK(2)]),
            lambda: dv(tt_(T[3][:], banks[bre][:], snb[pp][:], ALU.mult), [PS(bre), ("snb", pp)], [TK(3)]),
        ]

    def pool_adds(c):
        jp, tc, i, j, pp, pb = cpar(c)
        T = tbs[pb]
        wr, wi_ = WB[pb]
        TK = lambda q: ("tb", q)
        k.op("pe", lambda e: e.matmul(out=banks[wr][:], lhsT=cm[:, 0, :], rhs=T[0][:], start=True, stop=False), ["cm", TK(0)], [PS(wr)])
        k.op("pe", lambda e: e.matmul(out=banks[wr][:], lhsT=cm[:, 0, :], rhs=T[1][:], start=False, stop=True), ["cm", TK(1)], [], wr_late=[PS(wr)])
        k.op("pe", lambda e: e.matmul(out=banks[wi_][:], lhsT=cm[:, 0, :], rhs=T[2][:], start=True, stop=False), ["cm", TK(2)], [PS(wi_)])
        k.op("pe", lambda e: e.matmul(out=banks[wi_][:], lhsT=negI[:], rhs=T[3][:], start=False, stop=True), ["negI", TK(3)], [], wr_late=[PS(wi_)])
        ac(lambda e: e.activation(out=wsb[pb][0][:], in_=banks[wr][:], func=AF.Copy), [PS(wr)], [("wsb", pb, 0)])
        ac(lambda e: e.activation(out=wsb[pb][1][:], in_=banks[wi_][:], func=AF.Copy), [PS(wi_)], [("wsb", pb, 1)])

    def carry_ops(c):
        jp, tc, i, j, pp, pb = cpar(c)
        if tc == 0:
            return []
        zp = 1 - pb
        c5 = c512[:, jp:jp + 1]
        s5 = s512[:, jp:jp + 1]
        zl_re = zre[zp][:, 511:512]
        zl_im = zim[zp][:, 511:512]
        ci = cin[pb]
        ck = ("cin", pb)
        return [
            lambda: dv(lambda e: e.tensor_tensor(out=ci[:, 0:1], in0=zl_im, in1=s5, op=ALU.mult), [("zim", zp), "s512"], [ck]),
            lambda: dv(lambda e: e.scalar_tensor_tensor(out=ci[:, 1:2], in0=zl_re, scalar=c5, in1=ci[:, 0:1], op0=ALU.mult, op1=ALU.subtract), [("zre", zp), "c512", ck], [ck]),
            lambda: dv(lambda e: e.tensor_tensor(out=ci[:, 2:3], in0=zl_im, in1=c5, op=ALU.mult), [("zim", zp), "c512", ck], [ck]),
            lambda: dv(lambda e: e.scalar_tensor_tensor(out=ci[:, 3:4], in0=zl_re, scalar=s5, in1=ci[:, 2:3], op0=ALU.mult, op1=ALU.add), [("zre", zp), "s512", ck], [ck]),
        ]

    def scans(c):
        jp, tc, i, j, pp, pb = cpar(c)
        wr, wi_ = WB[pb]
        rho_c = rho[:, jp:jp + 1]
        if tc == 0:
            ini_re, ini_im, ikeys = 0.0, 0.0, []
        else:
            ini_re, ini_im, ikeys = cin[pb][:, 1:2], cin[pb][:, 3:4], [("cin", pb)]
        dv(lambda e: e.tensor_tensor_scan(out=zre[pb][:], data0=rho_c.to_broadcast([128, 512]), data1=wsb[pb][0][:], initial=ini_re, op0=ALU.mult, op1=ALU.add),
           [("wsb", pb, 0), "rho"] + ikeys, [("zre", pb)])
        dv(lambda e: e.tensor_tensor_scan(out=zim[pb][:], data0=rho_c.to_broadcast([128, 512]), data1=wsb[pb][1][:], initial=ini_im, op0=ALU.mult, op1=ALU.add),
           [("wsb", pb, 1), "rho"] + ikeys, [("zim", pb)])

    def remod(c):
        jp, tc, i, j, pp, pb = cpar(c)
        X = xq[pb]
        XK = lambda q: ("xq", pb, q)
        prs = slice(32 * j, 32 * j + 32)
        dv(tt_(X[0][:], zre[pb][:], csb[pp][:], ALU.mult), [("zre", pb), ("csb", pp)], [XK(0)])
        dv(lambda e: e.scalar_tensor_tensor(out=X[1][:], in0=zim[pb][:], scalar=-1.0, in1=snb[pp][:], op0=ALU.mult, op1=ALU.mult), [("zim", pb), ("snb", pp)], [XK(1)])
        k.op("pool", lambda e: e.tensor_tensor(out=X[2][:], in0=zre[pb][:], in1=snb[pp][:], op=ALU.mult), [("zre", pb), ("snb", pp)], [XK(2)])
        k.op("pool", lambda e: e.tensor_tensor(out=X[3][:], in0=zim[pb][:], in1=csb[pp][:], op=ALU.mult), [("zim", pb), ("csb", pp)], [XK(3)])
        yb = 4 + (tc - 2)
        if j < 2:
            ops_, cw = prs, 32
        else:
            ops_, cw = slice(64, 128), 64
        for q, cv in ((0, 0), (1, 0), (2, 1), (3, 1)):
            first = (q == 0)
            last = (q == 3)
            k.op("pe", lambda e, q=q, cv=cv, first=first, last=last: e.matmul(out=banks[yb][ops_, :], lhsT=Cblk[:, jp, cv, 0:cw], rhs=X[q][:], start=(first and j != 3), stop=(last and j != 2)),
                 ["Cblk", XK(q)], [PS(yb)] if (first and j == 0) else [], wr_late=[] if (first and j == 0) else [PS(yb)])

    def evac_tile(i):
        for tc in (2, 3):
            yb = 4 + (tc - 2)
            os_ = slice((tc - 2) * 512, (tc - 1) * 512)
            dv(lambda e, i=i, tc=tc, yb=yb: e.scalar_tensor_tensor(out=ytmp[:], in0=uT[:, i, tc * 512:(tc + 1) * 512], scalar=dg[:, i:i + 1], in1=banks[yb][:], op0=ALU.mult, op1=ALU.add),
               ["uT", "dg", PS(yb)], ["ytmp"])
            ac(lambda e, i=i, os_=os_: e.activation(out=ygf[:, i, os_], in_=ytmp[:], func=AF.Gelu), ["ytmp"], [("ygf", i)])
            k.op("pool", lambda e, i=i, os_=os_: e.tensor_copy(out=ygb[:, i, os_], in_=ygf[:, i, os_]), [("ygf", i)], ["ygb"])

    NCH = 128
    emit_tables(0)
    emit_bu(0)
    for m_ in demod_ops(0):
        m_()
    pool_adds(0)
    for c in range(NCH):
        jp, tc, i, j, pp, pb = cpar(c)
        if tc == 1 and jp + 1 < 32:
            emit_tables(jp + 1)
        nxt = c + 1 < NCH
        if nxt:
            emit_bu(c + 1)
        C_ = carry_ops(c)
        M_ = demod_ops(c + 1) if nxt else []
        for q in range(4):
            if q < len(C_):
                C_[q]()
            if q < len(M_):
                M_[q]()
        if nxt:
            pool_adds(c + 1)
        scans(c)
        if tc >= 2:
            remod(c)
        if tc == 3 and j == 3:
            evac_tile(i)
    for fo in range(8):
        for half in range(2):
            bk = half
            hs = slice(half * 512, (half + 1) * 512)
            for kc in range(8):
                k.op("pe", lambda e, fo=fo, kc=kc, hs=hs, bk=bk: e.matmul(out=banks[bk][:], lhsT=gw[:, kc, fo * 128:(fo + 1) * 128], rhs=ygb[:, kc, hs], start=(kc == 0), stop=(kc == 7)),
                     ["gw", "ygb"], [PS(bk)] if kc == 0 else [], wr_late=[PS(bk)] if kc else [])
            ac(lambda e, fo=fo, bk=bk: e.activation(out=sg[:], in_=banks[bk][:], func=AF.Sigmoid, bias=dg[:, 8 + fo:9 + fo]), [PS(bk), "dg"], ["sg"])
            dv(lambda e, fo=fo, hs=hs: e.tensor_tensor(out=ygf[:, fo, hs], in0=ygf[:, fo, hs], in1=sg[:], op=ALU.mult), [("ygf", fo), "sg"], [("ygf", fo)])
    k.flush()
    if dbg and stop == "ssm":
        dbg_dump("ygf", ygf[:], [128, 8, 1024], ("ygf", 0), F32)
    out_norm(ygf, 8, lambda c8: ("ygf", c8))
    k.flush()
    if dbg and stop == "ssm":
        dbg_dump("mixedT", mixedT[:], [128, 16, 1024], "mixedT", BF16)
    if stop == "ssm":
        k.flush()
        return nc

    w_out = din("w_out", [2048, 2048])
    RXR = Region(40 * KB_, 104 * KB_)
    x_res = k.sb("x_res", [128, 8, D], F32, RXR)
    RW = Region(104 * KB_, 207 * KB_)
    wob = [k.sb(f"wob{i}", [128, 16, 512], BF16, RW) for i in range(2)]
    xin = [k.sb(f"xin{i}", [128, 512], F32, RW) for i in range(2)]
    xc = 0
    for dc in range(4):
        ds_ = slice(dc * 512, (dc + 1) * 512)
        for hf in range(2):
            k.dma(wob[dc % 2][:, hf * 8:(hf + 1) * 8, :], w_out[hf * 1024:(hf + 1) * 1024, ds_].rearrange("(kc p) d -> p kc d", p=128), writes=[("wob", dc % 2, hf)], chan=f"wo{dc % 2}", eng="pool")
        for tt in range(8):
            bk = tt % 2
            xb_ = xc % 2
            xc += 1
            k.dma(xin[xb_][:], x_all[1024 + tt * 128: 1024 + (tt + 1) * 128, ds_], writes=[("xin", xb_)], chan=f"xi{xb_}")
            for kc in range(16):
                k.op("pe", lambda e, kc=kc, tt=tt, bk=bk, dc=dc: e.matmul(out=banks[bk][:], lhsT=mixedT[:, kc, tt * 128:(tt + 1) * 128], rhs=wob[dc % 2][:, kc, :], start=(kc == 0), stop=(kc == 15)),
                     ["mixedT", ("wob", dc % 2, kc // 8)], [PS(bk)] if kc == 0 else [], wr_late=[PS(bk)] if kc else [])
            k.op("dve", lambda e, tt=tt, ds_=ds_, bk=bk, xb_=xb_: e.tensor_tensor(out=x_res[:, tt, ds_], in0=banks[bk][:], in1=xin[xb_][:], op=ALU.add),
                 [PS(bk), ("xin", xb_)], [("x_res", tt)])
    k.flush()
    if dbg and stop == "x1":
        dbg_dump("x1", x_res[:], [128, 8, D], ("x_res", 0), F32)
    if stop == "x1":
        k.flush()
        return nc

    gffn = din("gffn", [128, 16])
    wq_t = din("wq_t", [16, 128, 16, 128])
    keysT = din("keysT", [128, 16, 128])
    U_t = din("U_t", [128, 128, 16, 128])
    Vp = din("Vp", [16384, D])
    gffn_sb = k.sb("gffn_sb", [128, 16], F32, RC)
    k.dma(gffn_sb[:], gffn[:, :], writes=["gffn"], chan="c5")
    RS = Region(8 * KB_, 40 * KB_)
    s12 = k.sb("s12", [128, 4, 2, 8, 128], F32, RS)
    for hh_ in range(2):
        RP2 = Region(104 * KB_, 207 * KB_)
        hn2T = k.sb("hn2T", [128, 16, 512], BF16, RP2)
        Dm = k.sb("Dm", [128, 4, 8, 128], BF16, RP2)
        tstat = k.sb("tstat", [128, 4, 8, 4], F32, RP2)
        mark = RP2.top
        qpT = k.sb("qpT", [128, 16, 512], BF16, RP2)
        wbf2 = [k.sb(f"wbf2{i}", [128, 16, 128], BF16, RP2) for i in range(4)]
        hnb2 = k.sb("hnb2", [128, D], BF16, RP2)
        keysb = k.sb("keysb", [128, 16, 128], BF16, RP2)
        st2 = k.sb("st2", [128, 4], F32, RP2)
        m16 = [k.sb(f"m16_{q}", [128, 2, 16], F32, RP2) for q in range(4)]
        c24 = [k.sb(f"c24_{q}", [128, 24], F32, RP2) for q in range(4)]
        tmp1 = [k.sb(f"tmp1_{q}", [128, 128], F32, RP2) for q in range(4)]
        cand = [k.sb(f"cand_{q}", [128, 16, 16], F32, RP2) for q in range(4)]
        cand2 = [k.sb(f"cand2_{q}", [128, 256], F32, RP2) for q in range(4)]
        ejunk = [k.sb(f"ejunk_{q}", [128, 16], F32, RP2) for q in range(4)]
        k.dma(keysb[:], keysT[:, :, :], writes=["keysb"], chan="p0", eng="pool")
        for tt in range(4):
            tg = hh_ * 4 + tt
            sc = st2[:, tt:tt + 1]
            sk = ("st2", tt)
            k.op("act", lambda e, tg=tg, sc=sc: e.activation(out=hnb2[:], in_=x_res[:, tg, :], func=AF.Square, accum_out=sc), [("x_res", tg)], ["hnb2", sk])
            k.op("dve", lambda e, sc=sc: e.tensor_scalar(out=sc, in0=sc, scalar1=1.0 / D, scalar2=EPS, op0=ALU.mult, op1=ALU.add), [sk], [sk])
            k.op("act", lambda e, sc=sc: e.activation(out=sc, in_=sc, func=AF.Sqrt), [sk], [sk])
            k.op("dve", lambda e, sc=sc: e.reciprocal(out=sc, in_=sc), [sk], [sk])
            k.op("dve", lambda e, tg=tg, sc=sc: e.tensor_scalar(out=hnb2[:], in0=x_res[:, tg, :], scalar1=sc, scalar2=None, op0=ALU.mult), [("x_res", tg), sk], ["hnb2"])
            for hf in range(2):
                bk = hf
                pv = banks[bk][:].bitcast(BF16)
                for j in range(8):
                    kc = hf * 8 + j
                    k.op("pe", lambda e, kc=kc, j=j, pv=pv: e.transpose(out=pv[:, j * 128:(j + 1) * 128], in_=hnb2[:, kc * 128:(kc + 1) * 128], identity=identb[:]),
                         ["hnb2", "identb"], [PS(bk)] if j == 0 else [], wr_late=[PS(bk)] if j else [])
                for j in range(8):
                    kc = hf * 8 + j
                    k.op("act", lambda e, kc=kc, j=j, tt=tt, pv=pv: e.activation(out=hn2T[:, kc, tt * 128:(tt + 1) * 128], in_=pv[:, j * 128:(j + 1) * 128], func=AF.Copy, scale=gffn_sb[:, kc:kc + 1]),
                         [PS(bk), "gffn"], ["hn2T"])
        for fc in range(16):
            wb = fc % 4
            k.dma(wbf2[wb][:], wq_t[fc], writes=[("wbf2", wb)], chan=f"wq{wb}", eng="pool")
            bk = 2 + fc % 2
            for kc in range(16):
                k.op("pe", lambda e, wb=wb, kc=kc, bk=bk: e.matmul(out=banks[bk][:], lhsT=wbf2[wb][:, kc, :], rhs=hn2T[:, kc, :], start=(kc == 0), stop=(kc == 15)),
                     [("wbf2", wb), "hn2T"], [PS(bk)] if kc == 0 else [], wr_late=[PS(bk)] if kc else [])
            k.op("act", lambda e, fc=fc, bk=bk: e.activation(out=qpT[:, fc, :], in_=banks[bk][:], func=AF.Copy), [PS(bk)], ["qpT"])
        for tt in range(4):
            for half in range(2):
                for hq in range(2):
                    bk = 4 + (2 * half + hq) % 4
                    for h4 in range(4):
                        h = hq * 4 + h4
                        k.op("pe", lambda e, tt=tt, half=half, h=h, h4=h4, bk=bk: e.matmul(out=banks[bk][:, h4 * 128:(h4 + 1) * 128], lhsT=qpT[:, 2 * h + half, tt * 128:(tt + 1) * 128], rhs=keysb[:, half * 8 + h, :], start=True, stop=True),
                             ["qpT", "keysb"], [PS(bk)] if h4 == 0 else [], wr_late=[PS(bk)] if h4 else [])
                    k.op("act", lambda e, tt=tt, half=half, hq=hq, bk=bk: e.activation(out=s12[:, tt, half, hq * 4:(hq + 1) * 4, :], in_=banks[bk][:].rearrange("p (a b) -> p a b", a=4), func=AF.Copy),
                         [PS(bk)], [("s12", tt)])
            for hq in range(2):
                hs4 = [hq * 4 + q for q in range(4)]
                for half in range(2):
                    svs = [s12[:, tt, half, h, :] for h in hs4]
                    for q in range(4):
                        dv(lambda e, half=half, q=q, sv=svs[q]: e.max(out=m16[q][:, half, 0:8], in_=sv), [("s12", tt)], [("m16", q, half)])
                    for q in range(4):
                        dv(lambda e, half=half, q=q, sv=svs[q]: e.match_replace(out=tmp1[q][:], in_to_replace=m16[q][:, half, 0:8], in_values=sv, imm_value=-1e30),
                           [("s12", tt), ("m16", q, half)], [("tmp1", q)])
                    for q in range(4):
                        dv(lambda e, half=half, q=q: e.max(out=m16[q][:, half, 8:16], in_=tmp1[q][:]), [("tmp1", q)], [("m16", q, half)])
                for q in range(4):
                    dv(lambda e, q=q: e.tensor_tensor(out=cand[q][:], in0=m16[q][:, 0, :].unsqueeze(2).to_broadcast([128, 16, 16]), in1=m16[q][:, 1:2, :].to_broadcast([128, 16, 16]), op=ALU.add),
                       [("m16", q, 0), ("m16", q, 1)], [("cand", q)])
                for q in range(4):
                    dv(lambda e, q=q: e.max(out=c24[q][:, 0:8], in_=cand[q][:].rearrange("p a b -> p (a b)")), [("cand", q)], [("c24", q)])
                for q in range(4):
                    dv(lambda e, q=q: e.match_replace(out=cand2[q][:], in_to_replace=c24[q][:, 0:8], in_values=cand[q][:].rearrange("p a b -> p (a b)"), imm_value=-1e30), [("cand", q), ("c24", q)], [("cand2", q)])
                for q in range(4):
                    dv(lambda e, q=q: e.max(out=c24[q][:, 8:16], in_=cand2[q][:]), [("cand2", q)], [("c24", q)])
                for q in range(4):
                    dv(lambda e, q=q: e.match_replace(out=cand2[q][:], in_to_replace=c24[q][:, 8:16], in_values=cand2[q][:], imm_value=-1e30), [("cand2", q), ("c24", q)], [("cand2", q)])
                for q in range(4):
                    dv(lambda e, q=q: e.max(out=c24[q][:, 16:24], in_=cand2[q][:]), [("cand2", q)], [("c24", q)])
                for q, h in enumerate(hs4):
                    ts0 = tstat[:, tt, h, 0:1]
                    dv(lambda e, ts0=ts0, q=q: e.tensor_tensor(out=ts0, in0=c24[q][:, 15:16], in1=c24[q][:, 16:17], op=ALU.add), [("c24", q)], [("tstat", tt, h)])
                for q, h in enumerate(hs4):
                    ts0 = tstat[:, tt, h, 0:1]
                    dv(lambda e, ts0=ts0: e.tensor_scalar(out=ts0, in0=ts0, scalar1=0.5, scalar2=None, op0=ALU.mult), [("tstat", tt, h)], [("tstat", tt, h)])
                for q, h in enumerate(hs4):
                    ts1 = tstat[:, tt, h, 1:2]
                    ac(lambda e, ts1=ts1, q=q: e.activation(out=ejunk[q][:], in_=c24[q][:, 0:16], func=AF.Exp, accum_out=ts1), [("c24", q)], [("ejunk", q), ("tstat1", tt, h)])
                for q, h in enumerate(hs4):
                    ts0 = tstat[:, tt, h, 0:1]
                    ts3 = tstat[:, tt, h, 3:4]
                    ac(lambda e, ts0=ts0, ts3=ts3: e.activation(out=ts3, in_=ts0, func=AF.Exp), [("tstat", tt, h)], [("tstat3", tt, h)])
                for q, h in enumerate(hs4):
                    ts1 = tstat[:, tt, h, 1:2]
                    ts2 = tstat[:, tt, h, 2:3]
                    dv(lambda e, ts1=ts1, ts2=ts2: e.reciprocal(out=ts2, in_=ts1), [("tstat1", tt, h)], [("tstat2", tt, h)])
                for q, h in enumerate(hs4):
                    ts2 = tstat[:, tt, h, 2:3]
                    dv(lambda e, tt=tt, h=h, ts2=ts2: e.tensor_scalar(out=Dm[:, tt, h, :], in0=identb[:], scalar1=ts2, scalar2=None, op0=ALU.mult), ["identb", ("tstat2", tt, h)], ["Dm"])
        k.flush()
        if dbg and stop == "score" and hh_ == 0:
            dbg_dump("s12", s12[:], [128, 4, 2, 8, 128], ("s12", 0), F32)
            dbg_dump("tstat", tstat[:], [128, 4, 8, 4], ("tstat", 0, 0), F32)
            k.flush()
            return nc
        RP2.top = mark
        NPB = 6
        ub = [k.sb(f"ub{i}", [128, 16, 128], BF16, RP2) for i in range(2)]
        ga = [k.sb(f"ga{i}", [128, 4, 512], BF16, RP2) for i in range(2)]
        vbb = [k.sb(f"vbb{i}", [128, D], BF16, RP2) for i in range(8)]
        pexp = [k.sb(f"pexp{i}", [128, 4, 128], F32, RP2) for i in range(NPB)]
        gq = [k.sb(f"gq{i}", [128, 4, 128], BF16, RP2) for i in range(NPB)]
        gsum = [k.sb(f"gsum{i}", [128, 4, 128], BF16, RP2) for i in range(2)]
        wT = [k.sb(f"wT{i}", [128, 4, 128], BF16, RP2) for i in range(2)]
        a_st = k.sb("a_st", [128, 4, 512], F32, RP2)
        cnt = {"st": 0, "a": 0, "g": 0, "w": 0, "p": 0}

        def load_chunk(g, s_):
            e1 = g * 4 + s_
            sl = (g % 2) * 4 + s_
            k.dma(ub[s_ % 2][:], U_t[e1], writes=[("ub", s_ % 2)], chan=f"pu{s_ % 2}", eng="pool")
            k.dma(vbb[sl][:], Vp[e1 * 128:(e1 + 1) * 128, :], writes=[("vbb", sl)], chan=f"pv{sl}", eng="pool")

        def act_mm(g, s_, kcs, bk):
            for kc in kcs:
                k.op("pe", lambda e, s_=s_, kc=kc, bk=bk: e.matmul(out=banks[bk][:], lhsT=ub[s_ % 2][:, kc, :], rhs=hn2T[:, kc, :], start=(kc == 0), stop=(kc == 15)),
                     [("ub", s_ % 2), "hn2T"], [PS(bk)] if kc == 0 else [], wr_late=[PS(bk)] if kc else [])

        def act_gelu(g, s_, bk):
            par = g % 2
            ac(lambda e, s_=s_, bk=bk: e.activation(out=a_st[:, s_, :], in_=banks[bk][:], func=AF.Copy), [PS(bk)], [("a_st", s_)])
            if s_ == 3:
                ac(lambda e, par=par: e.activation(out=ga[par][:].rearrange("p a b -> p (a b)"), in_=a_st[:].rearrange("p a b -> p (a b)"), func=AF.Gelu),
                   [("a_st", q) for q in range(4)], [("ga", par, q) for q in range(4)])

        def act_chunk(g, s_):
            bk = cnt["a"] % 2
            cnt["a"] += 1
            act_mm(g, s_, range(16), bk)
            act_gelu(g, s_, bk)

        gb_ = 2
        pvT = banks[3][:].bitcast(BF16)

        def stage_a(g, tt, pre=None, adds=None):
            if pre is not None:
                bk = cnt["a"] % 2
                cnt["a"] += 1
            for h in range(8):
                pi = cnt["p"] % NPB
                cnt["p"] += 1
                for s_ in range(4):
                    e1 = g * 4 + s_
                    ac(lambda e, tt=tt, h=h, e1=e1, pi=pi, s_=s_: e.activation(out=pexp[pi][:, s_, :], in_=s12[:, tt, 1, h, :], func=AF.Exp, bias=s12[:, tt, 0, h, e1:e1 + 1]),
                       [("s12", tt)], [("pexp", pi, s_)])
                dv(lambda e, tt=tt, h=h, pi=pi: e.scalar_tensor_tensor(out=gq[pi][:], in0=pexp[pi][:], scalar=tstat[:, tt, h, 3:4], in1=pexp[pi][:], op0=ALU.is_ge, op1=ALU.mult),
                   [("tstat3", tt, h)] + [("pexp", pi, s_) for s_ in range(4)], [("gq", pi)])
                k.op("pe", lambda e, tt=tt, h=h, pi=pi: e.matmul(out=banks[gb_][:], lhsT=Dm[:, tt, h, :], rhs=gq[pi][:].rearrange("p a b -> p (a b)"), start=(h == 0), stop=(h == 7)),
                     [("gq", pi), "Dm"], [PS(gb_)] if h == 0 else [], wr_late=[PS(gb_)] if h else [])
                if pre is not None:
                    act_mm(pre[0], pre[1], (2 * h, 2 * h + 1), bk)
                if adds is not None and h >= 4:
                    stage_c(adds[0], adds[1], dcs=(h - 4,))
            if pre is not None:
                act_gelu(pre[0], pre[1], bk)

        def stage_b(g, tt):
            par = g % 2
            gi = cnt["g"] % 2
            cnt["g"] += 1
            ac(lambda e, gi=gi: e.activation(out=gsum[gi][:].rearrange("p a b -> p (a b)"), in_=banks[gb_][:], func=AF.Copy), [PS(gb_)], [("gsum", gi)])
            for s_ in range(4):
                k.op("pe", lambda e, gi=gi, s_=s_: e.transpose(out=pvT[:, s_ * 128:(s_ + 1) * 128], in_=gsum[gi][:, s_, :], identity=identb[:]),
                     [("gsum", gi), "identb"], [PS(3)] if s_ == 0 else [], wr_late=[PS(3)] if s_ else [])
            wi = cnt["w"] % 2
            cnt["w"] += 1
            dv(lambda e, par=par, tt=tt, wi=wi: e.tensor_tensor(out=wT[wi][:], in0=pvT[:, 0:512].rearrange("p (a b) -> p a b", a=4), in1=ga[par][:, :, tt * 128:(tt + 1) * 128], op=ALU.mult),
               [PS(3)] + [("ga", par, s_) for s_ in range(4)], [("wT", wi)])
            for s_ in range(4):
                sl = par * 4 + s_
                for dc in range(4):
                    k.op("pe", lambda e, sl=sl, s_=s_, dc=dc, wi=wi: e.matmul(out=banks[4 + dc][:], lhsT=wT[wi][:, s_, :], rhs=vbb[sl][:, dc * 512:(dc + 1) * 512], start=(s_ == 0), stop=(s_ == 3)),
                         [("wT", wi), ("vbb", sl)], [PS(4 + dc)] if s_ == 0 else [], wr_late=[PS(4 + dc)] if s_ else [])

        def stage_c(g, tt, dcs=range(4)):
            tg = hh_ * 4 + tt
            for dc in dcs:
                ds_ = slice(dc * 512, (dc + 1) * 512)
                dv(lambda e, tg=tg, dc=dc, ds_=ds_: e.tensor_tensor(out=x_res[:, tg, ds_], in0=banks[4 + dc][:], in1=x_res[:, tg, ds_], op=ALU.add),
                   [PS(4 + dc), ("x_res", tg)], [("x_res", tg)])

        for s_ in range(4):
            load_chunk(0, s_)
            act_chunk(0, s_)
        its = [(g, tt) for g in range(32) for tt in range(4)]
        stage_a(*its[0])
        for n_, (g, tt) in enumerate(its):
            pre = (g + 1, tt) if g + 1 < 32 else None
            if pre is not None:
                load_chunk(g + 1, tt)
            stage_b(g, tt)
            if n_ + 1 < len(its):
                stage_a(*its[n_ + 1], pre=pre, adds=(g, tt))
            else:
                stage_c(g, tt)
        k.flush()
    for tt in range(8):
        k.dma(y[tt * 128:(tt + 1) * 128, :], x_res[:, tt, :], reads=[("x_res", tt)], writes=[("y", tt)], chan="yo")
    k.flush()
    return nc


def _static_tables():
    i = np.arange(GW)
    d = i - 127
    n = np.maximum(d, 0)
    nf = np.maximum(n, 1).astype(np.float32)
    large = 16 + (np.log(nf / np.float32(16)) / np.float32(math.log(2048 / 16)) * np.float32(16)).astype(np.int32)
    large = np.minimum(large, 31)
    bucket = np.where(n < 16, n, large)
    mult = ((d <= 128).astype(np.float32) + ((d % 4 == 0) & (d <= 512)) + ((d % 16 == 0) & (d <= 2048)))
    mult = np.where((d >= 0) & (d <= 2047), mult, 0.0).astype(np.float32)
    ohm = np.zeros((32, GW), np.float32)
    ohm[bucket, i] = mult
    cm = np.zeros((4, 128, 128), np.float32)
    cm[0] = np.eye(128)
    cm[1] = np.eye(128)[::-1]
    cm[2, :64, :64] = 1.0
    cm[2, 64:, 64:] = 1.0
    cm[3] = 1.0
    return ohm, cm


def _pcol(v, n):
    return np.ascontiguousarray(np.asarray(v, np.float32).reshape(n, 128).T)


def _prep(inp, stage=None):
    f = lambda a: np.ascontiguousarray(np.asarray(a, dtype=np.float32))
    x = f(inp["x"])
    ohm, cm = _static_tables()
    w_in_t = np.ascontiguousarray(f(inp["w_in"]).reshape(16, 128, 32, 128).transpose(2, 1, 0, 3))
    gq = np.tile(f(inp["q_norm_g"]), 2)
    gk = np.tile(f(inp["k_norm_g"]), 2)
    shared = {
        "gmix": _pcol(inp["norm_mix_g"], 16),
        "w_in_t": w_in_t,
        "gqk": np.ascontiguousarray(np.stack([gq, gk], axis=1)),
        "relb": f(inp["rel_bias"]),
        "ohm": ohm,
        "cmats": cm,
        "gmixed": np.ascontiguousarray(np.concatenate([_pcol(inp["attn_out_g"], 8), _pcol(inp["ssm_out_g"], 8)], axis=1)),
    }
    def lane(a):
        a = f(a)
        rest = a.shape[2:]
        return np.ascontiguousarray(a.reshape((32, 2, 64) + rest).transpose((1, 2, 0) + tuple(range(3, 3 + len(rest)))).reshape((128, 32) + rest))
    ldt = np.broadcast_to(f(inp["ssm_log_dt"])[:, None], (64, 64))
    shared["lam_l"] = np.ascontiguousarray(np.stack([lane(inp["ssm_lambda_re"]), lane(inp["ssm_lambda_im"]), lane(ldt)]))
    shared["bc_l"] = np.ascontiguousarray(np.stack([lane(inp["ssm_b_re"]), lane(inp["ssm_b_im"]),
                                                    lane(f(inp["ssm_c_re"]).transpose(0, 2, 1)), lane(f(inp["ssm_c_im"]).transpose(0, 2, 1))]))
    shared["dg_l"] = np.ascontiguousarray(np.concatenate([_pcol(f(inp["ssm_d"]).reshape(-1), 8), _pcol(inp["ssm_glu_b"], 8)], axis=1))
    shared["glu_w"] = f(inp["ssm_glu_w"])
    shared["tidx"] = np.ascontiguousarray(np.broadcast_to(np.arange(512, dtype=np.float32)[None, :], (128, 512)))
    shared["w_out"] = f(inp["w_out"])
    shared["gffn"] = _pcol(inp["norm_ffn_g"], 16)
    shared["wq_t"] = np.ascontiguousarray(f(inp["peer_w_q"]).reshape(16, 128, 16, 128).transpose(2, 1, 0, 3))
    k1 = f(inp["peer_keys1"]).transpose(2, 0, 1)
    k2 = f(inp["peer_keys2"]).transpose(2, 0, 1)
    shared["keysT"] = np.ascontiguousarray(np.concatenate([k1, k2], axis=1))
    shared["U_t"] = np.ascontiguousarray(f(inp["peer_u"]).reshape(128, 128, 16, 128).transpose(0, 3, 2, 1))
    shared["Vp"] = f(inp["peer_v"])
    maps = []
    for c in range(8):
        b, p = c // 2, c % 2
        xa = np.zeros((2048, D), np.float32)
        if p == 1:
            xa[:] = x[b]
        else:
            xa[1024:] = x[b, :1024]
        kv = np.ones((128, 16), np.float32)
        if p == 0:
            kv[:, :8] = 0.0
        m = dict(shared)
        m["x_all"] = xa
        m["kvalid"] = kv
        maps.append(m)
    return maps


_NC_CACHE = {}


def kernel(**inputs):
    if "nc" not in _NC_CACHE:
        _NC_CACHE["nc"] = build()
    nc = _NC_CACHE["nc"]
    maps = _prep(inputs)
    res = run_bass_kernel_spmd(nc, maps, core_ids=list(range(8)))
    out = np.zeros((4, 2048, D), np.float32)
    for c in range(8):
        b, p = c // 2, c % 2
        out[b, p * 1024:(p + 1) * 1024] = res.results[c]["y"]
    return out
```

```python
import math
from contextlib import ExitStack

import numpy as np
import concourse.bass as bass
import concourse.mybir as mybir
from concourse.bass_utils import run_bass_kernel_spmd

F32 = mybir.dt.float32
BF16 = mybir.dt.bfloat16
I32 = mybir.dt.int32
AF = mybir.ActivationFunctionType
ALU = mybir.AluOpType

D = 2048
EPS = 1e-6
GW = 2560


class _Res:
    __slots__ = ("w", "rs")

    def __init__(self):
        self.w = None
        self.rs = {}


class Region:
    def __init__(self, lo, hi):
        self.lo, self.hi, self.top = lo, hi, lo

    def take(self, nbytes, name=""):
        off = self.top
        self.top += nbytes
        assert self.top <= self.hi, f"SBUF region overflow at {name}: {self.top} > {self.hi}"
        return off


KBYTE = 1024
SB0 = 16512


class KB:
    ENG = ("pe", "act", "dve", "pool", "sp")

    def __init__(self, nc):
        self.nc = nc
        self.streams = {e: [] for e in self.ENG}
        self.sems = {}
        self.cnt = {}
        self.waited = {e: {} for e in self.ENG}
        self.res = {}
        self.es = ExitStack()

    def sb(self, name, shape, dtype, region):
        nbytes = int(np.prod(shape[1:])) * (2 if dtype == BF16 else 4)
        nbytes = (nbytes + 63) // 64 * 64
        off = region.take(nbytes, name) + SB0
        self.nalloc = getattr(self, "nalloc", 0) + 1
        return self.nc.alloc_sbuf_tensor_at(f"{name}_{self.nalloc}", list(shape), dtype, offset=off)

    def ps(self, name, shape, dtype, stack=None):
        return (stack or self.es).enter_context(self.nc.psum_tensor(name, list(shape), dtype))

    def _sem(self, name):
        if name not in self.sems:
            self.sems[name] = self.es.enter_context(self.nc.semaphore(name))
            self.cnt[name] = 0
        return name

    def _deps(self, eng, reads, writes):
        need = {}

        def add(s, v):
            if need.get(s, 0) < v:
                need[s] = v

        for r in reads:
            rr = self.res.get(r)
            if rr is not None and rr.w is not None:
                add(*rr.w)
        for w in writes:
            rr = self.res.get(w)
            if rr is not None:
                if rr.w is not None:
                    add(*rr.w)
                for s, v in rr.rs.items():
                    add(s, v)
        out = []
        wd = self.waited[eng]
        for s, v in need.items():
            if eng == "pe" and s == "e_pe":
                continue
            if wd.get(s, 0) < v:
                wd[s] = v
                out.append((s, v))
        return out

    def op(self, eng, fn, reads=(), writes=(), sem=None, inc=1, wr_late=()):
        waits = self._deps(eng, reads, writes)
        s = self._sem(sem or ("e_" + eng))
        self.cnt[s] += inc
        ev = (s, self.cnt[s])
        self.streams[eng].append((waits, fn, s, inc))
        for r in reads:
            rr = self.res.setdefault(r, _Res())
            if rr.rs.get(ev[0], 0) < ev[1]:
                rr.rs[ev[0]] = ev[1]
        for w in tuple(writes) + tuple(wr_late):
            rr = self.res.setdefault(w, _Res())
            rr.w = ev
            rr.rs = {}
        return ev

    def dma(self, out, in_, reads=(), writes=(), chan="d0", eng="sp", **kw):
        return self.op(eng, lambda e: e.dma_start(out=out, in_=in_, **kw), reads, writes,
                       sem="dma_" + chan, inc=16)

    def wait_all(self, eng, keys):
        waits = self._deps(eng, keys, ())
        self.streams[eng].append((waits, None, None, 0))

    def flush(self):
        for eng in self.ENG:
            waits = []
            wd = self.waited[eng]
            for s, v in self.cnt.items():
                if v > 0 and wd.get(s, 0) < v:
                    wd[s] = v
                    waits.append((s, v))
            self.streams[eng].append((waits, None, None, 0))
        with self.nc.Block() as block:
            def mk(stream):
                def body(e):
                    for waits, fn, s, inc in stream:
                        for ws, wv in waits:
                            e.wait_ge(self.sems[ws], wv)
                        if fn is not None:
                            fn(e).then_inc(self.sems[s], inc)
                return body
            block.tensor(mk(self.streams["pe"]))
            block.scalar(mk(self.streams["act"]))
            block.vector(mk(self.streams["dve"]))
            block.gpsimd(mk(self.streams["pool"]))
            block.sync(mk(self.streams["sp"]))
        self.streams = {e: [] for e in self.ENG}


def build(stop=None, dbg=False):
    nc = bass.Bass("TRN2", target_bir_lowering=False)
    k = KB(nc)
    KB_ = KBYTE

    def din(name, shape, dt=F32):
        return nc.dram_tensor(name, list(shape), dt, kind="ExternalInput").ap()

    def dout(name, shape, dt=F32):
        return nc.dram_tensor(name, list(shape), dt, kind="ExternalOutput").ap()

    x_all = din("x_all", [2048, D])
    gmix = din("gmix", [128, 16])
    w_in_t = din("w_in_t", [32, 128, 16, 128])
    gqk = din("gqk", [128, 2])
    relb = din("relb", [32, 16])
    ohm = din("ohm", [32, GW])
    kvalid = din("kvalid", [128, 16])
    cmats = din("cmats", [4, 128, 128])
    gmixed = din("gmixed", [128, 16])
    gsc = nc.dram_tensor("gsc", [16, GW], BF16, kind="Internal").ap()
    y = dout("y", [1024, D])

    def dbg_dump(name, src_ap, shape, key, dt=F32):
        if not dbg:
            return
        o = dout("dbg_" + name, shape, dt)
        k.dma(o, src_ap, reads=[key], writes=["dbg_" + name], chan="dbg")

    banks = [k.ps(f"bank{i}", [128, 512], F32) for i in range(8)]
    PS = lambda i: ("ps", i)

    RC = Region(0, 8 * KB_)
    cm = k.sb("cm", [128, 4, 128], F32, RC)
    k.dma(cm[:], cmats.rearrange("c p j -> p c j"), writes=["cm"], chan="c0")
    identb = k.sb("identb", [128, 128], BF16, RC)
    antib = k.sb("antib", [128, 128], BF16, RC)
    k.op("dve", lambda e: e.tensor_copy(out=identb[:], in_=cm[:, 0, :]), ["cm"], ["identb"])
    k.op("dve", lambda e: e.tensor_copy(out=antib[:], in_=cm[:, 1, :]), ["cm"], ["antib"])
    blk64 = cm[:, 2, :]
    onesf = cm[:, 3, :]
    gqk_sb = k.sb("gqk_sb", [128, 2], F32, RC)
    k.dma(gqk_sb[:], gqk[:, :], writes=["gqk"], chan="c1")
    kv_sb = k.sb("kv_sb", [128, 16], F32, RC)
    k.dma(kv_sb[:], kvalid[:, :], writes=["kv"], chan="c2")
    gmixed_sb = k.sb("gmixed_sb", [128, 16], F32, RC)
    k.dma(gmixed_sb[:], gmixed[:, :], writes=["gmixed"], chan="c3")
    gmix_sb = k.sb("gmix_sb", [128, 16], F32, RC)
    k.dma(gmix_sb[:], gmix[:, :], writes=["gmix"], chan="c4")

    RM = Region(8 * KB_, 120 * KB_)
    qT = k.sb("qT", [128, 8, 1024], BF16, RM)
    kT = k.sb("kT", [128, 8, 2048], BF16, RM)
    Vt = k.sb("Vt", [128, 16, 1024], BF16, RM)
    uT = k.sb("uT", [128, 8, 2048], BF16, RM)

    R = Region(120 * KB_, 207 * KB_)
    hnT2 = [k.sb(f"hnT{i}", [128, 16, 512], BF16, R) for i in range(2)]
    xbuf = [k.sb(f"xbuf{i}", [128, D], F32, R) for i in range(2)]
    hnb = [k.sb(f"hnb{i}", [128, D], BF16, R) for i in range(2)]
    stat = k.sb("stat", [128, 16], F32, R)
    wbf = [k.sb(f"wbf{i}", [128, 16, 128], BF16, R) for i in range(4)]
    sqb = [k.sb(f"sqb{i}", [128, 512], F32, R) for i in range(2)]
    rsb = [k.sb(f"rsb{i}", [128, 512], F32, R) for i in range(2)]
    pc_ = {"w": 0, "p": 0, "x": 0}

    def norm_tile(ps_, tt):
        hnT = hnT2[ps_ % 2]
        hk = ("hnT", ps_ % 2)
        b = pc_["x"] % 2
        pc_["x"] += 1
        xi = pc_["x"] % 16
        r0 = ps_ * 512 + tt * 128
        sk = ("stat", xi)
        sc = stat[:, xi: xi + 1]
        k.dma(xbuf[b][:], x_all[r0:r0 + 128, :], writes=[("xb", b)], chan=f"x{b}")
        k.op("act", lambda e: e.activation(out=hnb[b][:], in_=xbuf[b][:], func=AF.Square, accum_out=sc), [("xb", b)], [("hnb", b), sk])
        k.op("dve", lambda e: e.tensor_scalar(out=sc, in0=sc, scalar1=1.0 / D, scalar2=EPS, op0=ALU.mult, op1=ALU.add), [sk], [sk])
        k.op("act", lambda e: e.activation(out=sc, in_=sc, func=AF.Sqrt), [sk], [sk])
        k.op("dve", lambda e: e.reciprocal(out=sc, in_=sc), [sk], [sk])
        k.op("dve", lambda e: e.tensor_scalar(out=hnb[b][:], in0=xbuf[b][:], scalar1=sc, scalar2=None, op0=ALU.mult), [("xb", b), sk], [("hnb", b)])
        for hf in range(2):
            bk = hf
            pv = banks[bk][:].bitcast(BF16)
            for j in range(8):
                kc = hf * 8 + j
                k.op("pe", lambda e, kc=kc, j=j, pv=pv: e.transpose(out=pv[:, j * 128:(j + 1) * 128], in_=hnb[b][:, kc * 128:(kc + 1) * 128], identity=identb[:]),
                     [("hnb", b), "identb"], [PS(bk)] if j == 0 else [], wr_late=[PS(bk)] if j else [])
            for j in range(8):
                kc = hf * 8 + j
                k.op("act", lambda e, kc=kc, j=j, pv=pv: e.activation(out=hnT[:, kc, tt * 128:(tt + 1) * 128], in_=pv[:, j * 128:(j + 1) * 128],
                                                                 func=AF.Copy, scale=gmix_sb[:, kc:kc + 1]),
                     [PS(bk), "gmix"], [hk])

    def proj_chunk(ps_, fc):
        hnT = hnT2[ps_ % 2]
        hk = ("hnT", ps_ % 2)
        tok0 = ps_ * 512
        wb = pc_["w"] % 4
        pc_["w"] += 1
        k.dma(wbf[wb][:], w_in_t[fc], writes=[("wbf", wb)], chan=f"w{wb}", eng="pool")
        if fc < 16 or fc >= 24:
            bk = 2 + pc_["p"] % 2
            pb = pc_["p"] % 2
            pc_["p"] += 1
            for kc in range(16):
                k.op("pe", lambda e, kc=kc: e.matmul(out=banks[bk][:], lhsT=wbf[wb][:, kc, :], rhs=hnT[:, kc, :], start=(kc == 0), stop=(kc == 15)),
                     [("wbf", wb), hk], [PS(bk)] if kc == 0 else [], wr_late=[PS(bk)] if kc else [])
            if fc >= 24:
                dst = uT[:, fc - 24, tok0: tok0 + 512]
                k.op("act", lambda e: e.activation(out=dst, in_=banks[bk][:], func=AF.Copy), [PS(bk)], ["uT"])
                return
            sb_ = 4 + pb
            k.op("act", lambda e: e.activation(out=sqb[pb][:], in_=banks[bk][:], func=AF.Square), [PS(bk)], [("sqb", pb)])
            k.op("pe", lambda e: e.matmul(out=banks[sb_][:], lhsT=blk64, rhs=sqb[pb][:], start=True, stop=True), [("sqb", pb), "cm"], [PS(sb_)])
            k.op("dve", lambda e: e.tensor_scalar(out=rsb[pb][:], in0=banks[sb_][:], scalar1=1.0 / 64, scalar2=EPS, op0=ALU.mult, op1=ALU.add), [PS(sb_)], [("rsb", pb)])
            k.op("act", lambda e: e.activation(out=rsb[pb][:], in_=rsb[pb][:], func=AF.Sqrt), [("rsb", pb)], [("rsb", pb)])
            k.op("dve", lambda e: e.reciprocal(out=rsb[pb][:], in_=rsb[pb][:]), [("rsb", pb)], [("rsb", pb)])
            if fc < 8:
                dst = qT[:, fc, tok0 - 1024: tok0 - 512]
                g = gqk_sb[:, 0:1]
                dk = "qT"
            else:
                dst = kT[:, fc - 8, tok0: tok0 + 512]
                g = gqk_sb[:, 1:2]
                dk = "kT"
            k.op("dve", lambda e: e.scalar_tensor_tensor(out=dst, in0=banks[bk][:], scalar=g, in1=rsb[pb][:], op0=ALU.mult, op1=ALU.mult),
                 [PS(bk), ("rsb", pb), "gqk"], [dk])
        else:
            for tt in range(4):
                bk = 6 + tt % 2
                for kc in range(16):
                    k.op("pe", lambda e, kc=kc, tt=tt, bk=bk: e.matmul(out=banks[bk][:, 0:128], lhsT=hnT[:, kc, tt * 128:(tt + 1) * 128], rhs=wbf[wb][:, kc, :], start=(kc == 0), stop=(kc == 15)),
                         [("wbf", wb), hk], [PS(bk)] if kc == 0 else [], wr_late=[PS(bk)] if kc else [])
                dst = Vt[:, ps_ * 4 + tt, (fc - 16) * 128:(fc - 15) * 128]
                k.op("act", lambda e, dst=dst, bk=bk: e.activation(out=dst, in_=banks[bk][:, 0:128], func=AF.Copy), [PS(bk)], ["Vt"])

    for tt in range(4):
        norm_tile(0, tt)
    for ps_ in range(4):
        fcs = list(range(32) if ps_ >= 2 else range(8, 32))
        step = len(fcs) // 4
        for idx, fc in enumerate(fcs):
            proj_chunk(ps_, fc)
            if ps_ + 1 < 4 and idx % step == step - 1:
                norm_tile(ps_ + 1, idx // step)
    k.flush()
    if dbg and stop == "proj":
        dbg_dump("qT", qT[:], [128, 8, 1024], "qT", BF16)
        dbg_dump("kT", kT[:], [128, 8, 2048], "kT", BF16)
        dbg_dump("Vt", Vt[:], [128, 16, 1024], "Vt", BF16)
        dbg_dump("uT", uT[:], [128, 8, 2048], "uT", BF16)
    if stop == "proj":
        k.flush()
        return nc

    R = Region(120 * KB_, 207 * KB_)
    attnT = k.sb("attnT", [128, 8, 1024], F32, R)
    relb_sb = k.sb("relb_sb", [32, 16], F32, R)
    ohm_sb = k.sb("ohm_sb", [32, GW], F32, R)
    gs_sb = k.sb("gs_sb", [16, GW], BF16, R)
    Bt = [k.sb(f"Bt{i}", [128, 2048], BF16, R) for i in range(2)]
    Tt = [k.sb(f"Tt{i}", [128, 2048], BF16, R) for i in range(2)]
    pw = [k.sb(f"pw{i}", [128, 512], BF16, R) for i in range(3)]
    pw2 = [k.sb(f"pw2{i}", [128, 512], BF16, R) for i in range(3)]
    kvones = k.sb("kvones", [128, 16, 64], BF16, R)
    rden = k.sb("rden", [128, 1024], F32, R)
    k.dma(relb_sb[:], relb[:, :], writes=["relb"], chan="a0")
    k.dma(ohm_sb[:], ohm[:, :], writes=["ohm"], chan="a1")
    k.op("act", lambda e: e.activation(out=relb_sb[:], in_=relb_sb[:], func=AF.Exp), ["relb"], ["relb"])
    for j in range(GW // 512):
        k.op("pe", lambda e, j=j: e.matmul(out=banks[6][0:16, :], lhsT=relb_sb[:, :], rhs=ohm_sb[:, j * 512:(j + 1) * 512], start=True, stop=True),
             ["relb", "ohm"], [PS(6)])
        k.op("act", lambda e, j=j: e.activation(out=gs_sb[:, j * 512:(j + 1) * 512], in_=banks[6][0:16, :], func=AF.Copy), [PS(6)], ["gs_sb"])
    k.dma(gsc[:, :], gs_sb[:], reads=["gs_sb"], writes=["gsc"], chan="a2")
    k.op("dve", lambda e: e.tensor_copy(out=kvones[:], in_=kv_sb[:].unsqueeze(2).to_broadcast([128, 16, 64])), ["kv"], ["kvones"])
    def head_pieces(h):
        out = []
        for kt in range(16):
            qlo = max(0, (kt - 8) * 128)
            for half in range(2):
                lo = max(qlo, half * 512)
                hi = (half + 1) * 512
                if lo < hi:
                    out.append((h, kt, half, lo, hi))
        return out

    SB3 = (0, 1, 7)

    def build_table(h):
        tb = h % 2
        k.dma(Bt[tb][:], bass.AP(gsc.tensor, h * GW, [[1, 128], [1, 2048]]), reads=["gsc"], writes=[("Bt", tb)], chan=f"bt{tb}")
        for j in range(4):
            k.op("pe", lambda e, tb=tb, j=j: e.matmul(out=banks[6][:], lhsT=antib[:], rhs=Bt[tb][:, j * 512:(j + 1) * 512], start=True, stop=True),
                 [("Bt", tb), "antib"], [PS(6)])
            k.op("act", lambda e, tb=tb, j=j: e.activation(out=Tt[tb][:, j * 512:(j + 1) * 512], in_=banks[6][:], func=AF.Copy), [PS(6)], [("Tt", tb)])

    def s_mm(idx, pc):
        h, kt, half, lo, hi = pc
        hp, hh = h // 2, h % 2
        pl = slice(64 * hh, 64 * hh + 64)
        n = hi - lo
        sbk = SB3[idx % 3]
        k.op("pe", lambda e, pl=pl, hp=hp, kt=kt, lo=lo, hi=hi, n=n, sbk=sbk: e.matmul(out=banks[sbk][:, 0:n], lhsT=kT[pl, hp, kt * 128:(kt + 1) * 128], rhs=qT[pl, hp, lo:hi], start=True, stop=True),
             ["kT", "qT"], [PS(sbk)])

    def rest(idx, pc):
        h, kt, half, lo, hi = pc
        hp, hh = h // 2, h % 2
        tb = h % 2
        pl = slice(64 * hh, 64 * hh + 64)
        n = hi - lo
        c0 = 1024 + lo - kt * 128
        sbk = SB3[idx % 3]
        bi = idx % 3
        k.op("act", lambda e, sbk=sbk, n=n, bi=bi: e.activation(out=pw[bi][:, 0:n], in_=banks[sbk][:, 0:n], func=AF.Exp, scale=0.125), [PS(sbk)], [("pw", bi)])
        k.op("dve", lambda e, bi=bi, n=n, tb=tb, c0=c0: e.tensor_tensor(out=pw2[bi][:, 0:n], in0=pw[bi][:, 0:n], in1=Tt[tb][:, c0:c0 + n], op=ALU.mult),
             [("pw", bi), ("Tt", tb)], [("pw2", bi)])
        first = (kt == 0)
        last = (kt == (11 if half == 0 else 15))
        nb, db = 2 + half, 4 + half
        oc = slice(lo - half * 512, hi - half * 512)
        k.op("pe", lambda e, pl=pl, kt=kt, h=h, bi=bi, n=n, nb=nb, oc=oc, first=first, last=last: e.matmul(out=banks[nb][pl, oc], lhsT=Vt[:, kt, h * 64:(h + 1) * 64], rhs=pw2[bi][:, 0:n], start=first, stop=last),
             [("pw2", bi), "Vt"], [PS(nb)] if (first and hh == 0) else [], wr_late=[] if (first and hh == 0) else [PS(nb)])
        k.op("pe", lambda e, pl=pl, kt=kt, bi=bi, n=n, db=db, oc=oc, first=first, last=last: e.matmul(out=banks[db][pl, oc], lhsT=kvones[:, kt, :], rhs=pw2[bi][:, 0:n], start=first, stop=last),
             [("pw2", bi), "kvones"], [PS(db)] if (first and hh == 0) else [], wr_late=[] if (first and hh == 0) else [PS(db)])

    build_table(0)
    gidx = 0
    for hp in range(8):
        pcs = head_pieces(2 * hp) + head_pieces(2 * hp + 1)
        nh0 = len(head_pieces(2 * hp))
        base = gidx
        for q in range(min(2, len(pcs))):
            s_mm(base + q, pcs[q])
        for q, pc in enumerate(pcs):
            if q == 0:
                build_table(2 * hp + 1)
            if q == nh0 and hp < 7:
                build_table(2 * hp + 2)
            if q + 2 < len(pcs):
                s_mm(base + q + 2, pcs[q + 2])
            rest(base + q, pc)
        gidx += len(pcs)
        for half in range(2):
            hs = slice(half * 512, (half + 1) * 512)
            k.op("dve", lambda e, half=half, hs=hs: e.reciprocal(out=rden[:, hs], in_=banks[4 + half][:]), [PS(4 + half)], [("rden", half)])
            k.op("dve", lambda e, half=half, hs=hs, hp=hp: e.tensor_tensor(out=attnT[:, hp, hs], in0=banks[2 + half][:], in1=rden[:, hs], op=ALU.mult),
                 [PS(2 + half), ("rden", half)], ["attnT"])
    k.flush()
    if dbg and stop == "attn":
        dbg_dump("attnT", attnT[:], [128, 8, 1024], "attnT", F32)
    if stop == "attn":
        k.flush()
        return nc

    RX = Region(8 * KB_, 40 * KB_)
    mixedT = k.sb("mixedT", [128, 16, 1024], BF16, RX)
    RN = Region(152 * KB_, 168 * KB_)
    sqa = [k.sb(f"sqa{i}", [128, 1024], F32, RN) for i in range(2)]
    rstd_a = k.sb("rstd_a", [128, 1024], F32, RN)

    def out_norm(srcT, row0, keyfn):
        for c8 in range(8):
            sq = sqa[c8 % 2]
            k.op("act", lambda e, c8=c8, sq=sq: e.activation(out=sq[:], in_=srcT[:, c8, :], func=AF.Square), [keyfn(c8)], [("sqa", c8 % 2)])
            for half in range(2):
                k.op("pe", lambda e, c8=c8, sq=sq, half=half: e.matmul(out=banks[6 + half][:], lhsT=onesf, rhs=sq[:, half * 512:(half + 1) * 512], start=(c8 == 0), stop=(c8 == 7)),
                     [("sqa", c8 % 2), "cm"], [PS(6 + half)] if c8 == 0 else [], wr_late=[PS(6 + half)] if c8 else [])
        for half in range(2):
            hs = slice(half * 512, (half + 1) * 512)
            k.op("dve", lambda e, half=half, hs=hs: e.tensor_scalar(out=rstd_a[:, hs], in0=banks[6 + half][:], scalar1=1.0 / 1024, scalar2=EPS, op0=ALU.mult, op1=ALU.add),
                 [PS(6 + half)], [("rstd_a", half)])
            k.op("act", lambda e, hs=hs: e.activation(out=rstd_a[:, hs], in_=rstd_a[:, hs], func=AF.Sqrt), [("rstd_a", half)], [("rstd_a", half)])
            k.op("dve", lambda e, hs=hs: e.reciprocal(out=rstd_a[:, hs], in_=rstd_a[:, hs]), [("rstd_a", half)], [("rstd_a", half)])
        for c8 in range(8):
            k.op("dve", lambda e, c8=c8: e.scalar_tensor_tensor(out=mixedT[:, row0 + c8, :], in0=srcT[:, c8, :], scalar=gmixed_sb[:, row0 + c8:row0 + c8 + 1], in1=rstd_a[:],
                                                               op0=ALU.mult, op1=ALU.mult),
                 [keyfn(c8), ("rstd_a", 0), ("rstd_a", 1), "gmixed"], ["mixedT"])

    out_norm(attnT, 0, lambda c8: "attnT")
    k.flush()

    lam_l = din("lam_l", [3, 128, 32])
    bc_l = din("bc_l", [4, 128, 32, 16])
    dg_l = din("dg_l", [128, 16])
    glu_w = din("glu_w", [1024, 1024])
    tidx = din("tidx", [128, 512])
    RA = Region(40 * KB_, 88 * KB_)
    RB = Region(120 * KB_, 207 * KB_)
    BT = k.sb("BT", [128, 8, 2, 128], BF16, RA)
    Cblk = k.sb("Cblk", [128, 32, 2, 64], BF16, RA)
    rho = k.sb("rho", [128, 32], F32, RA)
    uturn = k.sb("uturn", [128, 32], F32, RA)
    c512 = k.sb("c512", [128, 32], F32, RA)
    s512 = k.sb("s512", [128, 32], F32, RA)
    tix = k.sb("tix", [128, 512], F32, RC)
    RA2 = Region(24 * KB_, 40 * KB_)
    BT3 = k.sb("BT3", [128, 8, 2, 128], BF16, RA)
    dg = k.sb("dg", [128, 16], F32, RA)
    k.dma(tix[:], tidx[:, :], writes=["tix"], chan="s0")
    k.dma(dg[:], dg_l[:, :], writes=["dg"], chan="s1")
    RP = Region(120 * KB_, 207 * KB_)
    lam = k.sb("lam", [128, 3, 32], F32, RP)
    bc = k.sb("bc", [128, 4, 32, 16], F32, RP)
    k.dma(lam[:], lam_l.rearrange("c p j -> p c j"), writes=["lam"], chan="s2")
    k.dma(bc[:], bc_l.rearrange("c p j x -> p c j x"), writes=["bc"], chan="s3")
    P_ = {}
    for nm in ["dt", "th", "v", "vf", "sn", "cs", "are", "aim", "den", "fre", "fim", "t1", "t2"]:
        P_[nm] = k.sb("p_" + nm, [128, 32], F32, RP)
    pvi = k.sb("p_vi", [128, 32], I32, RP)
    lr, li, ldt = lam[:, 0, :], lam[:, 1, :], lam[:, 2, :]
    TWO_PI = 6.2831845
    PI_ = 3.1415920

    def dv(fn, reads, writes):
        k.op("dve", fn, reads, writes)

    def ac(fn, reads, writes):
        k.op("act", fn, reads, writes)

    tt_ = lambda o, a, b, op: (lambda e: e.tensor_tensor(out=o, in0=a, in1=b, op=op))
    ac(lambda e: e.activation(out=P_["dt"][:], in_=ldt, func=AF.Exp), ["lam"], ["p_dt"])
    dv(tt_(P_["th"][:], li, P_["dt"][:], ALU.mult), ["lam", "p_dt"], ["p_th"])
    dv(tt_(P_["t1"][:], lr, P_["dt"][:], ALU.mult), ["lam", "p_dt"], ["p_t1"])
    ac(lambda e: e.activation(out=rho[:], in_=P_["t1"][:], func=AF.Exp), ["p_t1"], ["rho"])
    dv(lambda e: e.tensor_scalar(out=uturn[:], in0=P_["th"][:], scalar1=1.0 / (2 * math.pi), scalar2=None, op0=ALU.mult), ["p_th"], ["uturn"])
    for off, dst in ((0.0, "sn"), (0.25, "cs")):
        dv(lambda e, off=off: e.tensor_scalar(out=P_["v"][:], in0=uturn[:], scalar1=off, scalar2=None, op0=ALU.add), ["uturn"], ["p_v"])
        dv(lambda e: e.tensor_copy(out=pvi[:], in_=P_["v"][:]), ["p_v"], ["p_vi"])
        dv(tt_(P_["vf"][:], P_["v"][:], pvi[:], ALU.subtract), ["p_v", "p_vi"], ["p_vf"])
        ac(lambda e, dst=dst: e.activation(out=P_[dst][:], in_=P_["vf"][:], func=AF.Sin, scale=TWO_PI), ["p_vf"], ["p_" + dst])
    dv(lambda e: e.tensor_scalar(out=P_["v"][:], in0=uturn[:], scalar1=512.0, scalar2=None, op0=ALU.mult), ["uturn"], ["p_v"])
    dv(lambda e: e.tensor_copy(out=pvi[:], in_=P_["v"][:]), ["p_v"], ["p_vi"])
    dv(tt_(P_["vf"][:], P_["v"][:], pvi[:], ALU.subtract), ["p_v", "p_vi"], ["p_vf"])
    ac(lambda e: e.activation(out=s512[:], in_=P_["vf"][:], func=AF.Sin, scale=TWO_PI), ["p_vf"], ["s512"])
    ac(lambda e: e.activation(out=P_["t1"][:], in_=P_["vf"][:], func=AF.Sin, scale=PI_), ["p_vf"], ["p_t1"])
    ac(lambda e: e.activation(out=P_["t1"][:], in_=P_["t1"][:], func=AF.Square), ["p_t1"], ["p_t1"])
    dv(lambda e: e.tensor_scalar(out=c512[:], in0=P_["t1"][:], scalar1=-2.0, scalar2=1.0, op0=ALU.mult, op1=ALU.add), ["p_t1"], ["c512"])
    dv(tt_(P_["are"][:], rho[:], P_["cs"][:], ALU.mult), ["rho", "p_cs"], ["p_are"])
    dv(tt_(P_["aim"][:], rho[:], P_["sn"][:], ALU.mult), ["rho", "p_sn"], ["p_aim"])
    dv(tt_(P_["t1"][:], lr, lr, ALU.mult), ["lam"], ["p_t1"])
    dv(tt_(P_["t2"][:], li, li, ALU.mult), ["lam"], ["p_t2"])
    dv(tt_(P_["den"][:], P_["t1"][:], P_["t2"][:], ALU.add), ["p_t1", "p_t2"], ["p_den"])
    dv(lambda e: e.reciprocal(out=P_["den"][:], in_=P_["den"][:]), ["p_den"], ["p_den"])
    dv(lambda e: e.tensor_scalar(out=P_["are"][:], in0=P_["are"][:], scalar1=-1.0, scalar2=None, op0=ALU.add), ["p_are"], ["p_are"])
    dv(tt_(P_["t1"][:], P_["are"][:], lr, ALU.mult), ["p_are", "lam"], ["p_t1"])
    dv(tt_(P_["t2"][:], P_["aim"][:], li, ALU.mult), ["p_aim", "lam"], ["p_t2"])
    dv(tt_(P_["fre"][:], P_["t1"][:], P_["t2"][:], ALU.add), ["p_t1", "p_t2"], ["p_fre"])
    dv(tt_(P_["fre"][:], P_["fre"][:], P_["den"][:], ALU.mult), ["p_fre", "p_den"], ["p_fre"])
    dv(tt_(P_["t1"][:], P_["aim"][:], lr, ALU.mult), ["p_aim", "lam"], ["p_t1"])
    dv(tt_(P_["t2"][:], P_["are"][:], li, ALU.mult), ["p_are", "lam"], ["p_t2"])
    dv(tt_(P_["fim"][:], P_["t1"][:], P_["t2"][:], ALU.subtract), ["p_t1", "p_t2"], ["p_fim"])
    dv(tt_(P_["fim"][:], P_["fim"][:], P_["den"][:], ALU.mult), ["p_fim", "p_den"], ["p_fim"])
    bb = [k.sb(f"bb{i}", [128, 32, 16], F32, RP) for i in range(4)]
    fre_b = P_["fre"][:].unsqueeze(2).to_broadcast([128, 32, 16])
    fim_b = P_["fim"][:].unsqueeze(2).to_broadcast([128, 32, 16])
    dv(tt_(bb[0][:], bc[:, 0], fre_b, ALU.mult), ["bc", "p_fre"], ["bb0"])
    dv(tt_(bb[1][:], bc[:, 1], fim_b, ALU.mult), ["bc", "p_fim"], ["bb1"])
    dv(tt_(bb[0][:], bb[0][:], bb[1][:], ALU.subtract), ["bb0", "bb1"], ["bb0"])
    dv(tt_(bb[2][:], bc[:, 1], fre_b, ALU.mult), ["bc", "p_fre"], ["bb2"])
    dv(tt_(bb[3][:], bc[:, 0], fim_b, ALU.mult), ["bc", "p_fim"], ["bb3"])
    dv(tt_(bb[2][:], bb[2][:], bb[3][:], ALU.add), ["bb2", "bb3"], ["bb2"])
    Bblk = k.sb("Bblk", [128, 8, 2, 4, 32], F32, RP)
    Cf = k.sb("Cf", [128, 32, 2, 64], F32, RP)
    Bblk3 = k.sb("Bblk3", [128, 8, 2, 4, 32], F32, RP)
    dv(lambda e: e.memset(Bblk3[:], 0.0), [], ["Bblk3"])
    dv(lambda e: e.memset(Bblk[:], 0.0), [], ["Bblk"])
    dv(lambda e: e.memset(Cf[:], 0.0), [], ["Cf"])
    dv(lambda e: e.tensor_scalar(out=bc[:, 3], in0=bc[:, 3], scalar1=-1.0, scalar2=None, op0=ALU.mult), ["bc"], ["bc"])
    for gl in range(2):
        ls = slice(64 * gl, 64 * gl + 64)
        cs_ = slice(16 * gl, 16 * gl + 16)
        for ri, src in ((0, bb[0]), (1, bb[2])):
            dv(lambda e, ls=ls, cs_=cs_, ri=ri, src=src: e.tensor_copy(out=Bblk[ls, :, ri, :, cs_], in_=src[ls].rearrange("p (i j) c -> p i j c", j=4)),
               ["bb0", "bb2", "Bblk"], ["Bblk"])
        for ri in range(2):
            cv = Cf[ls, :, ri, :].rearrange("p (i j) c -> p i j c", j=4)
            sv = bc[ls, 2 + ri].rearrange("p (i j) c -> p i j c", j=4)
            dv(lambda e, cv=cv, sv=sv, gl=gl: e.tensor_copy(out=cv[:, :, 0:3, 16 * gl:16 * gl + 16], in_=sv[:, :, 0:3, :]), ["bc", "Cf"], ["Cf"])
            dv(lambda e, cv=cv, sv=sv, gl=gl: e.tensor_copy(out=cv[:, :, 3:4, 32 + 16 * gl:48 + 16 * gl], in_=sv[:, :, 3:4, :]), ["bc", "Cf"], ["Cf"])
    dv(lambda e: e.tensor_copy(out=Cblk[:], in_=Cf[:]), ["Cf"], ["Cblk"])
    for i in range(8):
        for ri in range(2):
            bk = 6 + (2 * i + ri) % 2
            k.op("pe", lambda e, i=i, ri=ri, bk=bk: e.transpose(out=banks[bk][:, 0:128], in_=Bblk[:, i, ri].rearrange("p j c -> p (j c)"), identity=cm[:, 0, :]),
                 ["Bblk", "cm"], [PS(bk)])
            ac(lambda e, i=i, ri=ri, bk=bk: e.activation(out=BT[:, i, ri, :], in_=banks[bk][:, 0:128], func=AF.Copy), [PS(bk)], ["BT"])
    dv(lambda e: e.tensor_copy(out=Bblk3[:, :, :, 3, :], in_=Bblk[:, :, :, 3, :]), ["Bblk", "Bblk3"], ["Bblk3"])
    for i in range(8):
        for ri in range(2):
            bk = 6 + (2 * i + ri) % 2
            k.op("pe", lambda e, i=i, ri=ri, bk=bk: e.transpose(out=banks[bk][:, 0:128], in_=Bblk3[:, i, ri].rearrange("p j c -> p (j c)"), identity=cm[:, 0, :]),
                 ["Bblk3", "cm"], [PS(bk)])
            ac(lambda e, i=i, ri=ri, bk=bk: e.activation(out=BT3[:, i, ri, :], in_=banks[bk][:, 0:128], func=AF.Copy), [PS(bk)], ["BT3"])
    k.flush()
    if dbg and stop == "ssmprep":
        dbg_dump("BT", BT[:], [128, 8, 2, 128], "BT", BF16)
        dbg_dump("Cblk", Cblk[:], [128, 32, 2, 32], "Cblk", BF16)
        dbg_dump("rho", rho[:], [128, 32], "rho")
        dbg_dump("uturn", uturn[:], [128, 32], "uturn")
        k.flush()
        return nc

    ygf = k.sb("ygf", [128, 8, 1024], F32, RB)
    ygb = k.sb("ygb", [128, 8, 1024], BF16, RB)
    gw = k.sb("gw", [128, 8, 1024], BF16, RB)
    sqa = [k.sb(f"sqb_{i}", [128, 1024], F32, RB) for i in range(2)]
    rstd_a = k.sb("rstd_b", [128, 1024], F32, RB)
    ytmp = k.sb("ytmp", [128, 512], F32, RB)
    sg = k.sb("sg", [128, 512], F32, RB)
    for kc in range(8):
        k.dma(gw[:, kc, :], glu_w[kc * 128:(kc + 1) * 128, :], writes=["gw"], chan="gw0", eng="pool")
    vb = k.sb("vb", [128, 512], F32, RA)
    vib = k.sb("vib", [128, 512], I32, RA)
    rb_ = k.sb("rb_", [128, 512], F32, RA)
    snb = [k.sb(f"snb{i}", [128, 512], F32, RA) for i in range(2)]
    csb = [k.sb(f"csb{i}", [128, 512], F32, RA) for i in range(2)]
    tb1 = [k.sb(f"tb_{i}", [128, 512], F32, RA) for i in range(4)]
    tbs = [tb1, tb1]
    negI = k.sb("negI", [128, 128], F32, RA)
    wsb = [[k.sb(f"wsb{p_}_{i}", [128, 512], F32, RA) for i in range(2)] for p_ in range(2)]
    dv(lambda e: e.tensor_scalar(out=negI[:], in0=cm[:, 0, :], scalar1=-1.0, scalar2=None, op0=ALU.mult), ["cm"], ["negI"])
    cin = [k.sb(f"cin{i}", [128, 4], F32, RA) for i in range(2)]
    zz = [k.sb(f"zz{i}", [128, 2, 512], F32, RA2) for i in range(2)]
    zre = [zz[i][:, 0, :] for i in range(2)]
    zim = [zz[i][:, 1, :] for i in range(2)]
    rot = k.sb("rot", [128, 32, 4], F32, RA)
    cin2 = [k.sb(f"cin2_{i}", [128, 2], F32, RA) for i in range(2)]
    dv(lambda e: e.tensor_copy(out=rot[:, :, 0], in_=c512[:]), ["c512"], ["rot"])
    dv(lambda e: e.tensor_scalar(out=rot[:, :, 1], in0=s512[:], scalar1=-1.0, scalar2=None, op0=ALU.mult), ["s512", "rot"], ["rot"])
    dv(lambda e: e.tensor_copy(out=rot[:, :, 2], in_=s512[:]), ["s512", "rot"], ["rot"])
    dv(lambda e: e.tensor_copy(out=rot[:, :, 3], in_=c512[:]), ["c512", "rot"], ["rot"])
    xq = [[k.sb(f"xq{p_}_{i}", [128, 512], BF16, RA2) for i in range(4)] for p_ in range(2)]
    def cpar(c):
        jp, tc = divmod(c, 4)
        i, j = divmod(jp, 4)
        return jp, tc, i, j, jp % 2, c % 2

    def emit_tables(jp):
        pp = jp % 2
        ac(lambda e, jp=jp: e.activation(out=vb[:], in_=tix[:], func=AF.Copy, scale=uturn[:, jp:jp + 1]), ["tix", "uturn"], ["vb"])
        dv(lambda e: e.tensor_copy(out=vib[:], in_=vb[:]), ["vb"], ["vib"])
        dv(tt_(rb_[:], vb[:], vib[:], ALU.subtract), ["vb", "vib"], ["rb_"])
        ac(lambda e, pp=pp: e.activation(out=snb[pp][:], in_=rb_[:], func=AF.Sin, scale=TWO_PI), ["rb_"], [("snb", pp)])
        ac(lambda e: e.activation(out=vb[:], in_=rb_[:], func=AF.Sin, scale=PI_), ["rb_"], ["vb"])
        ac(lambda e: e.activation(out=vb[:], in_=vb[:], func=AF.Square), ["vb"], ["vb"])
        dv(lambda e, pp=pp: e.tensor_scalar(out=csb[pp][:], in0=vb[:], scalar1=-2.0, scalar2=1.0, op0=ALU.mult, op1=ALU.add), ["vb"], [("csb", pp)])

    def emit_bu(c):
        jp, tc, i, j, pp, pb = cpar(c)
        prs = slice(32 * j, 32 * j + 32)
        ts_ = slice(tc * 512, (tc + 1) * 512)
        for ri, bnk in ((0, 0), (1, 1)):
            if j < 3:
                k.op("pe", lambda e, prs=prs, i=i, ts_=ts_, bnk=bnk, ri=ri: e.matmul(out=banks[bnk][:], lhsT=BT[prs, i, ri, :], rhs=uT[prs, i, ts_], start=True, stop=True),
                     ["BT", "uT"], [PS(bnk)])
            else:
                k.op("pe", lambda e, i=i, ts_=ts_, bnk=bnk, ri=ri: e.matmul(out=banks[bnk][:], lhsT=BT3[:, i, ri, :], rhs=uT[:, i, ts_], start=True, stop=True),
                     ["BT3", "uT"], [PS(bnk)])

    WB = ((2, 3), (6, 7))

    def demod_ops(c):
        jp, tc, i, j, pp, pb = cpar(c)
        T = tbs[pb]
        bre, bim = 0, 1
        TK = lambda q: ("tb", q)
        return [
            lambda: dv(tt_(T[0][:], banks[bre][:], csb[pp][:], ALU.mult), [PS(bre), ("csb", pp)], [TK(0)]),
            lambda: dv(tt_(T[1][:], banks[bim][:], snb[pp][:], ALU.mult), [PS(bim), ("snb", pp)], [TK(1)]),
            lambda: dv(tt_(T[2][:], banks[bim][:], csb[pp][:], ALU.mult), [PS(bim), ("csb", pp)], [TK(2)]),
            lambda: dv(tt_(T[3][:], banks[bre][:], snb[pp][:], ALU.mult), [PS(bre), ("snb", pp)], [TK(3)]),
        ]

    def pool_adds(c):
        jp, tc, i, j, pp, pb = cpar(c)
        T = tbs[pb]
        wr, wi_ = WB[pb]
        TK = lambda q: ("tb", q)
        k.op("pe", lambda e: e.matmul(out=banks[wr][:], lhsT=cm[:, 0, :], rhs=T[0][:], start=True, stop=False), ["cm", TK(0)], [PS(wr)])
        k.op("pe", lambda e: e.matmul(out=banks[wr][:], lhsT=cm[:, 0, :], rhs=T[1][:], start=False, stop=True), ["cm", TK(1)], [], wr_late=[PS(wr)])
        k.op("pe", lambda e: e.matmul(out=banks[wi_][:], lhsT=cm[:, 0, :], rhs=T[2][:], start=True, stop=False), ["cm", TK(2)], [PS(wi_)])
        k.op("pe", lambda e: e.matmul(out=banks[wi_][:], lhsT=negI[:], rhs=T[3][:], start=False, stop=True), ["negI", TK(3)], [], wr_late=[PS(wi_)])
        ac(lambda e: e.activation(out=wsb[pb][0][:], in_=banks[wr][:], func=AF.Copy), [PS(wr)], [("wsb", pb, 0)])
        ac(lambda e: e.activation(out=wsb[pb][1][:], in_=banks[wi_][:], func=AF.Copy), [PS(wi_)], [("wsb", pb, 1)])

    def carry_ops(c):
        jp, tc, i, j, pp, pb = cpar(c)
        if tc == 0:
            return []
        zp = 1 - pb
        zl = zz[zp][:, :, 511].unsqueeze(1).to_broadcast([128, 2, 2])
        rt = rot[:, jp, :].rearrange("p (o k) -> p o k", o=2)
        ci = cin[pb]
        return [
            lambda: dv(lambda e: e.tensor_tensor(out=ci[:, 0:4].rearrange("p (o k) -> p o k", o=2), in0=zl, in1=rt, op=ALU.mult),
                       [("zre", zp), ("zim", zp), "rot"], [("cin", pb)]),
            lambda: dv(lambda e: e.tensor_reduce(out=cin2[pb][:], in_=ci[:, 0:4].rearrange("p (o k) -> p o k", o=2), axis=mybir.AxisListType.X, op=ALU.add),
                       [("cin", pb)], [("cin2", pb)]),
        ]

    def scans(c):
        jp, tc, i, j, pp, pb = cpar(c)
        wr, wi_ = WB[pb]
        rho_c = rho[:, jp:jp + 1]
        if tc == 0:
            ini_re, ini_im, ikeys = 0.0, 0.0, []
        else:
            ini_re, ini_im, ikeys = cin2[pb][:, 0:1], cin2[pb][:, 1:2], [("cin2", pb)]
        dv(lambda e: e.tensor_tensor_scan(out=zre[pb][:], data0=rho_c.to_broadcast([128, 512]), data1=wsb[pb][0][:], initial=ini_re, op0=ALU.mult, op1=ALU.add),
           [("wsb", pb, 0), "rho"] + ikeys, [("zre", pb)])
        dv(lambda e: e.tensor_tensor_scan(out=zim[pb][:], data0=rho_c.to_broadcast([128, 512]), data1=wsb[pb][1][:], initial=ini_im, op0=ALU.mult, op1=ALU.add),
           [("wsb", pb, 1), "rho"] + ikeys, [("zim", pb)])

    def remod(c):
        jp, tc, i, j, pp, pb = cpar(c)
        X = xq[pb]
        XK = lambda q: ("xq", pb, q)
        prs = slice(32 * j, 32 * j + 32)
        dv(tt_(X[0][:], zre[pb][:], csb[pp][:], ALU.mult), [("zre", pb), ("csb", pp)], [XK(0)])
        dv(lambda e: e.scalar_tensor_tensor(out=X[1][:], in0=zim[pb][:], scalar=-1.0, in1=snb[pp][:], op0=ALU.mult, op1=ALU.mult), [("zim", pb), ("snb", pp)], [XK(1)])
        k.op("pool", lambda e: e.tensor_tensor(out=X[2][:], in0=zre[pb][:], in1=snb[pp][:], op=ALU.mult), [("zre", pb), ("snb", pp)], [XK(2)])
        k.op("pool", lambda e: e.tensor_tensor(out=X[3][:], in0=zim[pb][:], in1=csb[pp][:], op=ALU.mult), [("zim", pb), ("csb", pp)], [XK(3)])
        yb = 4 + (tc - 2)
        if j < 2:
            ops_, cw = prs, 32
        else:
            ops_, cw = slice(64, 128), 64
        for q, cv in ((0, 0), (1, 0), (2, 1), (3, 1)):
            first = (q == 0)
            last = (q == 3)
            k.op("pe", lambda e, q=q, cv=cv, first=first, last=last: e.matmul(out=banks[yb][ops_, :], lhsT=Cblk[:, jp, cv, 0:cw], rhs=X[q][:], start=(first and j != 3), stop=(last and j != 2)),
                 ["Cblk", XK(q)], [PS(yb)] if (first and j == 0) else [], wr_late=[] if (first and j == 0) else [PS(yb)])

    def evac_tile(i):
        for tc in (2, 3):
            yb = 4 + (tc - 2)
            os_ = slice((tc - 2) * 512, (tc - 1) * 512)
            dv(lambda e, i=i, tc=tc, yb=yb: e.scalar_tensor_tensor(out=ytmp[:], in0=uT[:, i, tc * 512:(tc + 1) * 512], scalar=dg[:, i:i + 1], in1=banks[yb][:], op0=ALU.mult, op1=ALU.add),
               ["uT", "dg", PS(yb)], ["ytmp"])
            ac(lambda e, i=i, os_=os_: e.activation(out=ygf[:, i, os_], in_=ytmp[:], func=AF.Gelu), ["ytmp"], [("ygf", i)])
            k.op("pool", lambda e, i=i, os_=os_: e.tensor_copy(out=ygb[:, i, os_], in_=ygf[:, i, os_]), [("ygf", i)], ["ygb"])

    NCH = 128
    emit_tables(0)
    emit_bu(0)
    for m_ in demod_ops(0):
        m_()
    pool_adds(0)
    for c in range(NCH):
        jp, tc, i, j, pp, pb = cpar(c)
        if tc == 1 and jp + 1 < 32:
            emit_tables(jp + 1)
        nxt = c + 1 < NCH
        if nxt:
            emit_bu(c + 1)
        C_ = carry_ops(c)
        M_ = demod_ops(c + 1) if nxt else []
        for q in range(4):
            if q < len(C_):
                C_[q]()
            if q < len(M_):
                M_[q]()
        if nxt:
            pool_adds(c + 1)
        scans(c)
        if tc >= 2:
            remod(c)
        if tc == 3 and j == 3:
            evac_tile(i)
    for fo in range(8):
        for half in range(2):
            bk = half
            hs = slice(half * 512, (half + 1) * 512)
            for kc in range(8):
                k.op("pe", lambda e, fo=fo, kc=kc, hs=hs, bk=bk: e.matmul(out=banks[bk][:], lhsT=gw[:, kc, fo * 128:(fo + 1) * 128], rhs=ygb[:, kc, hs], start=(kc == 0), stop=(kc == 7)),
                     ["gw", "ygb"], [PS(bk)] if kc == 0 else [], wr_late=[PS(bk)] if kc else [])
            ac(lambda e, fo=fo, bk=bk: e.activation(out=sg[:], in_=banks[bk][:], func=AF.Sigmoid, bias=dg[:, 8 + fo:9 + fo]), [PS(bk), "dg"], ["sg"])
            dv(lambda e, fo=fo, hs=hs: e.tensor_tensor(out=ygf[:, fo, hs], in0=ygf[:, fo, hs], in1=sg[:], op=ALU.mult), [("ygf", fo), "sg"], [("ygf", fo)])
    k.flush()
    if dbg and stop == "ssm":
        dbg_dump("ygf", ygf[:], [128, 8, 1024], ("ygf", 0), F32)
    out_norm(ygf, 8, lambda c8: ("ygf", c8))
    k.flush()
    if dbg and stop == "ssm":
        dbg_dump("mixedT", mixedT[:], [128, 16, 1024], "mixedT", BF16)
    if stop == "ssm":
        k.flush()
        return nc

    w_out = din("w_out", [2048, 2048])
    RXR = Region(40 * KB_, 104 * KB_)
    x_res = k.sb("x_res", [128, 8, D], F32, RXR)
    RW = Region(104 * KB_, 207 * KB_)
    wob = [k.sb(f"wob{i}", [128, 16, 512], BF16, RW) for i in range(2)]
    xin = [k.sb(f"xin{i}", [128, 512], F32, RW) for i in range(2)]
    xc = 0
    for dc in range(4):
        ds_ = slice(dc * 512, (dc + 1) * 512)
        for hf in range(2):
            k.dma(wob[dc % 2][:, hf * 8:(hf + 1) * 8, :], w_out[hf * 1024:(hf + 1) * 1024, ds_].rearrange("(kc p) d -> p kc d", p=128), writes=[("wob", dc % 2, hf)], chan=f"wo{dc % 2}", eng="pool")
        for tt in range(8):
            bk = tt % 2
            xb_ = xc % 2
            xc += 1
            k.dma(xin[xb_][:], x_all[1024 + tt * 128: 1024 + (tt + 1) * 128, ds_], writes=[("xin", xb_)], chan=f"xi{xb_}")
            for kc in range(16):
                k.op("pe", lambda e, kc=kc, tt=tt, bk=bk, dc=dc: e.matmul(out=banks[bk][:], lhsT=mixedT[:, kc, tt * 128:(tt + 1) * 128], rhs=wob[dc % 2][:, kc, :], start=(kc == 0), stop=(kc == 15)),
                     ["mixedT", ("wob", dc % 2, kc // 8)], [PS(bk)] if kc == 0 else [], wr_late=[PS(bk)] if kc else [])
            k.op("dve", lambda e, tt=tt, ds_=ds_, bk=bk, xb_=xb_: e.tensor_tensor(out=x_res[:, tt, ds_], in0=banks[bk][:], in1=xin[xb_][:], op=ALU.add),
                 [PS(bk), ("xin", xb_)], [("x_res", tt)])
    k.flush()
    if dbg and stop == "x1":
        dbg_dump("x1", x_res[:], [128, 8, D], ("x_res", 0), F32)
    if stop == "x1":
        k.flush()
        return nc

    gffn = din("gffn", [128, 16])
    wq_t = din("wq_t", [16, 128, 16, 128])
    keysT = din("keysT", [128, 16, 128])
    U_t = din("U_t", [128, 128, 16, 128])
    Vp = din("Vp", [16384, D])
    gffn_sb = k.sb("gffn_sb", [128, 16], F32, RC)
    k.dma(gffn_sb[:], gffn[:, :], writes=["gffn"], chan="c5")
    RS = Region(8 * KB_, 40 * KB_)
    s12 = k.sb("s12", [128, 4, 2, 8, 128], F32, RS)
    for hh_ in range(2):
        RP2 = Region(104 * KB_, 207 * KB_)
        hn2T = k.sb("hn2T", [128, 16, 512], BF16, RP2)
        Dm = k.sb("Dm", [128, 4, 8, 128], BF16, RP2)
        tstat = k.sb("tstat", [128, 4, 8, 4], F32, RP2)
        mark = RP2.top
        qpT = k.sb("qpT", [128, 16, 512], BF16, RP2)
        wbf2 = [k.sb(f"wbf2{i}", [128, 16, 128], BF16, RP2) for i in range(4)]
        hnb2 = k.sb("hnb2", [128, D], BF16, RP2)
        keysb = k.sb("keysb", [128, 16, 128], BF16, RP2)
        st2 = k.sb("st2", [128, 4], F32, RP2)
        m16 = [k.sb(f"m16_{q}", [128, 2, 16], F32, RP2) for q in range(4)]
        c24 = [k.sb(f"c24_{q}", [128, 24], F32, RP2) for q in range(4)]
        tmp1 = [k.sb(f"tmp1_{q}", [128, 128], F32, RP2) for q in range(4)]
        cand = [k.sb(f"cand_{q}", [128, 16, 16], F32, RP2) for q in range(4)]
        cand2 = [k.sb(f"cand2_{q}", [128, 256], F32, RP2) for q in range(4)]
        ejunk = [k.sb(f"ejunk_{q}", [128, 16], F32, RP2) for q in range(4)]
        k.dma(keysb[:], keysT[:, :, :], writes=["keysb"], chan="p0", eng="pool")
        for tt in range(4):
            tg = hh_ * 4 + tt
            sc = st2[:, tt:tt + 1]
            sk = ("st2", tt)
            k.op("act", lambda e, tg=tg, sc=sc: e.activation(out=hnb2[:], in_=x_res[:, tg, :], func=AF.Square, accum_out=sc), [("x_res", tg)], ["hnb2", sk])
            k.op("dve", lambda e, sc=sc: e.tensor_scalar(out=sc, in0=sc, scalar1=1.0 / D, scalar2=EPS, op0=ALU.mult, op1=ALU.add), [sk], [sk])
            k.op("act", lambda e, sc=sc: e.activation(out=sc, in_=sc, func=AF.Sqrt), [sk], [sk])
            k.op("dve", lambda e, sc=sc: e.reciprocal(out=sc, in_=sc), [sk], [sk])
            k.op("dve", lambda e, tg=tg, sc=sc: e.tensor_scalar(out=hnb2[:], in0=x_res[:, tg, :], scalar1=sc, scalar2=None, op0=ALU.mult), [("x_res", tg), sk], ["hnb2"])
            for hf in range(2):
                bk = hf
                pv = banks[bk][:].bitcast(BF16)
                for j in range(8):
                    kc = hf * 8 + j
                    k.op("pe", lambda e, kc=kc, j=j, pv=pv: e.transpose(out=pv[:, j * 128:(j + 1) * 128], in_=hnb2[:, kc * 128:(kc + 1) * 128], identity=identb[:]),
                         ["hnb2", "identb"], [PS(bk)] if j == 0 else [], wr_late=[PS(bk)] if j else [])
                for j in range(8):
                    kc = hf * 8 + j
                    k.op("act", lambda e, kc=kc, j=j, tt=tt, pv=pv: e.activation(out=hn2T[:, kc, tt * 128:(tt + 1) * 128], in_=pv[:, j * 128:(j + 1) * 128], func=AF.Copy, scale=gffn_sb[:, kc:kc + 1]),
                         [PS(bk), "gffn"], ["hn2T"])
        for fc in range(16):
            wb = fc % 4
            k.dma(wbf2[wb][:], wq_t[fc], writes=[("wbf2", wb)], chan=f"wq{wb}", eng="pool")
            bk = 2 + fc % 2
            for kc in range(16):
                k.op("pe", lambda e, wb=wb, kc=kc, bk=bk: e.matmul(out=banks[bk][:], lhsT=wbf2[wb][:, kc, :], rhs=hn2T[:, kc, :], start=(kc == 0), stop=(kc == 15)),
                     [("wbf2", wb), "hn2T"], [PS(bk)] if kc == 0 else [], wr_late=[PS(bk)] if kc else [])
            k.op("act", lambda e, fc=fc, bk=bk: e.activation(out=qpT[:, fc, :], in_=banks[bk][:], func=AF.Copy), [PS(bk)], ["qpT"])
        for tt in range(4):
            for half in range(2):
                for hq in range(2):
                    bk = 4 + (2 * half + hq) % 4
                    for h4 in range(4):
                        h = hq * 4 + h4
                        k.op("pe", lambda e, tt=tt, half=half, h=h, h4=h4, bk=bk: e.matmul(out=banks[bk][:, h4 * 128:(h4 + 1) * 128], lhsT=qpT[:, 2 * h + half, tt * 128:(tt + 1) * 128], rhs=keysb[:, half * 8 + h, :], start=True, stop=True),
                             ["qpT", "keysb"], [PS(bk)] if h4 == 0 else [], wr_late=[PS(bk)] if h4 else [])
                    k.op("act", lambda e, tt=tt, half=half, hq=hq, bk=bk: e.activation(out=s12[:, tt, half, hq * 4:(hq + 1) * 4, :], in_=banks[bk][:].rearrange("p (a b) -> p a b", a=4), func=AF.Copy),
                         [PS(bk)], [("s12", tt)])
            for hq in range(2):
                hs4 = [hq * 4 + q for q in range(4)]
                for half in range(2):
                    svs = [s12[:, tt, half, h, :] for h in hs4]
                    for q in range(4):
                        dv(lambda e, half=half, q=q, sv=svs[q]: e.max(out=m16[q][:, half, 0:8], in_=sv), [("s12", tt)], [("m16", q, half)])
                    for q in range(4):
                        dv(lambda e, half=half, q=q, sv=svs[q]: e.match_replace(out=tmp1[q][:], in_to_replace=m16[q][:, half, 0:8], in_values=sv, imm_value=-1e30),
                           [("s12", tt), ("m16", q, half)], [("tmp1", q)])
                    for q in range(4):
                        dv(lambda e, half=half, q=q: e.max(out=m16[q][:, half, 8:16], in_=tmp1[q][:]), [("tmp1", q)], [("m16", q, half)])
                for q in range(4):
                    dv(lambda e, q=q: e.tensor_tensor(out=cand[q][:], in0=m16[q][:, 0, :].unsqueeze(2).to_broadcast([128, 16, 16]), in1=m16[q][:, 1:2, :].to_broadcast([128, 16, 16]), op=ALU.add),
                       [("m16", q, 0), ("m16", q, 1)], [("cand", q)])
                for q in range(4):
                    dv(lambda e, q=q: e.max(out=c24[q][:, 0:8], in_=cand[q][:].rearrange("p a b -> p (a b)")), [("cand", q)], [("c24", q)])
                for q in range(4):
                    dv(lambda e, q=q: e.match_replace(out=cand2[q][:], in_to_replace=c24[q][:, 0:8], in_values=cand[q][:].rearrange("p a b -> p (a b)"), imm_value=-1e30), [("cand", q), ("c24", q)], [("cand2", q)])
                for q in range(4):
                    dv(lambda e, q=q: e.max(out=c24[q][:, 8:16], in_=cand2[q][:]), [("cand2", q)], [("c24", q)])
                for q in range(4):
                    dv(lambda e, q=q: e.match_replace(out=cand2[q][:], in_to_replace=c24[q][:, 8:16], in_values=cand2[q][:], imm_value=-1e30), [("cand2", q), ("c24", q)], [("cand2", q)])
                for q in range(4):
                    dv(lambda e, q=q: e.max(out=c24[q][:, 16:24], in_=cand2[q][:]), [("cand2", q)], [("c24", q)])
                for q, h in enumerate(hs4):
                    ts0 = tstat[:, tt, h, 0:1]
                    dv(lambda e, ts0=ts0, q=q: e.tensor_tensor(out=ts0, in0=c24[q][:, 15:16], in1=c24[q][:, 16:17], op=ALU.add), [("c24", q)], [("tstat", tt, h)])
                for q, h in enumerate(hs4):
                    ts0 = tstat[:, tt, h, 0:1]
                    dv(lambda e, ts0=ts0: e.tensor_scalar(out=ts0, in0=ts0, scalar1=0.5, scalar2=None, op0=ALU.mult), [("tstat", tt, h)], [("tstat", tt, h)])
                for q, h in enumerate(hs4):
                    ts1 = tstat[:, tt, h, 1:2]
                    ac(lambda e, ts1=ts1, q=q: e.activation(out=ejunk[q][:], in_=c24[q][:, 0:16], func=AF.Exp, accum_out=ts1), [("c24", q)], [("ejunk", q), ("tstat1", tt, h)])
                for q, h in enumerate(hs4):
                    ts0 = tstat[:, tt, h, 0:1]
                    ts3 = tstat[:, tt, h, 3:4]
                    ac(lambda e, ts0=ts0, ts3=ts3: e.activation(out=ts3, in_=ts0, func=AF.Exp), [("tstat", tt, h)], [("tstat3", tt, h)])
                for q, h in enumerate(hs4):
                    ts1 = tstat[:, tt, h, 1:2]
                    ts2 = tstat[:, tt, h, 2:3]
                    dv(lambda e, ts1=ts1, ts2=ts2: e.reciprocal(out=ts2, in_=ts1), [("tstat1", tt, h)], [("tstat2", tt, h)])
                for q, h in enumerate(hs4):
                    ts2 = tstat[:, tt, h, 2:3]
                    dv(lambda e, tt=tt, h=h, ts2=ts2: e.tensor_scalar(out=Dm[:, tt, h, :], in0=identb[:], scalar1=ts2, scalar2=None, op0=ALU.mult), ["identb", ("tstat2", tt, h)], ["Dm"])
        k.flush()
        if dbg and stop == "score" and hh_ == 0:
            dbg_dump("s12", s12[:], [128, 4, 2, 8, 128], ("s12", 0), F32)
            dbg_dump("tstat", tstat[:], [128, 4, 8, 4], ("tstat", 0, 0), F32)
            k.flush()
            return nc
        RP2.top = mark
        NPB = 6
        ub = [k.sb(f"ub{i}", [128, 16, 128], BF16, RP2) for i in range(2)]
        ga = [k.sb(f"ga{i}", [128, 4, 512], BF16, RP2) for i in range(2)]
        vbb = [k.sb(f"vbb{i}", [128, D], BF16, RP2) for i in range(8)]
        pexp = [k.sb(f"pexp{i}", [128, 4, 128], F32, RP2) for i in range(NPB)]
        gq = [k.sb(f"gq{i}", [128, 4, 128], BF16, RP2) for i in range(NPB)]
        gsum = [k.sb(f"gsum{i}", [128, 4, 128], BF16, RP2) for i in range(2)]
        wT = [k.sb(f"wT{i}", [128, 4, 128], BF16, RP2) for i in range(2)]
        a_st = k.sb("a_st", [128, 4, 512], F32, RP2)
        cnt = {"st": 0, "a": 0, "g": 0, "w": 0, "p": 0}

        def load_chunk(g, s_):
            e1 = g * 4 + s_
            sl = (g % 2) * 4 + s_
            k.dma(ub[s_ % 2][:], U_t[e1], writes=[("ub", s_ % 2)], chan=f"pu{s_ % 2}", eng="pool")
            k.dma(vbb[sl][:], Vp[e1 * 128:(e1 + 1) * 128, :], writes=[("vbb", sl)], chan=f"pv{sl}", eng="pool")

        def act_mm(g, s_, kcs, bk):
            for kc in kcs:
                k.op("pe", lambda e, s_=s_, kc=kc, bk=bk: e.matmul(out=banks[bk][:], lhsT=ub[s_ % 2][:, kc, :], rhs=hn2T[:, kc, :], start=(kc == 0), stop=(kc == 15)),
                     [("ub", s_ % 2), "hn2T"], [PS(bk)] if kc == 0 else [], wr_late=[PS(bk)] if kc else [])

        def act_gelu(g, s_, bk):
            par = g % 2
            dv(lambda e, s_=s_, bk=bk: e.tensor_copy(out=a_st[:, s_, :], in_=banks[bk][:]), [PS(bk)], [("a_st", s_)])
            if s_ == 3:
                ac(lambda e, par=par: e.activation(out=ga[par][:].rearrange("p a b -> p (a b)"), in_=a_st[:].rearrange("p a b -> p (a b)"), func=AF.Gelu),
                   [("a_st", q) for q in range(4)], [("ga", par, q) for q in range(4)])

        def act_chunk(g, s_):
            bk = cnt["a"] % 2
            cnt["a"] += 1
            act_mm(g, s_, range(16), bk)
            act_gelu(g, s_, bk)

        gb_ = 2
        pvT = banks[3][:].bitcast(BF16)

        def stage_a(g, tt, pre=None, adds=None):
            if pre is not None:
                bk = cnt["a"] % 2
                cnt["a"] += 1
            for h in range(8):
                pi = cnt["p"] % NPB
                cnt["p"] += 1
                for s_ in range(4):
                    e1 = g * 4 + s_
                    ac(lambda e, tt=tt, h=h, e1=e1, pi=pi, s_=s_: e.activation(out=pexp[pi][:, s_, :], in_=s12[:, tt, 1, h, :], func=AF.Exp, bias=s12[:, tt, 0, h, e1:e1 + 1]),
                       [("s12", tt)], [("pexp", pi, s_)])
                dv(lambda e, tt=tt, h=h, pi=pi: e.scalar_tensor_tensor(out=gq[pi][:], in0=pexp[pi][:], scalar=tstat[:, tt, h, 3:4], in1=pexp[pi][:], op0=ALU.is_ge, op1=ALU.mult),
                   [("tstat3", tt, h)] + [("pexp", pi, s_) for s_ in range(4)], [("gq", pi)])
                k.op("pe", lambda e, tt=tt, h=h, pi=pi: e.matmul(out=banks[gb_][:], lhsT=Dm[:, tt, h, :], rhs=gq[pi][:].rearrange("p a b -> p (a b)"), start=(h == 0), stop=(h == 7)),
                     [("gq", pi), "Dm"], [PS(gb_)] if h == 0 else [], wr_late=[PS(gb_)] if h else [])
                if pre is not None:
                    act_mm(pre[0], pre[1], (2 * h, 2 * h + 1), bk)
                if adds is not None and h >= 4:
                    stage_c(adds[0], adds[1], dcs=(h - 4,))
            if pre is not None:
                act_gelu(pre[0], pre[1], bk)

        def stage_b(g, tt):
            par = g % 2
            gi = cnt["g"] % 2
            cnt["g"] += 1
            ac(lambda e, gi=gi: e.activation(out=gsum[gi][:].rearrange("p a b -> p (a b)"), in_=banks[gb_][:], func=AF.Copy), [PS(gb_)], [("gsum", gi)])
            for s_ in range(4):
                k.op("pe", lambda e, gi=gi, s_=s_: e.transpose(out=pvT[:, s_ * 128:(s_ + 1) * 128], in_=gsum[gi][:, s_, :], identity=identb[:]),
                     [("gsum", gi), "identb"], [PS(3)] if s_ == 0 else [], wr_late=[PS(3)] if s_ else [])
            wi = cnt["w"] % 2
            cnt["w"] += 1
            dv(lambda e, par=par, tt=tt, wi=wi: e.tensor_tensor(out=wT[wi][:], in0=pvT[:, 0:512].rearrange("p (a b) -> p a b", a=4), in1=ga[par][:, :, tt * 128:(tt + 1) * 128], op=ALU.mult),
               [PS(3)] + [("ga", par, s_) for s_ in range(4)], [("wT", wi)])
            for s_ in range(4):
                sl = par * 4 + s_
                for dc in range(4):
                    k.op("pe", lambda e, sl=sl, s_=s_, dc=dc, wi=wi: e.matmul(out=banks[4 + dc][:], lhsT=wT[wi][:, s_, :], rhs=vbb[sl][:, dc * 512:(dc + 1) * 512], start=(s_ == 0), stop=(s_ == 3)),
                         [("wT", wi), ("vbb", sl)], [PS(4 + dc)] if s_ == 0 else [], wr_late=[PS(4 + dc)] if s_ else [])

        def stage_c(g, tt, dcs=range(4)):
            tg = hh_ * 4 + tt
            for dc in dcs:
                ds_ = slice(dc * 512, (dc + 1) * 512)
                dv(lambda e, tg=tg, dc=dc, ds_=ds_: e.tensor_tensor(out=x_res[:, tg, ds_], in0=banks[4 + dc][:], in1=x_res[:, tg, ds_], op=ALU.add),
                   [PS(4 + dc), ("x_res", tg)], [("x_res", tg)])

        for s_ in range(4):
            load_chunk(0, s_)
            act_chunk(0, s_)
        its = [(g, tt) for g in range(32) for tt in range(4)]
        stage_a(*its[0])
        for n_, (g, tt) in enumerate(its):
            pre = (g + 1, tt) if g + 1 < 32 else None
            if pre is not None:
                load_chunk(g + 1, tt)
            stage_b(g, tt)
            if n_ + 1 < len(its):
                stage_a(*its[n_ + 1], pre=pre, adds=(g, tt))
            else:
                stage_c(g, tt)
        k.flush()
    for tt in range(8):
        k.dma(y[tt * 128:(tt + 1) * 128, :], x_res[:, tt, :], reads=[("x_res", tt)], writes=[("y", tt)], chan="yo")
    k.flush()
    return nc


def _static_tables():
    i = np.arange(GW)
    d = i - 127
    n = np.maximum(d, 0)
    nf = np.maximum(n, 1).astype(np.float32)
    large = 16 + (np.log(nf / np.float32(16)) / np.float32(math.log(2048 / 16)) * np.float32(16)).astype(np.int32)
    large = np.minimum(large, 31)
    bucket = np.where(n < 16, n, large)
    mult = ((d <= 128).astype(np.float32) + ((d % 4 == 0) & (d <= 512)) + ((d % 16 == 0) & (d <= 2048)))
    mult = np.where((d >= 0) & (d <= 2047), mult, 0.0).astype(np.float32)
    ohm = np.zeros((32, GW), np.float32)
    ohm[bucket, i] = mult
    cm = np.zeros((4, 128, 128), np.float32)
    cm[0] = np.eye(128)
    cm[1] = np.eye(128)[::-1]
    cm[2, :64, :64] = 1.0
    cm[2, 64:, 64:] = 1.0
    cm[3] = 1.0
    return ohm, cm


def _pcol(v, n):
    return np.ascontiguousarray(np.asarray(v, np.float32).reshape(n, 128).T)


def _prep(inp, stage=None):
    f = lambda a: np.ascontiguousarray(np.asarray(a, dtype=np.float32))
    x = f(inp["x"])
    ohm, cm = _static_tables()
    w_in_t = np.ascontiguousarray(f(inp["w_in"]).reshape(16, 128, 32, 128).transpose(2, 1, 0, 3))
    gq = np.tile(f(inp["q_norm_g"]), 2)
    gk = np.tile(f(inp["k_norm_g"]), 2)
    shared = {
        "gmix": _pcol(inp["norm_mix_g"], 16),
        "w_in_t": w_in_t,
        "gqk": np.ascontiguousarray(np.stack([gq, gk], axis=1)),
        "relb": f(inp["rel_bias"]),
        "ohm": ohm,
        "cmats": cm,
        "gmixed": np.ascontiguousarray(np.concatenate([_pcol(inp["attn_out_g"], 8), _pcol(inp["ssm_out_g"], 8)], axis=1)),
    }
    def lane(a):
        a = f(a)
        rest = a.shape[2:]
        return np.ascontiguousarray(a.reshape((32, 2, 64) + rest).transpose((1, 2, 0) + tuple(range(3, 3 + len(rest)))).reshape((128, 32) + rest))
    ldt = np.broadcast_to(f(inp["ssm_log_dt"])[:, None], (64, 64))
    shared["lam_l"] = np.ascontiguousarray(np.stack([lane(inp["ssm_lambda_re"]), lane(inp["ssm_lambda_im"]), lane(ldt)]))
    shared["bc_l"] = np.ascontiguousarray(np.stack([lane(inp["ssm_b_re"]), lane(inp["ssm_b_im"]),
                                                    lane(f(inp["ssm_c_re"]).transpose(0, 2, 1)), lane(f(inp["ssm_c_im"]).transpose(0, 2, 1))]))
    shared["dg_l"] = np.ascontiguousarray(np.concatenate([_pcol(f(inp["ssm_d"]).reshape(-1), 8), _pcol(inp["ssm_glu_b"], 8)], axis=1))
    shared["glu_w"] = f(inp["ssm_glu_w"])
    shared["tidx"] = np.ascontiguousarray(np.broadcast_to(np.arange(512, dtype=np.float32)[None, :], (128, 512)))
    shared["w_out"] = f(inp["w_out"])
    shared["gffn"] = _pcol(inp["norm_ffn_g"], 16)
    shared["wq_t"] = np.ascontiguousarray(f(inp["peer_w_q"]).reshape(16, 128, 16, 128).transpose(2, 1, 0, 3))
    k1 = f(inp["peer_keys1"]).transpose(2, 0, 1)
    k2 = f(inp["peer_keys2"]).transpose(2, 0, 1)
    shared["keysT"] = np.ascontiguousarray(np.concatenate([k1, k2], axis=1))
    shared["U_t"] = np.ascontiguousarray(f(inp["peer_u"]).reshape(128, 128, 16, 128).transpose(0, 3, 2, 1))
    shared["Vp"] = f(inp["peer_v"])
    maps = []
    for c in range(8):
        b, p = c // 2, c % 2
        xa = np.zeros((2048, D), np.float32)
        if p == 1:
            xa[:] = x[b]
        else:
            xa[1024:] = x[b, :1024]
        kv = np.ones((128, 16), np.float32)
        if p == 0:
            kv[:, :8] = 0.0
        m = dict(shared)
        m["x_all"] = xa
        m["kvalid"] = kv
        maps.append(m)
    return maps


_NC_CACHE = {}


def kernel(**inputs):
    if "nc" not in _NC_CACHE:
        _NC_CACHE["nc"] = build()
    nc = _NC_CACHE["nc"]
    maps = _prep(inputs)
    res = run_bass_kernel_spmd(nc, maps, core_ids=list(range(8)))
    out = np.zeros((4, 2048, D), np.float32)
    for c in range(8):
        b, p = c // 2, c % 2
        out[b, p * 1024:(p + 1) * 1024] = res.results[c]["y"]
    return out
```

```python
import math
from contextlib import ExitStack

import numpy as np
import concourse.bass as bass
import concourse.mybir as mybir
from concourse.bass_utils import run_bass_kernel_spmd

F32 = mybir.dt.float32
BF16 = mybir.dt.bfloat16
I32 = mybir.dt.int32
AF = mybir.ActivationFunctionType
ALU = mybir.AluOpType

D = 2048
EPS = 1e-6
GW = 2560


class _Res:
    __slots__ = ("w", "rs")

    def __init__(self):
        self.w = None
        self.rs = {}


class Region:
    def __init__(self, lo, hi):
        self.lo, self.hi, self.top = lo, hi, lo

    def take(self, nbytes, name=""):
        off = self.top
        self.top += nbytes
        assert self.top <= self.hi, f"SBUF region overflow at {name}: {self.top} > {self.hi}"
        return off


KBYTE = 1024
SB0 = 16512


class KB:
    ENG = ("pe", "act", "dve", "pool", "sp")

    def __init__(self, nc):
        self.nc = nc
        self.streams = {e: [] for e in self.ENG}
        self.sems = {}
        self.cnt = {}
        self.waited = {e: {} for e in self.ENG}
        self.res = {}
        self.es = ExitStack()

    def sb(self, name, shape, dtype, region):
        nbytes = int(np.prod(shape[1:])) * (2 if dtype == BF16 else 4)
        nbytes = (nbytes + 63) // 64 * 64
        off = region.take(nbytes, name) + SB0
        self.nalloc = getattr(self, "nalloc", 0) + 1
        return self.nc.alloc_sbuf_tensor_at(f"{name}_{self.nalloc}", list(shape), dtype, offset=off)

    def ps(self, name, shape, dtype, stack=None):
        return (stack or self.es).enter_context(self.nc.psum_tensor(name, list(shape), dtype))

    def _sem(self, name):
        if name not in self.sems:
            self.sems[name] = self.es.enter_context(self.nc.semaphore(name))
            self.cnt[name] = 0
        return name

    def _deps(self, eng, reads, writes):
        need = {}

        def add(s, v):
            if need.get(s, 0) < v:
                need[s] = v

        for r in reads:
            rr = self.res.get(r)
            if rr is not None and rr.w is not None:
                add(*rr.w)
        for w in writes:
            rr = self.res.get(w)
            if rr is not None:
                if rr.w is not None:
                    add(*rr.w)
                for s, v in rr.rs.items():
                    add(s, v)
        out = []
        wd = self.waited[eng]
        for s, v in need.items():
            if eng == "pe" and s == "e_pe":
                continue
            if wd.get(s, 0) < v:
                wd[s] = v
                out.append((s, v))
        return out

    def op(self, eng, fn, reads=(), writes=(), sem=None, inc=1, wr_late=()):
        waits = self._deps(eng, reads, writes)
        s = self._sem(sem or ("e_" + eng))
        self.cnt[s] += inc
        ev = (s, self.cnt[s])
        self.streams[eng].append((waits, fn, s, inc))
        for r in reads:
            rr = self.res.setdefault(r, _Res())
            if rr.rs.get(ev[0], 0) < ev[1]:
                rr.rs[ev[0]] = ev[1]
        for w in tuple(writes) + tuple(wr_late):
            rr = self.res.setdefault(w, _Res())
            rr.w = ev
            rr.rs = {}
        return ev

    def dma(self, out, in_, reads=(), writes=(), chan="d0", eng="sp", **kw):
        return self.op(eng, lambda e: e.dma_start(out=out, in_=in_, **kw), reads, writes,
                       sem="dma_" + chan, inc=16)

    def wait_all(self, eng, keys):
        waits = self._deps(eng, keys, ())
        self.streams[eng].append((waits, None, None, 0))

    def flush(self):
        for eng in self.ENG:
            waits = []
            wd = self.waited[eng]
            for s, v in self.cnt.items():
                if v > 0 and wd.get(s, 0) < v:
                    wd[s] = v
                    waits.append((s, v))
            self.streams[eng].append((waits, None, None, 0))
        with self.nc.Block() as block:
            def mk(stream):
                def body(e):
                    for waits, fn, s, inc in stream:
                        for ws, wv in waits:
                            e.wait_ge(self.sems[ws], wv)
                        if fn is not None:
                            fn(e).then_inc(self.sems[s], inc)
                return body
            block.tensor(mk(self.streams["pe"]))
            block.scalar(mk(self.streams["act"]))
            block.vector(mk(self.streams["dve"]))
            block.gpsimd(mk(self.streams["pool"]))
            block.sync(mk(self.streams["sp"]))
        self.streams = {e: [] for e in self.ENG}


def build(stop=None, dbg=False):
    nc = bass.Bass("TRN2", target_bir_lowering=False)
    k = KB(nc)
    KB_ = KBYTE

    def din(name, shape, dt=F32):
        return nc.dram_tensor(name, list(shape), dt, kind="ExternalInput").ap()

    def dout(name, shape, dt=F32):
        return nc.dram_tensor(name, list(shape), dt, kind="ExternalOutput").ap()

    x_all = din("x_all", [2048, D])
    gmix = din("gmix", [128, 16])
    w_in_t = din("w_in_t", [32, 128, 16, 128])
    gqk = din("gqk", [128, 2])
    relb = din("relb", [32, 16])
    ohm = din("ohm", [32, GW])
    kvalid = din("kvalid", [128, 16])
    cmats = din("cmats", [4, 128, 128])
    gmixed = din("gmixed", [128, 16])
    gsc = nc.dram_tensor("gsc", [16, GW], BF16, kind="Internal").ap()
    y = dout("y", [1024, D])

    def dbg_dump(name, src_ap, shape, key, dt=F32):
        if not dbg:
            return
        o = dout("dbg_" + name, shape, dt)
        k.dma(o, src_ap, reads=[key], writes=["dbg_" + name], chan="dbg")

    banks = [k.ps(f"bank{i}", [128, 512], F32) for i in range(8)]
    PS = lambda i: ("ps", i)

    RC = Region(0, 8 * KB_)
    cm = k.sb("cm", [128, 4, 128], F32, RC)
    k.dma(cm[:], cmats.rearrange("c p j -> p c j"), writes=["cm"], chan="c0")
    identb = k.sb("identb", [128, 128], BF16, RC)
    antib = k.sb("antib", [128, 128], BF16, RC)
    k.op("dve", lambda e: e.tensor_copy(out=identb[:], in_=cm[:, 0, :]), ["cm"], ["identb"])
    k.op("dve", lambda e: e.tensor_copy(out=antib[:], in_=cm[:, 1, :]), ["cm"], ["antib"])
    blk64 = cm[:, 2, :]
    onesf = cm[:, 3, :]
    gqk_sb = k.sb("gqk_sb", [128, 2], F32, RC)
    k.dma(gqk_sb[:], gqk[:, :], writes=["gqk"], chan="c1")
    kv_sb = k.sb("kv_sb", [128, 16], F32, RC)
    k.dma(kv_sb[:], kvalid[:, :], writes=["kv"], chan="c2")
    gmixed_sb = k.sb("gmixed_sb", [128, 16], F32, RC)
    k.dma(gmixed_sb[:], gmixed[:, :], writes=["gmixed"], chan="c3")
    gmix_sb = k.sb("gmix_sb", [128, 16], F32, RC)
    k.dma(gmix_sb[:], gmix[:, :], writes=["gmix"], chan="c4")

    RM = Region(8 * KB_, 120 * KB_)
    qT = k.sb("qT", [128, 8, 1024], BF16, RM)
    kT = k.sb("kT", [128, 8, 2048], BF16, RM)
    Vt = k.sb("Vt", [128, 16, 1024], BF16, RM)
    uT = k.sb("uT", [128, 8, 2048], BF16, RM)

    R = Region(120 * KB_, 207 * KB_)
    hnT2 = [k.sb(f"hnT{i}", [128, 16, 512], BF16, R) for i in range(2)]
    xbuf = [k.sb(f"xbuf{i}", [128, D], F32, R) for i in range(2)]
    hnb = [k.sb(f"hnb{i}", [128, D], BF16, R) for i in range(2)]
    stat = k.sb("stat", [128, 16], F32, R)
    wbf = [k.sb(f"wbf{i}", [128, 16, 128], BF16, R) for i in range(4)]
    sqb = [k.sb(f"sqb{i}", [128, 512], F32, R) for i in range(2)]
    rsb = [k.sb(f"rsb{i}", [128, 512], F32, R) for i in range(2)]
    pc_ = {"w": 0, "p": 0, "x": 0}

    def norm_tile(ps_, tt):
        hnT = hnT2[ps_ % 2]
        hk = ("hnT", ps_ % 2)
        b = pc_["x"] % 2
        pc_["x"] += 1
        xi = pc_["x"] % 16
        r0 = ps_ * 512 + tt * 128
        sk = ("stat", xi)
        sc = stat[:, xi: xi + 1]
        k.dma(xbuf[b][:], x_all[r0:r0 + 128, :], writes=[("xb", b)], chan=f"x{b}")
        k.op("act", lambda e: e.activation(out=hnb[b][:], in_=xbuf[b][:], func=AF.Square, accum_out=sc), [("xb", b)], [("hnb", b), sk])
        k.op("dve", lambda e: e.tensor_scalar(out=sc, in0=sc, scalar1=1.0 / D, scalar2=EPS, op0=ALU.mult, op1=ALU.add), [sk], [sk])
        k.op("act", lambda e: e.activation(out=sc, in_=sc, func=AF.Sqrt), [sk], [sk])
        k.op("dve", lambda e: e.reciprocal(out=sc, in_=sc), [sk], [sk])
        k.op("dve", lambda e: e.tensor_scalar(out=hnb[b][:], in0=xbuf[b][:], scalar1=sc, scalar2=None, op0=ALU.mult), [("xb", b), sk], [("hnb", b)])
        for hf in range(2):
            bk = hf
            pv = banks[bk][:].bitcast(BF16)
            for j in range(8):
                kc = hf * 8 + j
                k.op("pe", lambda e, kc=kc, j=j, pv=pv: e.transpose(out=pv[:, j * 128:(j + 1) * 128], in_=hnb[b][:, kc * 128:(kc + 1) * 128], identity=identb[:]),
                     [("hnb", b), "identb"], [PS(bk)] if j == 0 else [], wr_late=[PS(bk)] if j else [])
            for j in range(8):
                kc = hf * 8 + j
                k.op("act", lambda e, kc=kc, j=j, pv=pv: e.activation(out=hnT[:, kc, tt * 128:(tt + 1) * 128], in_=pv[:, j * 128:(j + 1) * 128],
                                                                 func=AF.Copy, scale=gmix_sb[:, kc:kc + 1]),
                     [PS(bk), "gmix"], [hk])

    def proj_chunk(ps_, fc):
        hnT = hnT2[ps_ % 2]
        hk = ("hnT", ps_ % 2)
        tok0 = ps_ * 512
        wb = pc_["w"] % 4
        pc_["w"] += 1
        k.dma(wbf[wb][:], w_in_t[fc], writes=[("wbf", wb)], chan=f"w{wb}", eng="pool")
        if fc < 16 or fc >= 24:
            bk = 2 + pc_["p"] % 2
            pb = pc_["p"] % 2
            pc_["p"] += 1
            for kc in range(16):
                k.op("pe", lambda e, kc=kc: e.matmul(out=banks[bk][:], lhsT=wbf[wb][:, kc, :], rhs=hnT[:, kc, :], start=(kc == 0), stop=(kc == 15)),
                     [("wbf", wb), hk], [PS(bk)] if kc == 0 else [], wr_late=[PS(bk)] if kc else [])
            if fc >= 24:
                dst = uT[:, fc - 24, tok0: tok0 + 512]
                k.op("act", lambda e: e.activation(out=dst, in_=banks[bk][:], func=AF.Copy), [PS(bk)], ["uT"])
                return
            sb_ = 4 + pb
            k.op("act", lambda e: e.activation(out=sqb[pb][:], in_=banks[bk][:], func=AF.Square), [PS(bk)], [("sqb", pb)])
            k.op("pe", lambda e: e.matmul(out=banks[sb_][:], lhsT=blk64, rhs=sqb[pb][:], start=True, stop=True), [("sqb", pb), "cm"], [PS(sb_)])
            k.op("dve", lambda e: e.tensor_scalar(out=rsb[pb][:], in0=banks[sb_][:], scalar1=1.0 / 64, scalar2=EPS, op0=ALU.mult, op1=ALU.add), [PS(sb_)], [("rsb", pb)])
            k.op("act", lambda e: e.activation(out=rsb[pb][:], in_=rsb[pb][:], func=AF.Sqrt), [("rsb", pb)], [("rsb", pb)])
            k.op("dve", lambda e: e.reciprocal(out=rsb[pb][:], in_=rsb[pb][:]), [("rsb", pb)], [("rsb", pb)])
            if fc < 8:
                dst = qT[:, fc, tok0 - 1024: tok0 - 512]
                g = gqk_sb[:, 0:1]
                dk = "qT"
            else:
                dst = kT[:, fc - 8, tok0: tok0 + 512]
                g = gqk_sb[:, 1:2]
                dk = "kT"
            k.op("dve", lambda e: e.scalar_tensor_tensor(out=dst, in0=banks[bk][:], scalar=g, in1=rsb[pb][:], op0=ALU.mult, op1=ALU.mult),
                 [PS(bk), ("rsb", pb), "gqk"], [dk])
        else:
            for tt in range(4):
                bk = 6 + tt % 2
                for kc in range(16):
                    k.op("pe", lambda e, kc=kc, tt=tt, bk=bk: e.matmul(out=banks[bk][:, 0:128], lhsT=hnT[:, kc, tt * 128:(tt + 1) * 128], rhs=wbf[wb][:, kc, :], start=(kc == 0), stop=(kc == 15)),
                         [("wbf", wb), hk], [PS(bk)] if kc == 0 else [], wr_late=[PS(bk)] if kc else [])
                dst = Vt[:, ps_ * 4 + tt, (fc - 16) * 128:(fc - 15) * 128]
                k.op("act", lambda e, dst=dst, bk=bk: e.activation(out=dst, in_=banks[bk][:, 0:128], func=AF.Copy), [PS(bk)], ["Vt"])

    for tt in range(4):
        norm_tile(0, tt)
    for ps_ in range(4):
        fcs = list(range(32) if ps_ >= 2 else range(8, 32))
        step = len(fcs) // 4
        for idx, fc in enumerate(fcs):
            proj_chunk(ps_, fc)
            if ps_ + 1 < 4 and idx % step == step - 1:
                norm_tile(ps_ + 1, idx // step)
    k.flush()
    if dbg and stop == "proj":
        dbg_dump("qT", qT[:], [128, 8, 1024], "qT", BF16)
        dbg_dump("kT", kT[:], [128, 8, 2048], "kT", BF16)
        dbg_dump("Vt", Vt[:], [128, 16, 1024], "Vt", BF16)
        dbg_dump("uT", uT[:], [128, 8, 2048], "uT", BF16)
    if stop == "proj":
        k.flush()
        return nc

    R = Region(120 * KB_, 207 * KB_)
    attnT = k.sb("attnT", [128, 8, 1024], F32, R)
    relb_sb = k.sb("relb_sb", [32, 16], F32, R)
    ohm_sb = k.sb("ohm_sb", [32, GW], F32, R)
    gs_sb = k.sb("gs_sb", [16, GW], BF16, R)
    Bt = [k.sb(f"Bt{i}", [128, 2048], BF16, R) for i in range(2)]
    Tt = [k.sb(f"Tt{i}", [128, 2048], BF16, R) for i in range(2)]
    pw = [k.sb(f"pw{i}", [128, 512], BF16, R) for i in range(3)]
    pw2 = [k.sb(f"pw2{i}", [128, 512], BF16, R) for i in range(3)]
    kvones = k.sb("kvones", [128, 16, 64], BF16, R)
    rden = k.sb("rden", [128, 1024], F32, R)
    k.dma(relb_sb[:], relb[:, :], writes=["relb"], chan="a0")
    k.dma(ohm_sb[:], ohm[:, :], writes=["ohm"], chan="a1")
    k.op("act", lambda e: e.activation(out=relb_sb[:], in_=relb_sb[:], func=AF.Exp), ["relb"], ["relb"])
    for j in range(GW // 512):
        k.op("pe", lambda e, j=j: e.matmul(out=banks[6][0:16, :], lhsT=relb_sb[:, :], rhs=ohm_sb[:, j * 512:(j + 1) * 512], start=True, stop=True),
             ["relb", "ohm"], [PS(6)])
        k.op("act", lambda e, j=j: e.activation(out=gs_sb[:, j * 512:(j + 1) * 512], in_=banks[6][0:16, :], func=AF.Copy), [PS(6)], ["gs_sb"])
    k.dma(gsc[:, :], gs_sb[:], reads=["gs_sb"], writes=["gsc"], chan="a2")
    k.op("dve", lambda e: e.tensor_copy(out=kvones[:], in_=kv_sb[:].unsqueeze(2).to_broadcast([128, 16, 64])), ["kv"], ["kvones"])
    def head_pieces(h):
        out = []
        for kt in range(16):
            qlo = max(0, (kt - 8) * 128)
            for half in range(2):
                lo = max(qlo, half * 512)
                hi = (half + 1) * 512
                if lo < hi:
                    out.append((h, kt, half, lo, hi))
        return out

    SB3 = (0, 1, 7)

    def build_table(h):
        tb = h % 2
        k.dma(Bt[tb][:], bass.AP(gsc.tensor, h * GW, [[1, 128], [1, 2048]]), reads=["gsc"], writes=[("Bt", tb)], chan=f"bt{tb}")
        for j in range(4):
            k.op("pe", lambda e, tb=tb, j=j: e.matmul(out=banks[6][:], lhsT=antib[:], rhs=Bt[tb][:, j * 512:(j + 1) * 512], start=True, stop=True),
                 [("Bt", tb), "antib"], [PS(6)])
            k.op("act", lambda e, tb=tb, j=j: e.activation(out=Tt[tb][:, j * 512:(j + 1) * 512], in_=banks[6][:], func=AF.Copy), [PS(6)], [("Tt", tb)])

    def s_mm(idx, pc):
        h, kt, half, lo, hi = pc
        hp, hh = h // 2, h % 2
        pl = slice(64 * hh, 64 * hh + 64)
        n = hi - lo
        sbk = SB3[idx % 3]
        k.op("pe", lambda e, pl=pl, hp=hp, kt=kt, lo=lo, hi=hi, n=n, sbk=sbk: e.matmul(out=banks[sbk][:, 0:n], lhsT=kT[pl, hp, kt * 128:(kt + 1) * 128], rhs=qT[pl, hp, lo:hi], start=True, stop=True),
             ["kT", "qT"], [PS(sbk)])

    def rest(idx, pc):
        h, kt, half, lo, hi = pc
        hp, hh = h // 2, h % 2
        tb = h % 2
        pl = slice(64 * hh, 64 * hh + 64)
        n = hi - lo
        c0 = 1024 + lo - kt * 128
        sbk = SB3[idx % 3]
        bi = idx % 3
        k.op("act", lambda e, sbk=sbk, n=n, bi=bi: e.activation(out=pw[bi][:, 0:n], in_=banks[sbk][:, 0:n], func=AF.Exp, scale=0.125), [PS(sbk)], [("pw", bi)])
        k.op("dve", lambda e, bi=bi, n=n, tb=tb, c0=c0: e.tensor_tensor(out=pw2[bi][:, 0:n], in0=pw[bi][:, 0:n], in1=Tt[tb][:, c0:c0 + n], op=ALU.mult),
             [("pw", bi), ("Tt", tb)], [("pw2", bi)])
        first = (kt == 0)
        last = (kt == (11 if half == 0 else 15))
        nb, db = 2 + half, 4 + half
        oc = slice(lo - half * 512, hi - half * 512)
        k.op("pe", lambda e, pl=pl, kt=kt, h=h, bi=bi, n=n, nb=nb, oc=oc, first=first, last=last: e.matmul(out=banks[nb][pl, oc], lhsT=Vt[:, kt, h * 64:(h + 1) * 64], rhs=pw2[bi][:, 0:n], start=first, stop=last),
             [("pw2", bi), "Vt"], [PS(nb)] if (first and hh == 0) else [], wr_late=[] if (first and hh == 0) else [PS(nb)])
        k.op("pe", lambda e, pl=pl, kt=kt, bi=bi, n=n, db=db, oc=oc, first=first, last=last: e.matmul(out=banks[db][pl, oc], lhsT=kvones[:, kt, :], rhs=pw2[bi][:, 0:n], start=first, stop=last),
             [("pw2", bi), "kvones"], [PS(db)] if (first and hh == 0) else [], wr_late=[] if (first and hh == 0) else [PS(db)])

    build_table(0)
    gidx = 0
    for hp in range(8):
        pcs = head_pieces(2 * hp) + head_pieces(2 * hp + 1)
        nh0 = len(head_pieces(2 * hp))
        base = gidx
        for q in range(min(2, len(pcs))):
            s_mm(base + q, pcs[q])
        for q, pc in enumerate(pcs):
            if q == 0:
                build_table(2 * hp + 1)
            if q == nh0 and hp < 7:
                build_table(2 * hp + 2)
            if q + 2 < len(pcs):
                s_mm(base + q + 2, pcs[q + 2])
            rest(base + q, pc)
        gidx += len(pcs)
        for half in range(2):
            hs = slice(half * 512, (half + 1) * 512)
            k.op("dve", lambda e, half=half, hs=hs: e.reciprocal(out=rden[:, hs], in_=banks[4 + half][:]), [PS(4 + half)], [("rden", half)])
            k.op("dve", lambda e, half=half, hs=hs, hp=hp: e.tensor_tensor(out=attnT[:, hp, hs], in0=banks[2 + half][:], in1=rden[:, hs], op=ALU.mult),
                 [PS(2 + half), ("rden", half)], ["attnT"])
    k.flush()
    if dbg and stop == "attn":
        dbg_dump("attnT", attnT[:], [128, 8, 1024], "attnT", F32)
    if stop == "attn":
        k.flush()
        return nc

    RX = Region(8 * KB_, 40 * KB_)
    mixedT = k.sb("mixedT", [128, 16, 1024], BF16, RX)
    RN = Region(152 * KB_, 168 * KB_)
    sqa = [k.sb(f"sqa{i}", [128, 1024], F32, RN) for i in range(2)]
    rstd_a = k.sb("rstd_a", [128, 1024], F32, RN)

    def out_norm(srcT, row0, keyfn):
        for c8 in range(8):
            sq = sqa[c8 % 2]
            k.op("act", lambda e, c8=c8, sq=sq: e.activation(out=sq[:], in_=srcT[:, c8, :], func=AF.Square), [keyfn(c8)], [("sqa", c8 % 2)])
            for half in range(2):
                k.op("pe", lambda e, c8=c8, sq=sq, half=half: e.matmul(out=banks[6 + half][:], lhsT=onesf, rhs=sq[:, half * 512:(half + 1) * 512], start=(c8 == 0), stop=(c8 == 7)),
                     [("sqa", c8 % 2), "cm"], [PS(6 + half)] if c8 == 0 else [], wr_late=[PS(6 + half)] if c8 else [])
        for half in range(2):
            hs = slice(half * 512, (half + 1) * 512)
            k.op("dve", lambda e, half=half, hs=hs: e.tensor_scalar(out=rstd_a[:, hs], in0=banks[6 + half][:], scalar1=1.0 / 1024, scalar2=EPS, op0=ALU.mult, op1=ALU.add),
                 [PS(6 + half)], [("rstd_a", half)])
            k.op("act", lambda e, hs=hs: e.activation(out=rstd_a[:, hs], in_=rstd_a[:, hs], func=AF.Sqrt), [("rstd_a", half)], [("rstd_a", half)])
            k.op("dve", lambda e, hs=hs: e.reciprocal(out=rstd_a[:, hs], in_=rstd_a[:, hs]), [("rstd_a", half)], [("rstd_a", half)])
        for c8 in range(8):
            k.op("dve", lambda e, c8=c8: e.scalar_tensor_tensor(out=mixedT[:, row0 + c8, :], in0=srcT[:, c8, :], scalar=gmixed_sb[:, row0 + c8:row0 + c8 + 1], in1=rstd_a[:],
                                                               op0=ALU.mult, op1=ALU.mult),
                 [keyfn(c8), ("rstd_a", 0), ("rstd_a", 1), "gmixed"], ["mixedT"])

    out_norm(attnT, 0, lambda c8: "attnT")
    k.flush()

    lam_l = din("lam_l", [3, 128, 32])
    bc_l = din("bc_l", [4, 128, 32, 16])
    dg_l = din("dg_l", [128, 16])
    glu_w = din("glu_w", [1024, 1024])
    tidx = din("tidx", [128, 512])
    RA = Region(40 * KB_, 88 * KB_)
    RB = Region(120 * KB_, 207 * KB_)
    BT = k.sb("BT", [128, 8, 2, 128], BF16, RA)
    Cblk = k.sb("Cblk", [128, 32, 2, 64], BF16, RA)
    rho = k.sb("rho", [128, 32], F32, RA)
    uturn = k.sb("uturn", [128, 32], F32, RA)
    c512 = k.sb("c512", [128, 32], F32, RA)
    s512 = k.sb("s512", [128, 32], F32, RA)
    tix = k.sb("tix", [128, 512], F32, RC)
    RA2 = Region(24 * KB_, 40 * KB_)
    BT3 = k.sb("BT3", [128, 8, 2, 128], BF16, RA)
    dg = k.sb("dg", [128, 16], F32, RA)
    k.dma(tix[:], tidx[:, :], writes=["tix"], chan="s0")
    k.dma(dg[:], dg_l[:, :], writes=["dg"], chan="s1")
    RP = Region(120 * KB_, 207 * KB_)
    lam = k.sb("lam", [128, 3, 32], F32, RP)
    bc = k.sb("bc", [128, 4, 32, 16], F32, RP)
    k.dma(lam[:], lam_l.rearrange("c p j -> p c j"), writes=["lam"], chan="s2")
    k.dma(bc[:], bc_l.rearrange("c p j x -> p c j x"), writes=["bc"], chan="s3")
    P_ = {}
    for nm in ["dt", "th", "v", "vf", "sn", "cs", "are", "aim", "den", "fre", "fim", "t1", "t2"]:
        P_[nm] = k.sb("p_" + nm, [128, 32], F32, RP)
    pvi = k.sb("p_vi", [128, 32], I32, RP)
    lr, li, ldt = lam[:, 0, :], lam[:, 1, :], lam[:, 2, :]
    TWO_PI = 6.2831845
    PI_ = 3.1415920

    def dv(fn, reads, writes):
        k.op("dve", fn, reads, writes)

    def ac(fn, reads, writes):
        k.op("act", fn, reads, writes)

    tt_ = lambda o, a, b, op: (lambda e: e.tensor_tensor(out=o, in0=a, in1=b, op=op))
    ac(lambda e: e.activation(out=P_["dt"][:], in_=ldt, func=AF.Exp), ["lam"], ["p_dt"])
    dv(tt_(P_["th"][:], li, P_["dt"][:], ALU.mult), ["lam", "p_dt"], ["p_th"])
    dv(tt_(P_["t1"][:], lr, P_["dt"][:], ALU.mult), ["lam", "p_dt"], ["p_t1"])
    ac(lambda e: e.activation(out=rho[:], in_=P_["t1"][:], func=AF.Exp), ["p_t1"], ["rho"])
    dv(lambda e: e.tensor_scalar(out=uturn[:], in0=P_["th"][:], scalar1=1.0 / (2 * math.pi), scalar2=None, op0=ALU.mult), ["p_th"], ["uturn"])
    for off, dst in ((0.0, "sn"), (0.25, "cs")):
        dv(lambda e, off=off: e.tensor_scalar(out=P_["v"][:], in0=uturn[:], scalar1=off, scalar2=None, op0=ALU.add), ["uturn"], ["p_v"])
        dv(lambda e: e.tensor_copy(out=pvi[:], in_=P_["v"][:]), ["p_v"], ["p_vi"])
        dv(tt_(P_["vf"][:], P_["v"][:], pvi[:], ALU.subtract), ["p_v", "p_vi"], ["p_vf"])
        ac(lambda e, dst=dst: e.activation(out=P_[dst][:], in_=P_["vf"][:], func=AF.Sin, scale=TWO_PI), ["p_vf"], ["p_" + dst])
    dv(lambda e: e.tensor_scalar(out=P_["v"][:], in0=uturn[:], scalar1=512.0, scalar2=None, op0=ALU.mult), ["uturn"], ["p_v"])
    dv(lambda e: e.tensor_copy(out=pvi[:], in_=P_["v"][:]), ["p_v"], ["p_vi"])
    dv(tt_(P_["vf"][:], P_["v"][:], pvi[:], ALU.subtract), ["p_v", "p_vi"], ["p_vf"])
    ac(lambda e: e.activation(out=s512[:], in_=P_["vf"][:], func=AF.Sin, scale=TWO_PI), ["p_vf"], ["s512"])
    ac(lambda e: e.activation(out=P_["t1"][:], in_=P_["vf"][:], func=AF.Sin, scale=PI_), ["p_vf"], ["p_t1"])
    ac(lambda e: e.activation(out=P_["t1"][:], in_=P_["t1"][:], func=AF.Square), ["p_t1"], ["p_t1"])
    dv(lambda e: e.tensor_scalar(out=c512[:], in0=P_["t1"][:], scalar1=-2.0, scalar2=1.0, op0=ALU.mult, op1=ALU.add), ["p_t1"], ["c512"])
    dv(tt_(P_["are"][:], rho[:], P_["cs"][:], ALU.mult), ["rho", "p_cs"], ["p_are"])
    dv(tt_(P_["aim"][:], rho[:], P_["sn"][:], ALU.mult), ["rho", "p_sn"], ["p_aim"])
    dv(tt_(P_["t1"][:], lr, lr, ALU.mult), ["lam"], ["p_t1"])
    dv(tt_(P_["t2"][:], li, li, ALU.mult), ["lam"], ["p_t2"])
    dv(tt_(P_["den"][:], P_["t1"][:], P_["t2"][:], ALU.add), ["p_t1", "p_t2"], ["p_den"])
    dv(lambda e: e.reciprocal(out=P_["den"][:], in_=P_["den"][:]), ["p_den"], ["p_den"])
    dv(lambda e: e.tensor_scalar(out=P_["are"][:], in0=P_["are"][:], scalar1=-1.0, scalar2=None, op0=ALU.add), ["p_are"], ["p_are"])
    dv(tt_(P_["t1"][:], P_["are"][:], lr, ALU.mult), ["p_are", "lam"], ["p_t1"])
    dv(tt_(P_["t2"][:], P_["aim"][:], li, ALU.mult), ["p_aim", "lam"], ["p_t2"])
    dv(tt_(P_["fre"][:], P_["t1"][:], P_["t2"][:], ALU.add), ["p_t1", "p_t2"], ["p_fre"])
    dv(tt_(P_["fre"][:], P_["fre"][:], P_["den"][:], ALU.mult), ["p_fre", "p_den"], ["p_fre"])
    dv(tt_(P_["t1"][:], P_["aim"][:], lr, ALU.mult), ["p_aim", "lam"], ["p_t1"])
    dv(tt_(P_["t2"][:], P_["are"][:], li, ALU.mult), ["p_are", "lam"], ["p_t2"])
    dv(tt_(P_["fim"][:], P_["t1"][:], P_["t2"][:], ALU.subtract), ["p_t1", "p_t2"], ["p_fim"])
    dv(tt_(P_["fim"][:], P_["fim"][:], P_["den"][:], ALU.mult), ["p_fim", "p_den"], ["p_fim"])
    bb = [k.sb(f"bb{i}", [128, 32, 16], F32, RP) for i in range(4)]
    fre_b = P_["fre"][:].unsqueeze(2).to_broadcast([128, 32, 16])
    fim_b = P_["fim"][:].unsqueeze(2).to_broadcast([128, 32, 16])
    dv(tt_(bb[0][:], bc[:, 0], fre_b, ALU.mult), ["bc", "p_fre"], ["bb0"])
    dv(tt_(bb[1][:], bc[:, 1], fim_b, ALU.mult), ["bc", "p_fim"], ["bb1"])
    dv(tt_(bb[0][:], bb[0][:], bb[1][:], ALU.subtract), ["bb0", "bb1"], ["bb0"])
    dv(tt_(bb[2][:], bc[:, 1], fre_b, ALU.mult), ["bc", "p_fre"], ["bb2"])
    dv(tt_(bb[3][:], bc[:, 0], fim_b, ALU.mult), ["bc", "p_fim"], ["bb3"])
    dv(tt_(bb[2][:], bb[2][:], bb[3][:], ALU.add), ["bb2", "bb3"], ["bb2"])
    Bblk = k.sb("Bblk", [128, 8, 2, 4, 32], F32, RP)
    Cf = k.sb("Cf", [128, 32, 2, 64], F32, RP)
    Bblk3 = k.sb("Bblk3", [128, 8, 2, 4, 32], F32, RP)
    dv(lambda e: e.memset(Bblk3[:], 0.0), [], ["Bblk3"])
    dv(lambda e: e.memset(Bblk[:], 0.0), [], ["Bblk"])
    dv(lambda e: e.memset(Cf[:], 0.0), [], ["Cf"])
    dv(lambda e: e.tensor_scalar(out=bc[:, 3], in0=bc[:, 3], scalar1=-1.0, scalar2=None, op0=ALU.mult), ["bc"], ["bc"])
    for gl in range(2):
        ls = slice(64 * gl, 64 * gl + 64)
        cs_ = slice(16 * gl, 16 * gl + 16)
        for ri, src in ((0, bb[0]), (1, bb[2])):
            dv(lambda e, ls=ls, cs_=cs_, ri=ri, src=src: e.tensor_copy(out=Bblk[ls, :, ri, :, cs_], in_=src[ls].rearrange("p (i j) c -> p i j c", j=4)),
               ["bb0", "bb2", "Bblk"], ["Bblk"])
        for ri in range(2):
            cv = Cf[ls, :, ri, :].rearrange("p (i j) c -> p i j c", j=4)
            sv = bc[ls, 2 + ri].rearrange("p (i j) c -> p i j c", j=4)
            dv(lambda e, cv=cv, sv=sv, gl=gl: e.tensor_copy(out=cv[:, :, 0:3, 16 * gl:16 * gl + 16], in_=sv[:, :, 0:3, :]), ["bc", "Cf"], ["Cf"])
            dv(lambda e, cv=cv, sv=sv, gl=gl: e.tensor_copy(out=cv[:, :, 3:4, 32 + 16 * gl:48 + 16 * gl], in_=sv[:, :, 3:4, :]), ["bc", "Cf"], ["Cf"])
    dv(lambda e: e.tensor_copy(out=Cblk[:], in_=Cf[:]), ["Cf"], ["Cblk"])
    for i in range(8):
        for ri in range(2):
            bk = 6 + (2 * i + ri) % 2
            k.op("pe", lambda e, i=i, ri=ri, bk=bk: e.transpose(out=banks[bk][:, 0:128], in_=Bblk[:, i, ri].rearrange("p j c -> p (j c)"), identity=cm[:, 0, :]),
                 ["Bblk", "cm"], [PS(bk)])
            ac(lambda e, i=i, ri=ri, bk=bk: e.activation(out=BT[:, i, ri, :], in_=banks[bk][:, 0:128], func=AF.Copy), [PS(bk)], ["BT"])
    dv(lambda e: e.tensor_copy(out=Bblk3[:, :, :, 3, :], in_=Bblk[:, :, :, 3, :]), ["Bblk", "Bblk3"], ["Bblk3"])
    for i in range(8):
        for ri in range(2):
            bk = 6 + (2 * i + ri) % 2
            k.op("pe", lambda e, i=i, ri=ri, bk=bk: e.transpose(out=banks[bk][:, 0:128], in_=Bblk3[:, i, ri].rearrange("p j c -> p (j c)"), identity=cm[:, 0, :]),
                 ["Bblk3", "cm"], [PS(bk)])
            ac(lambda e, i=i, ri=ri, bk=bk: e.activation(out=BT3[:, i, ri, :], in_=banks[bk][:, 0:128], func=AF.Copy), [PS(bk)], ["BT3"])
    k.flush()
    if dbg and stop == "ssmprep":
        dbg_dump("BT", BT[:], [128, 8, 2, 128], "BT", BF16)
        dbg_dump("Cblk", Cblk[:], [128, 32, 2, 32], "Cblk", BF16)
        dbg_dump("rho", rho[:], [128, 32], "rho")
        dbg_dump("uturn", uturn[:], [128, 32], "uturn")
        k.flush()
        return nc

    ygf = k.sb("ygf", [128, 8, 1024], F32, RB)
    ygb = k.sb("ygb", [128, 8, 1024], BF16, RB)
    gw = k.sb("gw", [128, 8, 1024], BF16, RB)
    sqa = [k.sb(f"sqb_{i}", [128, 1024], F32, RB) for i in range(2)]
    rstd_a = k.sb("rstd_b", [128, 1024], F32, RB)
    ytmp = k.sb("ytmp", [128, 512], F32, RB)
    sg = k.sb("sg", [128, 512], F32, RB)
    for kc in range(8):
        k.dma(gw[:, kc, :], glu_w[kc * 128:(kc + 1) * 128, :], writes=["gw"], chan="gw0", eng="pool")
    vb = k.sb("vb", [128, 512], F32, RA)
    vib = k.sb("vib", [128, 512], I32, RA)
    rb_ = k.sb("rb_", [128, 512], F32, RA)
    snb = [k.sb(f"snb{i}", [128, 512], F32, RA) for i in range(2)]
    csb = [k.sb(f"csb{i}", [128, 512], F32, RA) for i in range(2)]
    tb1 = [k.sb(f"tb_{i}", [128, 512], F32, RA) for i in range(4)]
    tbs = [tb1, tb1]
    negI = k.sb("negI", [128, 128], F32, RA)
    wsb = [[k.sb(f"wsb{p_}_{i}", [128, 512], F32, RA) for i in range(2)] for p_ in range(2)]
    dv(lambda e: e.tensor_scalar(out=negI[:], in0=cm[:, 0, :], scalar1=-1.0, scalar2=None, op0=ALU.mult), ["cm"], ["negI"])
    cin = [k.sb(f"cin{i}", [128, 4], F32, RA) for i in range(2)]
    zz = [k.sb(f"zz{i}", [128, 2, 512], F32, RA2) for i in range(2)]
    zre = [zz[i][:, 0, :] for i in range(2)]
    zim = [zz[i][:, 1, :] for i in range(2)]
    rot = k.sb("rot", [128, 32, 4], F32, RA)
    cin2 = [k.sb(f"cin2_{i}", [128, 2], F32, RA) for i in range(2)]
    dv(lambda e: e.tensor_copy(out=rot[:, :, 0], in_=c512[:]), ["c512"], ["rot"])
    dv(lambda e: e.tensor_scalar(out=rot[:, :, 1], in0=s512[:], scalar1=-1.0, scalar2=None, op0=ALU.mult), ["s512", "rot"], ["rot"])
    dv(lambda e: e.tensor_copy(out=rot[:, :, 2], in_=s512[:]), ["s512", "rot"], ["rot"])
    dv(lambda e: e.tensor_copy(out=rot[:, :, 3], in_=c512[:]), ["c512", "rot"], ["rot"])
    xq = [[k.sb(f"xq{p_}_{i}", [128, 512], BF16, RA2) for i in range(4)] for p_ in range(2)]
    def cpar(c):
        jp, tc = divmod(c, 4)
        i, j = divmod(jp, 4)
        return jp, tc, i, j, jp % 2, c % 2

    def emit_tables(jp):
        pp = jp % 2
        ac(lambda e, jp=jp: e.activation(out=vb[:], in_=tix[:], func=AF.Copy, scale=uturn[:, jp:jp + 1]), ["tix", "uturn"], ["vb"])
        dv(lambda e: e.tensor_copy(out=vib[:], in_=vb[:]), ["vb"], ["vib"])
        dv(tt_(rb_[:], vb[:], vib[:], ALU.subtract), ["vb", "vib"], ["rb_"])
        ac(lambda e, pp=pp: e.activation(out=snb[pp][:], in_=rb_[:], func=AF.Sin, scale=TWO_PI), ["rb_"], [("snb", pp)])
        ac(lambda e: e.activation(out=vb[:], in_=rb_[:], func=AF.Sin, scale=PI_), ["rb_"], ["vb"])
        ac(lambda e: e.activation(out=vb[:], in_=vb[:], func=AF.Square), ["vb"], ["vb"])
        ac(lambda e, pp=pp: e.activation(out=csb[pp][:], in_=vb[:], func=AF.Identity, scale=-2.0, bias=cm[:, 3, 0:1]), ["vb", "cm"], [("csb", pp)])

    def emit_bu(c):
        jp, tc, i, j, pp, pb = cpar(c)
        prs = slice(32 * j, 32 * j + 32)
        ts_ = slice(tc * 512, (tc + 1) * 512)
        for ri, bnk in ((0, 0), (1, 1)):
            if j < 3:
                k.op("pe", lambda e, prs=prs, i=i, ts_=ts_, bnk=bnk, ri=ri: e.matmul(out=banks[bnk][:], lhsT=BT[prs, i, ri, :], rhs=uT[prs, i, ts_], start=True, stop=True),
                     ["BT", "uT"], [PS(bnk)])
            else:
                k.op("pe", lambda e, i=i, ts_=ts_, bnk=bnk, ri=ri: e.matmul(out=banks[bnk][:], lhsT=BT3[:, i, ri, :], rhs=uT[:, i, ts_], start=True, stop=True),
                     ["BT3", "uT"], [PS(bnk)])

    WB = ((2, 3), (6, 7))

    def demod_ops(c):
        jp, tc, i, j, pp, pb = cpar(c)
        T = tbs[pb]
        bre, bim = 0, 1
        TK = lambda q: ("tb", q)
        return [
            lambda: dv(tt_(T[0][:], banks[bre][:], csb[pp][:], ALU.mult), [PS(bre), ("csb", pp)], [TK(0)]),
            lambda: dv(tt_(T[1][:], banks[bim][:], snb[pp][:], ALU.mult), [PS(bim), ("snb", pp)], [TK(1)]),
            lambda: dv(tt_(T[2][:], banks[bim][:], csb[pp][:], ALU.mult), [PS(bim), ("csb", pp)], [TK(2)]),
            lambda: dv(tt_(T[3][:], banks[bre][:], snb[pp][:], ALU.mult), [PS(bre), ("snb", pp)], [TK(3)]),
        ]

    def pool_adds(c):
        jp, tc, i, j, pp, pb = cpar(c)
        T = tbs[pb]
        wr, wi_ = WB[pb]
        TK = lambda q: ("tb", q)
        k.op("pe", lambda e: e.matmul(out=banks[wr][:], lhsT=cm[:, 0, :], rhs=T[0][:], start=True, stop=False), ["cm", TK(0)], [PS(wr)])
        k.op("pe", lambda e: e.matmul(out=banks[wr][:], lhsT=cm[:, 0, :], rhs=T[1][:], start=False, stop=True), ["cm", TK(1)], [], wr_late=[PS(wr)])
        k.op("pe", lambda e: e.matmul(out=banks[wi_][:], lhsT=cm[:, 0, :], rhs=T[2][:], start=True, stop=False), ["cm", TK(2)], [PS(wi_)])
        k.op("pe", lambda e: e.matmul(out=banks[wi_][:], lhsT=negI[:], rhs=T[3][:], start=False, stop=True), ["negI", TK(3)], [], wr_late=[PS(wi_)])
        ac(lambda e: e.activation(out=wsb[pb][0][:], in_=banks[wr][:], func=AF.Copy), [PS(wr)], [("wsb", pb, 0)])
        ac(lambda e: e.activation(out=wsb[pb][1][:], in_=banks[wi_][:], func=AF.Copy), [PS(wi_)], [("wsb", pb, 1)])

    def carry_ops(c):
        jp, tc, i, j, pp, pb = cpar(c)
        if tc == 0:
            return []
        zp = 1 - pb
        zl = zz[zp][:, :, 511].unsqueeze(1).to_broadcast([128, 2, 2])
        rt = rot[:, jp, :].rearrange("p (o k) -> p o k", o=2)
        ci = cin[pb]
        return [
            lambda: dv(lambda e: e.tensor_tensor(out=ci[:, 0:4].rearrange("p (o k) -> p o k", o=2), in0=zl, in1=rt, op=ALU.mult),
                       [("zre", zp), ("zim", zp), "rot"], [("cin", pb)]),
            lambda: dv(lambda e: e.tensor_reduce(out=cin2[pb][:], in_=ci[:, 0:4].rearrange("p (o k) -> p o k", o=2), axis=mybir.AxisListType.X, op=ALU.add),
                       [("cin", pb)], [("cin2", pb)]),
        ]

    def scans(c):
        jp, tc, i, j, pp, pb = cpar(c)
        wr, wi_ = WB[pb]
        rho_c = rho[:, jp:jp + 1]
        if tc == 0:
            ini_re, ini_im, ikeys = 0.0, 0.0, []
        else:
            ini_re, ini_im, ikeys = cin2[pb][:, 0:1], cin2[pb][:, 1:2], [("cin2", pb)]
        dv(lambda e: e.tensor_tensor_scan(out=zre[pb][:], data0=rho_c.to_broadcast([128, 512]), data1=wsb[pb][0][:], initial=ini_re, op0=ALU.mult, op1=ALU.add),
           [("wsb", pb, 0), "rho"] + ikeys, [("zre", pb)])
        dv(lambda e: e.tensor_tensor_scan(out=zim[pb][:], data0=rho_c.to_broadcast([128, 512]), data1=wsb[pb][1][:], initial=ini_im, op0=ALU.mult, op1=ALU.add),
           [("wsb", pb, 1), "rho"] + ikeys, [("zim", pb)])

    def remod(c):
        jp, tc, i, j, pp, pb = cpar(c)
        X = xq[pb]
        XK = lambda q: ("xq", pb, q)
        prs = slice(32 * j, 32 * j + 32)
        dv(tt_(X[0][:], zre[pb][:], csb[pp][:], ALU.mult), [("zre", pb), ("csb", pp)], [XK(0)])
        dv(lambda e: e.scalar_tensor_tensor(out=X[1][:], in0=zim[pb][:], scalar=-1.0, in1=snb[pp][:], op0=ALU.mult, op1=ALU.mult), [("zim", pb), ("snb", pp)], [XK(1)])
        k.op("pool", lambda e: e.tensor_tensor(out=X[2][:], in0=zre[pb][:], in1=snb[pp][:], op=ALU.mult), [("zre", pb), ("snb", pp)], [XK(2)])
        k.op("pool", lambda e: e.tensor_tensor(out=X[3][:], in0=zim[pb][:], in1=csb[pp][:], op=ALU.mult), [("zim", pb), ("csb", pp)], [XK(3)])
        yb = 4 + (tc - 2)
        if j < 2:
            ops_, cw = prs, 32
        else:
            ops_, cw = slice(64, 128), 64
        for q, cv in ((0, 0), (1, 0), (2, 1), (3, 1)):
            first = (q == 0)
            last = (q == 3)
            k.op("pe", lambda e, q=q, cv=cv, first=first, last=last: e.matmul(out=banks[yb][ops_, :], lhsT=Cblk[:, jp, cv, 0:cw], rhs=X[q][:], start=(first and j != 3), stop=(last and j != 2)),
                 ["Cblk", XK(q)], [PS(yb)] if (first and j == 0) else [], wr_late=[] if (first and j == 0) else [PS(yb)])

    def evac_tile(i):
        for tc in (2, 3):
            yb = 4 + (tc - 2)
            os_ = slice((tc - 2) * 512, (tc - 1) * 512)
            dv(lambda e, i=i, tc=tc, yb=yb: e.scalar_tensor_tensor(out=ytmp[:], in0=uT[:, i, tc * 512:(tc + 1) * 512], scalar=dg[:, i:i + 1], in1=banks[yb][:], op0=ALU.mult, op1=ALU.add),
               ["uT", "dg", PS(yb)], ["ytmp"])
            ac(lambda e, i=i, os_=os_: e.activation(out=ygf[:, i, os_], in_=ytmp[:], func=AF.Gelu), ["ytmp"], [("ygf", i)])
            k.op("pool", lambda e, i=i, os_=os_: e.tensor_copy(out=ygb[:, i, os_], in_=ygf[:, i, os_]), [("ygf", i)], ["ygb"])

    NCH = 128
    emit_tables(0)
    emit_bu(0)
    for m_ in demod_ops(0):
        m_()
    pool_adds(0)
    for c in range(NCH):
        jp, tc, i, j, pp, pb = cpar(c)
        if tc == 1 and jp + 1 < 32:
            emit_tables(jp + 1)
        nxt = c + 1 < NCH
        if nxt:
            emit_bu(c + 1)
        C_ = carry_ops(c)
        M_ = demod_ops(c + 1) if nxt else []
        for q in range(4):
            if q < len(C_):
                C_[q]()
            if q < len(M_):
                M_[q]()
        if nxt:
            pool_adds(c + 1)
        scans(c)
        if tc >= 2:
            remod(c)
        if tc == 3 and j == 3:
            evac_tile(i)
    for fo in range(8):
        for half in range(2):
            bk = half
            hs = slice(half * 512, (half + 1) * 512)
            for kc in range(8):
                k.op("pe", lambda e, fo=fo, kc=kc, hs=hs, bk=bk: e.matmul(out=banks[bk][:], lhsT=gw[:, kc, fo * 128:(fo + 1) * 128], rhs=ygb[:, kc, hs], start=(kc == 0), stop=(kc == 7)),
                     ["gw", "ygb"], [PS(bk)] if kc == 0 else [], wr_late=[PS(bk)] if kc else [])
            ac(lambda e, fo=fo, bk=bk: e.activation(out=sg[:], in_=banks[bk][:], func=AF.Sigmoid, bias=dg[:, 8 + fo:9 + fo]), [PS(bk), "dg"], ["sg"])
            dv(lambda e, fo=fo, hs=hs: e.tensor_tensor(out=ygf[:, fo, hs], in0=ygf[:, fo, hs], in1=sg[:], op=ALU.mult), [("ygf", fo), "sg"], [("ygf", fo)])
    k.flush()
    if dbg and stop == "ssm":
        dbg_dump("ygf", ygf[:], [128, 8, 1024], ("ygf", 0), F32)
    out_norm(ygf, 8, lambda c8: ("ygf", c8))
    k.flush()
    if dbg and stop == "ssm":
        dbg_dump("mixedT", mixedT[:], [128, 16, 1024], "mixedT", BF16)
    if stop == "ssm":
        k.flush()
        return nc

    w_out = din("w_out", [2048, 2048])
    RXR = Region(40 * KB_, 104 * KB_)
    x_res = k.sb("x_res", [128, 8, D], F32, RXR)
    RW = Region(104 * KB_, 207 * KB_)
    wob = [k.sb(f"wob{i}", [128, 16, 512], BF16, RW) for i in range(2)]
    xin = [k.sb(f"xin{i}", [128, 512], F32, RW) for i in range(2)]
    xc = 0
    for dc in range(4):
        ds_ = slice(dc * 512, (dc + 1) * 512)
        for hf in range(2):
            k.dma(wob[dc % 2][:, hf * 8:(hf + 1) * 8, :], w_out[hf * 1024:(hf + 1) * 1024, ds_].rearrange("(kc p) d -> p kc d", p=128), writes=[("wob", dc % 2, hf)], chan=f"wo{dc % 2}", eng="pool")
        for tt in range(8):
            bk = tt % 2
            xb_ = xc % 2
            xc += 1
            k.dma(xin[xb_][:], x_all[1024 + tt * 128: 1024 + (tt + 1) * 128, ds_], writes=[("xin", xb_)], chan=f"xi{xb_}")
            for kc in range(16):
                k.op("pe", lambda e, kc=kc, tt=tt, bk=bk, dc=dc: e.matmul(out=banks[bk][:], lhsT=mixedT[:, kc, tt * 128:(tt + 1) * 128], rhs=wob[dc % 2][:, kc, :], start=(kc == 0), stop=(kc == 15)),
                     ["mixedT", ("wob", dc % 2, kc // 8)], [PS(bk)] if kc == 0 else [], wr_late=[PS(bk)] if kc else [])
            k.op("dve", lambda e, tt=tt, ds_=ds_, bk=bk, xb_=xb_: e.tensor_tensor(out=x_res[:, tt, ds_], in0=banks[bk][:], in1=xin[xb_][:], op=ALU.add),
                 [PS(bk), ("xin", xb_)], [("x_res", tt)])
    k.flush()
    if dbg and stop == "x1":
        dbg_dump("x1", x_res[:], [128, 8, D], ("x_res", 0), F32)
    if stop == "x1":
        k.flush()
        return nc

    gffn = din("gffn", [128, 16])
    wq_t = din("wq_t", [16, 128, 16, 128])
    keysT = din("keysT", [128, 16, 128])
    U_t = din("U_t", [128, 128, 16, 128])
    Vp = din("Vp", [16384, D])
    gffn_sb = k.sb("gffn_sb", [128, 16], F32, RC)
    k.dma(gffn_sb[:], gffn[:, :], writes=["gffn"], chan="c5")
    RS = Region(8 * KB_, 40 * KB_)
    s12 = k.sb("s12", [128, 4, 2, 8, 128], F32, RS)
    for hh_ in range(2):
        RP2 = Region(104 * KB_, 207 * KB_)
        hn2T = k.sb("hn2T", [128, 16, 512], BF16, RP2)
        Dm = k.sb("Dm", [128, 4, 8, 128], BF16, RP2)
        tstat = k.sb("tstat", [128, 4, 8, 4], F32, RP2)
        mark = RP2.top
        qpT = k.sb("qpT", [128, 16, 512], BF16, RP2)
        wbf2 = [k.sb(f"wbf2{i}", [128, 16, 128], BF16, RP2) for i in range(4)]
        hnb2 = k.sb("hnb2", [128, D], BF16, RP2)
        keysb = k.sb("keysb", [128, 16, 128], BF16, RP2)
        st2 = k.sb("st2", [128, 4], F32, RP2)
        m16 = [k.sb(f"m16_{q}", [128, 2, 16], F32, RP2) for q in range(4)]
        c24 = [k.sb(f"c24_{q}", [128, 24], F32, RP2) for q in range(4)]
        tmp1 = [k.sb(f"tmp1_{q}", [128, 128], F32, RP2) for q in range(4)]
        cand = [k.sb(f"cand_{q}", [128, 16, 16], F32, RP2) for q in range(4)]
        cand2 = [k.sb(f"cand2_{q}", [128, 256], F32, RP2) for q in range(4)]
        ejunk = [k.sb(f"ejunk_{q}", [128, 16], F32, RP2) for q in range(4)]
        k.dma(keysb[:], keysT[:, :, :], writes=["keysb"], chan="p0", eng="pool")
        for tt in range(4):
            tg = hh_ * 4 + tt
            sc = st2[:, tt:tt + 1]
            sk = ("st2", tt)
            k.op("act", lambda e, tg=tg, sc=sc: e.activation(out=hnb2[:], in_=x_res[:, tg, :], func=AF.Square, accum_out=sc), [("x_res", tg)], ["hnb2", sk])
            k.op("dve", lambda e, sc=sc: e.tensor_scalar(out=sc, in0=sc, scalar1=1.0 / D, scalar2=EPS, op0=ALU.mult, op1=ALU.add), [sk], [sk])
            k.op("act", lambda e, sc=sc: e.activation(out=sc, in_=sc, func=AF.Sqrt), [sk], [sk])
            k.op("dve", lambda e, sc=sc: e.reciprocal(out=sc, in_=sc), [sk], [sk])
            k.op("dve", lambda e, tg=tg, sc=sc: e.tensor_scalar(out=hnb2[:], in0=x_res[:, tg, :], scalar1=sc, scalar2=None, op0=ALU.mult), [("x_res", tg), sk], ["hnb2"])
            for hf in range(2):
                bk = hf
                pv = banks[bk][:].bitcast(BF16)
                for j in range(8):
                    kc = hf * 8 + j
                    k.op("pe", lambda e, kc=kc, j=j, pv=pv: e.transpose(out=pv[:, j * 128:(j + 1) * 128], in_=hnb2[:, kc * 128:(kc + 1) * 128], identity=identb[:]),
                         ["hnb2", "identb"], [PS(bk)] if j == 0 else [], wr_late=[PS(bk)] if j else [])
                for j in range(8):
                    kc = hf * 8 + j
                    k.op("act", lambda e, kc=kc, j=j, tt=tt, pv=pv: e.activation(out=hn2T[:, kc, tt * 128:(tt + 1) * 128], in_=pv[:, j * 128:(j + 1) * 128], func=AF.Copy, scale=gffn_sb[:, kc:kc + 1]),
                         [PS(bk), "gffn"], ["hn2T"])
        for fc in range(16):
            wb = fc % 4
            k.dma(wbf2[wb][:], wq_t[fc], writes=[("wbf2", wb)], chan=f"wq{wb}", eng="pool")
            bk = 2 + fc % 2
            for kc in range(16):
                k.op("pe", lambda e, wb=wb, kc=kc, bk=bk: e.matmul(out=banks[bk][:], lhsT=wbf2[wb][:, kc, :], rhs=hn2T[:, kc, :], start=(kc == 0), stop=(kc == 15)),
                     [("wbf2", wb), "hn2T"], [PS(bk)] if kc == 0 else [], wr_late=[PS(bk)] if kc else [])
            k.op("act", lambda e, fc=fc, bk=bk: e.activation(out=qpT[:, fc, :], in_=banks[bk][:], func=AF.Copy), [PS(bk)], ["qpT"])
        for tt in range(4):
            for half in range(2):
                for hq in range(2):
                    bk = 4 + (2 * half + hq) % 4
                    for h4 in range(4):
                        h = hq * 4 + h4
                        k.op("pe", lambda e, tt=tt, half=half, h=h, h4=h4, bk=bk: e.matmul(out=banks[bk][:, h4 * 128:(h4 + 1) * 128], lhsT=qpT[:, 2 * h + half, tt * 128:(tt + 1) * 128], rhs=keysb[:, half * 8 + h, :], start=True, stop=True),
                             ["qpT", "keysb"], [PS(bk)] if h4 == 0 else [], wr_late=[PS(bk)] if h4 else [])
                    k.op("act", lambda e, tt=tt, half=half, hq=hq, bk=bk: e.activation(out=s12[:, tt, half, hq * 4:(hq + 1) * 4, :], in_=banks[bk][:].rearrange("p (a b) -> p a b", a=4), func=AF.Copy),
                         [PS(bk)], [("s12", tt)])
            for hq in range(2):
                hs4 = [hq * 4 + q for q in range(4)]
                for half in range(2):
                    svs = [s12[:, tt, half, h, :] for h in hs4]
                    for q in range(4):
                        dv(lambda e, half=half, q=q, sv=svs[q]: e.max(out=m16[q][:, half, 0:8], in_=sv), [("s12", tt)], [("m16", q, half)])
                    for q in range(4):
                        dv(lambda e, half=half, q=q, sv=svs[q]: e.match_replace(out=tmp1[q][:], in_to_replace=m16[q][:, half, 0:8], in_values=sv, imm_value=-1e30),
                           [("s12", tt), ("m16", q, half)], [("tmp1", q)])
                    for q in range(4):
                        dv(lambda e, half=half, q=q: e.max(out=m16[q][:, half, 8:16], in_=tmp1[q][:]), [("tmp1", q)], [("m16", q, half)])
                for q in range(4):
                    dv(lambda e, q=q: e.tensor_tensor(out=cand[q][:], in0=m16[q][:, 0, :].unsqueeze(2).to_broadcast([128, 16, 16]), in1=m16[q][:, 1:2, :].to_broadcast([128, 16, 16]), op=ALU.add),
                       [("m16", q, 0), ("m16", q, 1)], [("cand", q)])
                for q in range(4):
                    dv(lambda e, q=q: e.max(out=c24[q][:, 0:8], in_=cand[q][:].rearrange("p a b -> p (a b)")), [("cand", q)], [("c24", q)])
                for q in range(4):
                    dv(lambda e, q=q: e.match_replace(out=cand2[q][:], in_to_replace=c24[q][:, 0:8], in_values=cand[q][:].rearrange("p a b -> p (a b)"), imm_value=-1e30), [("cand", q), ("c24", q)], [("cand2", q)])
                for q in range(4):
                    dv(lambda e, q=q: e.max(out=c24[q][:, 8:16], in_=cand2[q][:]), [("cand2", q)], [("c24", q)])
                for q in range(4):
                    dv(lambda e, q=q: e.match_replace(out=cand2[q][:], in_to_replace=c24[q][:, 8:16], in_values=cand2[q][:], imm_value=-1e30), [("cand2", q), ("c24", q)], [("cand2", q)])
                for q in range(4):
                    dv(lambda e, q=q: e.max(out=c24[q][:, 16:24], in_=cand2[q][:]), [("cand2", q)], [("c24", q)])
                for q, h in enumerate(hs4):
                    ts0 = tstat[:, tt, h, 0:1]
                    dv(lambda e, ts0=ts0, q=q: e.tensor_tensor(out=ts0, in0=c24[q][:, 15:16], in1=c24[q][:, 16:17], op=ALU.add), [("c24", q)], [("tstat", tt, h)])
                for q, h in enumerate(hs4):
                    ts0 = tstat[:, tt, h, 0:1]
                    dv(lambda e, ts0=ts0: e.tensor_scalar(out=ts0, in0=ts0, scalar1=0.5, scalar2=None, op0=ALU.mult), [("tstat", tt, h)], [("tstat", tt, h)])
                for q, h in enumerate(hs4):
                    ts1 = tstat[:, tt, h, 1:2]
                    ac(lambda e, ts1=ts1, q=q: e.activation(out=ejunk[q][:], in_=c24[q][:, 0:16], func=AF.Exp, accum_out=ts1), [("c24", q)], [("ejunk", q), ("tstat1", tt, h)])
                for q, h in enumerate(hs4):
                    ts0 = tstat[:, tt, h, 0:1]
                    ts3 = tstat[:, tt, h, 3:4]
                    ac(lambda e, ts0=ts0, ts3=ts3: e.activation(out=ts3, in_=ts0, func=AF.Exp), [("tstat", tt, h)], [("tstat3", tt, h)])
                for q, h in enumerate(hs4):
                    ts1 = tstat[:, tt, h, 1:2]
                    ts2 = tstat[:, tt, h, 2:3]
                    dv(lambda e, ts1=ts1, ts2=ts2: e.reciprocal(out=ts2, in_=ts1), [("tstat1", tt, h)], [("tstat2", tt, h)])
                for q, h in enumerate(hs4):
                    ts2 = tstat[:, tt, h, 2:3]
                    dv(lambda e, tt=tt, h=h, ts2=ts2: e.tensor_scalar(out=Dm[:, tt, h, :], in0=identb[:], scalar1=ts2, scalar2=None, op0=ALU.mult), ["identb", ("tstat2", tt, h)], ["Dm"])
        k.flush()
        if dbg and stop == "score" and hh_ == 0:
            dbg_dump("s12", s12[:], [128, 4, 2, 8, 128], ("s12", 0), F32)
            dbg_dump("tstat", tstat[:], [128, 4, 8, 4], ("tstat", 0, 0), F32)
            k.flush()
            return nc
        RP2.top = mark
        NPB = 6
        ub = [k.sb(f"ub{i}", [128, 16, 128], BF16, RP2) for i in range(2)]
        ga = [k.sb(f"ga{i}", [128, 4, 512], BF16, RP2) for i in range(2)]
        vbb = [k.sb(f"vbb{i}", [128, D], BF16, RP2) for i in range(8)]
        pexp = [k.sb(f"pexp{i}", [128, 4, 128], F32, RP2) for i in range(NPB)]
        gq = [k.sb(f"gq{i}", [128, 4, 128], BF16, RP2) for i in range(NPB)]
        gsum = [k.sb(f"gsum{i}", [128, 4, 128], BF16, RP2) for i in range(2)]
        wT = [k.sb(f"wT{i}", [128, 4, 128], BF16, RP2) for i in range(2)]
        a_st = k.sb("a_st", [128, 4, 512], F32, RP2)
        cnt = {"st": 0, "a": 0, "g": 0, "w": 0, "p": 0}

        def load_chunk(g, s_):
            e1 = g * 4 + s_
            sl = (g % 2) * 4 + s_
            k.dma(ub[s_ % 2][:], U_t[e1], writes=[("ub", s_ % 2)], chan=f"pu{s_ % 2}", eng="pool")
            k.dma(vbb[sl][:], Vp[e1 * 128:(e1 + 1) * 128, :], writes=[("vbb", sl)], chan=f"pv{sl}", eng="pool")

        def act_mm(g, s_, kcs, bk):
            for kc in kcs:
                k.op("pe", lambda e, s_=s_, kc=kc, bk=bk: e.matmul(out=banks[bk][:], lhsT=ub[s_ % 2][:, kc, :], rhs=hn2T[:, kc, :], start=(kc == 0), stop=(kc == 15)),
                     [("ub", s_ % 2), "hn2T"], [PS(bk)] if kc == 0 else [], wr_late=[PS(bk)] if kc else [])

        def act_gelu(g, s_, bk):
            par = g % 2
            dv(lambda e, s_=s_, bk=bk: e.tensor_copy(out=a_st[:, s_, :], in_=banks[bk][:]), [PS(bk)], [("a_st", s_)])
            if s_ == 3:
                ac(lambda e, par=par: e.activation(out=ga[par][:].rearrange("p a b -> p (a b)"), in_=a_st[:].rearrange("p a b -> p (a b)"), func=AF.Gelu),
                   [("a_st", q) for q in range(4)], [("ga", par, q) for q in range(4)])

        def act_chunk(g, s_):
            bk = cnt["a"] % 2
            cnt["a"] += 1
            act_mm(g, s_, range(16), bk)
            act_gelu(g, s_, bk)

        gb_ = 2
        pvT = banks[3][:].bitcast(BF16)

        def stage_a(g, tt, pre=None, adds=None):
            if pre is not None:
                bk = cnt["a"] % 2
                cnt["a"] += 1
            for h in range(8):
                pi = cnt["p"] % NPB
                cnt["p"] += 1
                for s_ in range(4):
                    e1 = g * 4 + s_
                    ac(lambda e, tt=tt, h=h, e1=e1, pi=pi, s_=s_: e.activation(out=pexp[pi][:, s_, :], in_=s12[:, tt, 1, h, :], func=AF.Exp, bias=s12[:, tt, 0, h, e1:e1 + 1]),
                       [("s12", tt)], [("pexp", pi, s_)])
                dv(lambda e, tt=tt, h=h, pi=pi: e.scalar_tensor_tensor(out=gq[pi][:], in0=pexp[pi][:], scalar=tstat[:, tt, h, 3:4], in1=pexp[pi][:], op0=ALU.is_ge, op1=ALU.mult),
                   [("tstat3", tt, h)] + [("pexp", pi, s_) for s_ in range(4)], [("gq", pi)])
                k.op("pe", lambda e, tt=tt, h=h, pi=pi: e.matmul(out=banks[gb_][:], lhsT=Dm[:, tt, h, :], rhs=gq[pi][:].rearrange("p a b -> p (a b)"), start=(h == 0), stop=(h == 7)),
                     [("gq", pi), "Dm"], [PS(gb_)] if h == 0 else [], wr_late=[PS(gb_)] if h else [])
                if pre is not None:
                    act_mm(pre[0], pre[1], (2 * h, 2 * h + 1), bk)
                if adds is not None and h >= 4:
                    stage_c(adds[0], adds[1], dcs=(h - 4,))
            if pre is not None:
                act_gelu(pre[0], pre[1], bk)

        def stage_b(g, tt):
            par = g % 2
            gi = cnt["g"] % 2
            cnt["g"] += 1
            ac(lambda e, gi=gi: e.activation(out=gsum[gi][:].rearrange("p a b -> p (a b)"), in_=banks[gb_][:], func=AF.Copy), [PS(gb_)], [("gsum", gi)])
            for s_ in range(4):
                k.op("pe", lambda e, gi=gi, s_=s_: e.transpose(out=pvT[:, s_ * 128:(s_ + 1) * 128], in_=gsum[gi][:, s_, :], identity=identb[:]),
                     [("gsum", gi), "identb"], [PS(3)] if s_ == 0 else [], wr_late=[PS(3)] if s_ else [])
            wi = cnt["w"] % 2
            cnt["w"] += 1
            dv(lambda e, par=par, tt=tt, wi=wi: e.tensor_tensor(out=wT[wi][:], in0=pvT[:, 0:512].rearrange("p (a b) -> p a b", a=4), in1=ga[par][:, :, tt * 128:(tt + 1) * 128], op=ALU.mult),
               [PS(3)] + [("ga", par, s_) for s_ in range(4)], [("wT", wi)])
            for s_ in range(4):
                sl = par * 4 + s_
                for dc in range(4):
                    k.op("pe", lambda e, sl=sl, s_=s_, dc=dc, wi=wi: e.matmul(out=banks[4 + dc][:], lhsT=wT[wi][:, s_, :], rhs=vbb[sl][:, dc * 512:(dc + 1) * 512], start=(s_ == 0), stop=(s_ == 3)),
                         [("wT", wi), ("vbb", sl)], [PS(4 + dc)] if s_ == 0 else [], wr_late=[PS(4 + dc)] if s_ else [])

        def stage_c(g, tt, dcs=range(4)):
            tg = hh_ * 4 + tt
            for dc in dcs:
                ds_ = slice(dc * 512, (dc + 1) * 512)
                dv(lambda e, tg=tg, dc=dc, ds_=ds_: e.tensor_tensor(out=x_res[:, tg, ds_], in0=banks[4 + dc][:], in1=x_res[:, tg, ds_], op=ALU.add),
                   [PS(4 + dc), ("x_res", tg)], [("x_res", tg)])

        for s_ in range(4):
            load_chunk(0, s_)
            act_chunk(0, s_)
        its = [(g, tt) for g in range(32) for tt in range(4)]
        stage_a(*its[0])
        for n_, (g, tt) in enumerate(its):
            pre = (g + 1, tt) if g + 1 < 32 else None
            if pre is not None:
                load_chunk(g + 1, tt)
            stage_b(g, tt)
            if n_ + 1 < len(its):
                stage_a(*its[n_ + 1], pre=pre, adds=(g, tt))
            else:
                stage_c(g, tt)
        k.flush()
    for tt in range(8):
        k.dma(y[tt * 128:(tt + 1) * 128, :], x_res[:, tt, :], reads=[("x_res", tt)], writes=[("y", tt)], chan="yo")
    k.flush()
    return nc


def _static_tables():
    i = np.arange(GW)
    d = i - 127
    n = np.maximum(d, 0)
    nf = np.maximum(n, 1).astype(np.float32)
    large = 16 + (np.log(nf / np.float32(16)) / np.float32(math.log(2048 / 16)) * np.float32(16)).astype(np.int32)
    large = np.minimum(large, 31)
    bucket = np.where(n < 16, n, large)
    mult = ((d <= 128).astype(np.float32) + ((d % 4 == 0) & (d <= 512)) + ((d % 16 == 0) & (d <= 2048)))
    mult = np.where((d >= 0) & (d <= 2047), mult, 0.0).astype(np.float32)
    ohm = np.zeros((32, GW), np.float32)
    ohm[bucket, i] = mult
    cm = np.zeros((4, 128, 128), np.float32)
    cm[0] = np.eye(128)
    cm[1] = np.eye(128)[::-1]
    cm[2, :64, :64] = 1.0
    cm[2, 64:, 64:] = 1.0
    cm[3] = 1.0
    return ohm, cm


def _pcol(v, n):
    return np.ascontiguousarray(np.asarray(v, np.float32).reshape(n, 128).T)


def _prep(inp, stage=None):
    f = lambda a: np.ascontiguousarray(np.asarray(a, dtype=np.float32))
    x = f(inp["x"])
    ohm, cm = _static_tables()
    w_in_t = np.ascontiguousarray(f(inp["w_in"]).reshape(16, 128, 32, 128).transpose(2, 1, 0, 3))
    gq = np.tile(f(inp["q_norm_g"]), 2)
    gk = np.tile(f(inp["k_norm_g"]), 2)
    shared = {
        "gmix": _pcol(inp["norm_mix_g"], 16),
        "w_in_t": w_in_t,
        "gqk": np.ascontiguousarray(np.stack([gq, gk], axis=1)),
        "relb": f(inp["rel_bias"]),
        "ohm": ohm,
        "cmats": cm,
        "gmixed": np.ascontiguousarray(np.concatenate([_pcol(inp["attn_out_g"], 8), _pcol(inp["ssm_out_g"], 8)], axis=1)),
    }
    def lane(a):
        a = f(a)
        rest = a.shape[2:]
        return np.ascontiguousarray(a.reshape((32, 2, 64) + rest).transpose((1, 2, 0) + tuple(range(3, 3 + len(rest)))).reshape((128, 32) + rest))
    ldt = np.broadcast_to(f(inp["ssm_log_dt"])[:, None], (64, 64))
    shared["lam_l"] = np.ascontiguousarray(np.stack([lane(inp["ssm_lambda_re"]), lane(inp["ssm_lambda_im"]), lane(ldt)]))
    shared["bc_l"] = np.ascontiguousarray(np.stack([lane(inp["ssm_b_re"]), lane(inp["ssm_b_im"]),
                                                    lane(f(inp["ssm_c_re"]).transpose(0, 2, 1)), lane(f(inp["ssm_c_im"]).transpose(0, 2, 1))]))
    shared["dg_l"] = np.ascontiguousarray(np.concatenate([_pcol(f(inp["ssm_d"]).reshape(-1), 8), _pcol(inp["ssm_glu_b"], 8)], axis=1))
    shared["glu_w"] = f(inp["ssm_glu_w"])
    shared["tidx"] = np.ascontiguousarray(np.broadcast_to(np.arange(512, dtype=np.float32)[None, :], (128, 512)))
    shared["w_out"] = f(inp["w_out"])
    shared["gffn"] = _pcol(inp["norm_ffn_g"], 16)
    shared["wq_t"] = np.ascontiguousarray(f(inp["peer_w_q"]).reshape(16, 128, 16, 128).transpose(2, 1, 0, 3))
    k1 = f(inp["peer_keys1"]).transpose(2, 0, 1)
    k2 = f(inp["peer_keys2"]).transpose(2, 0, 1)
    shared["keysT"] = np.ascontiguousarray(np.concatenate([k1, k2], axis=1))
    shared["U_t"] = np.ascontiguousarray(f(inp["peer_u"]).reshape(128, 128, 16, 128).transpose(0, 3, 2, 1))
    shared["Vp"] = f(inp["peer_v"])
    maps = []
    for c in range(8):
        b, p = c // 2, c % 2
        xa = np.zeros((2048, D), np.float32)
        if p == 1:
            xa[:] = x[b]
        else:
            xa[1024:] = x[b, :1024]
        kv = np.ones((128, 16), np.float32)
        if p == 0:
            kv[:, :8] = 0.0
        m = dict(shared)
        m["x_all"] = xa
        m["kvalid"] = kv
        maps.append(m)
    return maps


_NC_CACHE = {}


def kernel(**inputs):
    if "nc" not in _NC_CACHE:
        _NC_CACHE["nc"] = build()
    nc = _NC_CACHE["nc"]
    maps = _prep(inputs)
    res = run_bass_kernel_spmd(nc, maps, core_ids=list(range(8)))
    out = np.zeros((4, 2048, D), np.float32)
    for c in range(8):
        b, p = c // 2, c % 2
        out[b, p * 1024:(p + 1) * 1024] = res.results[c]["y"]
    return out
```
